# Optimizing a Trainium2 kernel written in Bass

```python
import jax, jax.numpy as jnp
from jax import lax
import numpy as np

D_MODEL = 2048
BATCH = 2
SEQ = 16384
DEPTH = 2

GRID_W = 64
CTX_LEN = 256

RWKV_HEADS = 16
RWKV_HEAD = 64
RWKV_WIDTH = RWKV_HEADS * RWKV_HEAD
DECAY_LORA = 64
AAA_LORA = 64
GATE_LORA = 160
GN_EPS = 64e-5
ATT_Q_HEADS = 16
ATT_KV_HEADS = 4
ATT_HEAD = 64
ATT_GROUP = ATT_Q_HEADS // ATT_KV_HEADS
ATT_WIDTH = ATT_Q_HEADS * ATT_HEAD
ATT_KV_WIDTH = ATT_KV_HEADS * ATT_HEAD
ATT_SCALE = ATT_HEAD ** -0.5
WINDOW = 128
BLOCK = 128
ROPE_THETA = 10000.0
RWKV_COLS = 3 * RWKV_WIDTH + 2 * DECAY_LORA + 2 * AAA_LORA + GATE_LORA
IN_COLS = RWKV_COLS + ATT_WIDTH + 2 * ATT_KV_WIDTH
MIX_WIDTH = RWKV_WIDTH + ATT_WIDTH
POOL_WINDOWS = (2, 4, 8, 16)
POOL_GROUP = D_MODEL // 4
N_EXPERTS = 16
CAPACITY_FACTOR = 2
EXPERT_FF = D_MODEL // 2
NORM_EPS = 1e-6
N_EVEN = (DEPTH + 1) // 2
N_ODD = DEPTH // 2

kernel_name = "hybrid_rwkv7_swa_pool_ecmoe_diffusion"


def rms_norm(x, gain):
    x32 = x.astype(jnp.float32)
    y = x32 * lax.rsqrt(jnp.mean(x32 * x32, axis=-1, keepdims=True) + NORM_EPS)
    return (y * gain.astype(jnp.float32)).astype(x.dtype)


def centred_shift(p):
    zero = jnp.zeros_like(p[:, :1])
    prev = jnp.concatenate([zero, p[:, :-1]], axis=1)
    nxt = jnp.concatenate([p[:, 1:], zero], axis=1)
    return 0.5 * (prev + nxt)


def axial_angles(length):
    rows = length // GRID_W
    row = jnp.repeat(jnp.arange(rows, dtype=jnp.float32), GRID_W)
    col = jnp.tile(jnp.arange(GRID_W, dtype=jnp.float32), rows)
    n_freq = ATT_HEAD // 4
    inv = ROPE_THETA ** (-jnp.arange(n_freq, dtype=jnp.float32) / n_freq)
    return row[:, None] * inv, col[:, None] * inv


def rotate_half(x, ang):
    x1, x2 = jnp.split(x, 2, axis=-1)
    cos = jnp.cos(ang)[None, :, None, :].astype(x.dtype)
    sin = jnp.sin(ang)[None, :, None, :].astype(x.dtype)
    return jnp.concatenate([x1 * cos - x2 * sin, x2 * cos + x1 * sin], axis=-1)


def rope_2d(x, ang_row, ang_col):
    xr, xc = jnp.split(x, 2, axis=-1)
    return jnp.concatenate([rotate_half(xr, ang_row), rotate_half(xc, ang_col)], axis=-1)


def split_heads(t):
    return t.reshape(t.shape[:-1] + (RWKV_HEADS, RWKV_HEAD))


def rwkv_inputs(p, mu):
    m = p + mu * (centred_shift(p) - p)
    sizes = [RWKV_WIDTH] * 3 + [DECAY_LORA] * 2 + [AAA_LORA] * 2 + [GATE_LORA]
    return jnp.split(m, np.cumsum(sizes)[:-1].tolist(), axis=-1)


def rwkv_prep(r, k, v, wd, ad, w0, w2, a0, a2, k_k, k_a):
    w_log = -jax.nn.softplus(-(w0 + jnp.tanh(wd) @ w2).astype(jnp.float32)) - 0.5
    decay = jnp.exp(-jnp.exp(w_log))
    a = jax.nn.sigmoid((a0 + ad @ a2).astype(jnp.float32))
    k32 = k.astype(jnp.float32)
    kk = split_heads(k32 * k_k.astype(jnp.float32))
    kk = kk * lax.rsqrt(jnp.maximum(jnp.sum(kk * kk, axis=-1, keepdims=True), 1e-24))
    k_mod = k32 * (1.0 + (a - 1.0) * k_a.astype(jnp.float32))
    return (split_heads(r.astype(jnp.float32)), split_heads(decay), split_heads(k_mod),
            split_heads(v.astype(jnp.float32)), kk, split_heads(a))


def wkv_step(state, inp):
    r, w, k, v, kk, a = inp
    sa = jnp.einsum('bhvk,bhk->bhv', state, kk)
    state = (state * w[:, :, None, :] - sa[..., None] * (kk * a)[:, :, None, :]
             + v[..., None] * k[:, :, None, :])
    return state, jnp.einsum('bhvk,bhk->bhv', state, r)


def wkv_scan(inputs, state0, reverse):
    xs = tuple(jnp.swapaxes(t, 0, 1) for t in inputs)
    state, ys = lax.scan(wkv_step, state0, xs, reverse=reverse)
    return state, jnp.swapaxes(ys, 0, 1)


def rwkv_bonus(q, r_k_h):
    r, _, k, v, _, _ = q
    return jnp.sum(r * k * r_k_h, axis=-1, keepdims=True) * v


def rwkv_finish(y, bonus, gd, g2, ln_w, ln_b, dtype):
    b, t = y.shape[:2]
    mean = jnp.mean(y, axis=-1, keepdims=True)
    var = jnp.mean(jnp.square(y - mean), axis=-1, keepdims=True)
    yn = ((y - mean) * lax.rsqrt(var + GN_EPS)).reshape(b, t, RWKV_WIDTH)
    yn = yn * ln_w.astype(jnp.float32) + ln_b.astype(jnp.float32)
    g = (jax.nn.sigmoid(gd) @ g2).astype(jnp.float32)
    return ((yn + bonus.reshape(b, t, RWKV_WIDTH)) * g).astype(dtype)


def att_split(p):
    o = RWKV_COLS
    q = p[..., o:o + ATT_WIDTH]
    k = p[..., o + ATT_WIDTH:o + ATT_WIDTH + ATT_KV_WIDTH]
    v = p[..., o + ATT_WIDTH + ATT_KV_WIDTH:]
    sh = p.shape[:2]
    return (q.reshape(sh + (ATT_Q_HEADS, ATT_HEAD)), k.reshape(sh + (ATT_KV_HEADS, ATT_HEAD)),
            v.reshape(sh + (ATT_KV_HEADS, ATT_HEAD)))


def window_attention(q, k, v, kc, vc, sink):
    b, length = q.shape[:2]
    n_ctx = kc.shape[1]
    nb = length // BLOCK
    span = BLOCK + 2 * WINDOW
    qb = q.reshape(b, nb, BLOCK, ATT_KV_HEADS, ATT_GROUP, ATT_HEAD).swapaxes(0, 1)
    pad = ((0, 0), (WINDOW, WINDOW), (0, 0), (0, 0))
    kp = jnp.pad(k, pad)
    vp = jnp.pad(v, pad)
    sink_l = sink.reshape(ATT_KV_HEADS, ATT_GROUP)[None, :, :, None, None].astype(jnp.float32)

    def one_block(args):
        q_blk, n = args
        start = n * BLOCK
        kw = lax.dynamic_slice_in_dim(kp, start, span, axis=1)
        vw = lax.dynamic_slice_in_dim(vp, start, span, axis=1)
        s_c = jnp.einsum('bqkgd,bjkd->bkgqj', q_blk, kc, preferred_element_type=jnp.float32) * ATT_SCALE
        s_w = jnp.einsum('bqkgd,bjkd->bkgqj', q_blk, kw, preferred_element_type=jnp.float32) * ATT_SCALE
        qpos = start + jnp.arange(BLOCK)
        kpos = start - WINDOW + jnp.arange(span)
        ok = ((jnp.abs(qpos[:, None] - kpos[None, :]) <= WINDOW)
              & (kpos >= 0)[None, :] & (kpos < length)[None, :])
        s_w = jnp.where(ok, s_w, -jnp.inf)
        s_sink = jnp.broadcast_to(sink_l, s_c.shape[:-1] + (1,))
        p = jax.nn.softmax(jnp.concatenate([s_c, s_w, s_sink], axis=-1), axis=-1).astype(v.dtype)
        return (jnp.einsum('bkgqj,bjkd->bqkgd', p[..., :n_ctx], vc)
                + jnp.einsum('bkgqj,bjkd->bqkgd', p[..., n_ctx:n_ctx + span], vw))

    out = lax.map(one_block, (qb, jnp.arange(nb)))
    return out.swapaxes(0, 1).reshape(b, length, ATT_WIDTH)


def context_attention(qc, kc, vc, sink):
    b, n_ctx = qc.shape[:2]
    qg = qc.reshape(b, n_ctx, ATT_KV_HEADS, ATT_GROUP, ATT_HEAD)
    s = jnp.einsum('bqkgd,bjkd->bkgqj', qg, kc, preferred_element_type=jnp.float32) * ATT_SCALE
    sink_l = sink.reshape(ATT_KV_HEADS, ATT_GROUP)[None, :, :, None, None].astype(jnp.float32)
    s_sink = jnp.broadcast_to(sink_l, s.shape[:-1] + (1,))
    p = jax.nn.softmax(jnp.concatenate([s, s_sink], axis=-1), axis=-1)[..., :n_ctx].astype(vc.dtype)
    return jnp.einsum('bkgqj,bjkd->bqkgd', p, vc).reshape(b, n_ctx, ATT_WIDTH)


def even_mixer(hx, hc, w_in, mu, w_out, w0, w2, a0, a2, g2, k_k, k_a, r_k, ln_w, ln_b, sink,
               ang_row, ang_col, want_ctx):
    px = hx @ w_in
    pc = hc @ w_in
    rx = rwkv_inputs(px[..., :RWKV_COLS], mu)
    rc = rwkv_inputs(pc[..., :RWKV_COLS], mu)
    zero_state = jnp.zeros((hx.shape[0], RWKV_HEADS, RWKV_HEAD, RWKV_HEAD), jnp.float32)
    r_k_h = r_k.reshape(RWKV_HEADS, RWKV_HEAD).astype(jnp.float32)
    y_x, b_x, y_c, b_c = 0.0, 0.0, 0.0, 0.0
    for d in range(2):
        rev = d == 1
        qx = rwkv_prep(rx[0], rx[1], rx[2], rx[3 + d], rx[5 + d], w0[d], w2[d], a0[d], a2[d], k_k, k_a)
        qc = rwkv_prep(rc[0], rc[1], rc[2], rc[3 + d], rc[5 + d], w0[d], w2[d], a0[d], a2[d], k_k, k_a)
        state_c, yc_d = wkv_scan(qc, zero_state, rev)
        _, yx_d = wkv_scan(qx, state_c, rev)
        y_x = y_x + yx_d
        b_x = b_x + rwkv_bonus(qx, r_k_h)
        if want_ctx:
            y_c = y_c + yc_d
            b_c = b_c + rwkv_bonus(qc, r_k_h)
    rwkv_x = rwkv_finish(y_x, b_x, rx[7], g2, ln_w, ln_b, hx.dtype)

    q_x, k_x, v_x = att_split(px)
    q_c, k_c, v_c = att_split(pc)
    q_x = rope_2d(q_x, ang_row, ang_col)
    k_x = rope_2d(k_x, ang_row, ang_col)
    att_x = window_attention(q_x, k_x, v_x, k_c, v_c, sink)
    out_x = jnp.concatenate([rwkv_x, att_x], axis=-1) @ w_out
    if not want_ctx:
        return out_x, None
    rwkv_c = rwkv_finish(y_c, b_c, rc[7], g2, ln_w, ln_b, hc.dtype)
    att_c = context_attention(q_c, k_c, v_c, sink)
    return out_x, jnp.concatenate([rwkv_c, att_c], axis=-1) @ w_out


def pool_mixer(h, pool_w, pool_scale):
    b, t, d = h.shape
    h32 = h.astype(jnp.float32)
    cs = jnp.concatenate([jnp.zeros((b, 1, d), jnp.float32), jnp.cumsum(h32, axis=1)], axis=1)
    pos = jnp.arange(t)
    outs = []
    for g, w in enumerate(POOL_WINDOWS):
        sl = slice(g * POOL_GROUP, (g + 1) * POOL_GROUP)
        lo = jnp.clip(pos - w // 2, 0, t)
        hi = jnp.clip(pos + w // 2, 0, t)
        cnt = (hi - lo).astype(jnp.float32)[None, :, None]
        cs_g = cs[..., sl]
        mean = (cs_g[:, hi] - cs_g[:, lo]) / cnt
        outs.append((mean - h32[..., sl]).astype(h.dtype) @ pool_w[g])
    return jnp.concatenate(outs, axis=-1) * pool_scale


def expert_choice_ffn(h, router_w, w_gate, w_up, w_down):
    b, t, _ = h.shape
    cap = max(1, CAPACITY_FACTOR * t // N_EXPERTS)
    aff = jax.nn.softmax(jnp.einsum('btd,de->bte', h, router_w, preferred_element_type=jnp.float32), axis=-1)
    gate, idx = lax.top_k(jnp.swapaxes(aff, 1, 2), cap)
    b_idx = jnp.arange(b)[:, None, None]
    xs = h[b_idx, idx]
    hid = jax.nn.silu(jnp.einsum('becd,edf->becf', xs, w_gate)) * jnp.einsum('becd,edf->becf', xs, w_up)
    y = jnp.einsum('becf,efd->becd', hid, w_down) * gate[..., None].astype(h.dtype)
    return jnp.zeros_like(h).at[b_idx, idx].add(y)


def setup_inputs(seed: int = 0) -> dict:
    key = jax.random.key(seed)
    ks = iter(jax.random.split(key, 40))
    D = D_MODEL

    def nrm(shape, s):
        return jax.random.normal(next(ks), shape, jnp.float32) * s

    def unif(shape, lo, hi):
        return jax.random.uniform(next(ks), shape, jnp.float32, minval=lo, maxval=hi)

    return {
        "x": nrm((BATCH, SEQ, D), 1.0),
        "c": nrm((BATCH, D), 1.0),
        "ctx": nrm((BATCH, CTX_LEN, D), 1.0),
        "c_ctx": nrm((D,), 1.0),
        "ada_w": nrm((DEPTH, D, 6 * D), 0.5 * D ** -0.5),
        "ada_b": nrm((DEPTH, 6 * D), 0.02),
        "norm_mix": 1.0 + nrm((DEPTH, D), 0.01),
        "norm_ffn": 1.0 + nrm((DEPTH, D), 0.01),
        "w_in": nrm((N_EVEN, D, IN_COLS), D ** -0.5),
        "shift_mu": unif((N_EVEN, RWKV_COLS), 0.0, 1.0),
        "decay_w0": unif((N_EVEN, 2, RWKV_WIDTH), -5.0, 1.0),
        "decay_w2": nrm((N_EVEN, 2, DECAY_LORA, RWKV_WIDTH), 0.5 * DECAY_LORA ** -0.5),
        "iclr_a0": nrm((N_EVEN, 2, RWKV_WIDTH), 0.5),
        "iclr_a2": nrm((N_EVEN, 2, AAA_LORA, RWKV_WIDTH), 0.5 * AAA_LORA ** -0.5),
        "gate_g2": nrm((N_EVEN, GATE_LORA, RWKV_WIDTH), GATE_LORA ** -0.5),
        "k_k": 0.85 + nrm((N_EVEN, RWKV_WIDTH), 0.05),
        "k_a": 1.0 + nrm((N_EVEN, RWKV_WIDTH), 0.05),
        "r_k": nrm((N_EVEN, RWKV_WIDTH), 0.1),
        "ln_w": 1.0 + nrm((N_EVEN, RWKV_WIDTH), 0.05),
        "ln_b": nrm((N_EVEN, RWKV_WIDTH), 0.01),
        "sink": nrm((N_EVEN, ATT_Q_HEADS), 1.0),
        "w_out": nrm((N_EVEN, MIX_WIDTH, D), MIX_WIDTH ** -0.5),
        "pool_w": nrm((N_ODD, len(POOL_WINDOWS), POOL_GROUP, POOL_GROUP), POOL_GROUP ** -0.5),
        "pool_scale": 1.0 + nrm((N_ODD, D), 0.1),
        "router_w": nrm((DEPTH, D, N_EXPERTS), D ** -0.5),
        "exp_w_gate": nrm((DEPTH, N_EXPERTS, D, EXPERT_FF), D ** -0.5),
        "exp_w_up": nrm((DEPTH, N_EXPERTS, D, EXPERT_FF), D ** -0.5),
        "exp_w_down": nrm((DEPTH, N_EXPERTS, EXPERT_FF, D), EXPERT_FF ** -0.5),
        "norm_final": 1.0 + nrm((D,), 0.01),
    }


def reference(x, c, ctx, c_ctx, ada_w, ada_b, norm_mix, norm_ffn, w_in, shift_mu, decay_w0, decay_w2,
              iclr_a0, iclr_a2, gate_g2, k_k, k_a, r_k, ln_w, ln_b, sink, w_out, pool_w, pool_scale,
              router_w, exp_w_gate, exp_w_up, exp_w_down, norm_final):
    ang_row, ang_col = axial_angles(x.shape[1])
    for l in range(DEPTH):
        want_ctx = any(j % 2 == 0 for j in range(l + 1, DEPTH))
        sh_a, sc_a, gt_a, sh_f, sc_f, gt_f = jnp.split(
            (jax.nn.silu(c) @ ada_w[l] + ada_b[l])[:, None, :], 6, axis=-1)
        hx = rms_norm(x, norm_mix[l]) * (1.0 + sc_a) + sh_a
        if l % 2 == 0 or want_ctx:
            csh_a, csc_a, cgt_a, csh_f, csc_f, cgt_f = jnp.split(
                jax.nn.silu(c_ctx) @ ada_w[l] + ada_b[l], 6, axis=-1)
            hc = rms_norm(ctx, norm_mix[l]) * (1.0 + csc_a) + csh_a
        if l % 2 == 0:
            e = l // 2
            yx, yc = even_mixer(hx, hc, w_in[e], shift_mu[e], w_out[e], decay_w0[e], decay_w2[e],
                                iclr_a0[e], iclr_a2[e], gate_g2[e], k_k[e], k_a[e], r_k[e], ln_w[e],
                                ln_b[e], sink[e], ang_row, ang_col, want_ctx)
        else:
            o = l // 2
            yx = pool_mixer(hx, pool_w[o], pool_scale[o])
            yc = pool_mixer(hc, pool_w[o], pool_scale[o]) if want_ctx else None
        x = x + gt_a * yx
        x = x + gt_f * expert_choice_ffn(rms_norm(x, norm_ffn[l]) * (1.0 + sc_f) + sh_f,
                                         router_w[l], exp_w_gate[l], exp_w_up[l], exp_w_down[l])
        if want_ctx:
            ctx = ctx + cgt_a * yc
            ctx = ctx + cgt_f * expert_choice_ffn(rms_norm(ctx, norm_ffn[l]) * (1.0 + csc_f) + csh_f,
                                                  router_w[l], exp_w_gate[l], exp_w_up[l], exp_w_down[l])
    return rms_norm(x, norm_final)
```

```python
import numpy as np
from contextlib import ExitStack
import concourse.bass as bass
import concourse.mybir as mybir
from concourse.bass_utils import run_bass_kernel_spmd

F32 = mybir.dt.float32
BF16 = mybir.dt.bfloat16
I32 = mybir.dt.int32
U32 = mybir.dt.uint32
AF = mybir.ActivationFunctionType
ALU = mybir.AluOpType
AX = mybir.AxisListType

D = 2048
KC = D // 128
NORM_EPS = 1e-6
GN_EPS = 64e-5
C0 = float(np.exp(-0.5))
NF = 1760
NB = 896
RSTOP = None
RVAR = 0
SKIPP = False
NSTREAM = 2


class Tracker:
    EPOCH = 30000
    NDMA = 24

    def __init__(self, nc, stack):
        self.nc = nc
        self.stack = stack
        self.engines = {'pe': nc.tensor, 'act': nc.scalar, 'dve': nc.vector,
                        'pool': nc.gpsimd, 'sp': nc.sync}
        self.cur = {}
        self.nsem = 0
        self.seen = {e: {} for e in self.engines}
        self.regs = {}
        self.dsems = []
        self.dnext = 0
        self.n_inst = 0
        self.rr = 0

    def _newsem(self, tag):
        self.nsem += 1
        return self.stack.enter_context(self.nc.semaphore(f"s{self.nsem}_{tag}"))

    def sb(self, name, shape, dtype):
        return self.stack.enter_context(self.nc.sbuf_tensor(name, shape, dtype))

    def ps(self, name, shape, dtype):
        return self.stack.enter_context(self.nc.psum_tensor(name, shape, dtype))

    def _tick(self, e):
        c = self.cur.get(e)
        if c is None or c[2] >= self.EPOCH:
            ep = 0 if c is None else c[3] + 1
            c = [(e, ep), self._newsem(f"{e}{ep}"), 0, ep]
            self.cur[e] = c
        c[2] += 1
        return (c[0], c[1], c[2])

    def _wait(self, e, dep):
        key, sem, cnt = dep
        if e == 'pe' and key[0] == 'pe':
            return
        if self.seen[e].get(key, 0) >= cnt:
            return
        self.engines[e].wait_ge(sem, cnt)
        self.seen[e][key] = cnt

    def _deps(self, e, reads, writes):
        for r in reads:
            info = self.regs.get(r)
            if info and info['w']:
                self._wait(e, info['w'])
            if info and r.startswith('ps'):
                for rd in info['r']:
                    if rd[0][0] != e:
                        self._wait(e, rd)
        for w in writes:
            info = self.regs.get(w)
            if info:
                if info['w']:
                    self._wait(e, info['w'])
                for rd in info['r']:
                    self._wait(e, rd)

    def _record(self, tok, reads, writes):
        for r in reads:
            info = self.regs.setdefault(r, {'w': None, 'r': []})
            info['r'] = [x for x in info['r'] if x[0] != tok[0]] + [tok]
        for w in writes:
            self.regs[w] = {'w': tok, 'r': []}

    def op(self, e, fn, reads=(), writes=()):
        self._deps(e, reads, writes)
        tok = self._tick(e)
        inst = fn()
        inst.then_inc(tok[1], 1)
        self._record(tok, reads, writes)
        self.n_inst += 1
        return inst

    def dma(self, e, out, in_, reads=(), writes=(), **kw):
        self._deps(e, reads, writes)
        if len(self.dsems) < self.NDMA:
            self.dsems.append([('d', len(self.dsems)), self._newsem(f"d{len(self.dsems)}"), 0])
            d = self.dsems[-1]
        else:
            d = self.dsems[self.dnext % self.NDMA]
        self.dnext += 1
        if d[2] > 0:
            self._wait(e, (d[0], d[1], d[2]))
        d[2] += 16
        tok = (d[0], d[1], d[2])
        inst = self.engines[e].dma_start(out=out, in_=in_, **kw)
        inst.then_inc(d[1], 16)
        self._record(tok, reads, writes)
        self.n_inst += 1
        return inst

    def idma(self, out, out_offset, in_, in_offset, reads=(), writes=(), **kw):
        e = 'pool'
        self._deps(e, reads, writes)
        if not hasattr(self, 'isems'):
            self.isems = []
            self.inext = 0
        if len(self.isems) < 8:
            self.isems.append([('i', len(self.isems)), self._newsem(f"i{len(self.isems)}"), 0])
            d = self.isems[-1]
        else:
            d = self.isems[self.inext % 8]
        self.inext += 1
        if d[2] > 0:
            self._wait(e, (d[0], d[1], d[2]))
        d[2] += 16
        tok = (d[0], d[1], d[2])
        inst = self.nc.gpsimd.indirect_dma_start(out=out, out_offset=out_offset, in_=in_, in_offset=in_offset, **kw)
        inst.then_inc(d[1], 16)
        self._record(tok, reads, writes)
        self.n_inst += 1
        return inst

    def finish(self):
        e = 'sp'
        for d in self.dsems + getattr(self, 'isems', []):
            if d[2] > 0:
                self._wait(e, (d[0], d[1], d[2]))
        for k, c in self.cur.items():
            if k != e:
                self._wait(e, (c[0], c[1], c[2]))


class Scope:
    UID = 0
    def __init__(self, nc, T):
        self.nc = nc
        self.T = T
        self.st = ExitStack()
        self.n = 0

    def sb(self, name, shape, dtype=F32):
        self.n += 1
        Scope.UID += 1
        return self.st.enter_context(self.nc.sbuf_tensor(f"{name}__{Scope.UID}", shape, dtype))

    def ps(self, name, shape, dtype=F32):
        Scope.UID += 1
        return self.st.enter_context(self.nc.psum_tensor(f"{name}__{Scope.UID}", shape, dtype))

    def sb_once(self, name, shape, dtype=F32):
        if not hasattr(self, "_once"):
            self._once = {}
        if name not in self._once:
            self._once[name] = self.sb(name, shape, dtype)
        return self._once[name]

    def close(self):
        self.st.close()


def _barrier(T):
    toks = [(c[0], c[1], c[2]) for c in T.cur.values()]
    dtoks = [(d[0], d[1], d[2]) for d in T.dsems + getattr(T, 'isems', []) if d[2] > 0]
    for e in T.engines:
        for t in toks + dtoks:
            if t[0][0] != e:
                T._wait(e, t)
            elif e != 'pe':
                T._wait(e, t)
    T.regs = {}


Tracker.barrier = _barrier


def build_all(SEQ, CTX, dbg=False, upto=None):
    nc = bass.Bass("TRN2", target_bir_lowering=False)
    TOT = CTX + SEQ
    CAP = 2 * SEQ // 16
    NBLK = SEQ // 128
    XR = 2080
    assert CAP % 128 == 0 and SEQ % 512 == 0 and CTX % 256 == 0

    def din(name, shape, dt=F32):
        return nc.dram_tensor(name, shape, dt, kind="ExternalInput").ap()

    def scr(name, shape, dt=F32):
        return nc.dram_tensor(name, shape, dt, kind=("ExternalOutput" if dbg else "Internal")).ap()

    x_in = din("x", [SEQ, D]); c_in = din("ctx", [CTX, D])
    cvec = din("cvec", [128, KC, 2])
    ada_w = din("ada_w", [2, D, 6 * D]); ada_b = din("ada_b", [2, 1, 6 * D])
    nrm = din("nrm", [6, 1, D])
    Wf_all = din("Wf_all", [4, D, NF]); Wb_all = din("Wb_all", [4, D, NB])
    vec_all = din("vec_all", [4, 128, 32]); w2a2_all = din("w2a2_all", [4, 128, 2, 2, 128])
    g2a_all = din("g2a_all", [4, 128, 256]); g2b_all = din("g2b_all", [4, 32, 256])
    mugd = din("mugd", [128, 2]); sinkb_all = din("sinkb_all", [4, 128, 4])
    cosT = din("cosT", [128, SEQ]); sinT = din("sinT", [128, SEQ])
    cst = din("cst", [128, 9, 128]); cst2 = din("cst2", [128, 4, 128])
    w_out = din("w_out", [D, D])
    pool_w = din("pool_w", [4, 512, 512]); invcnt = din("invcnt", [4, 1, SEQ])
    router_w = din("router_w", [2, D, 16])
    wg = din("exp_w_gate", [2, 16, D, 1024]); wu = din("exp_w_up", [2, 16, D, 1024]); wd = din("exp_w_down", [2, 16, 1024, D])
    out = nc.dram_tensor("out", [SEQ, D], F32, kind="ExternalOutput").ap()

    modrow = scr("modrow", [2, 2, 6 * D])
    hxf = scr("hxf", [D, TOT], BF16); hxr = scr("hxr", [D, TOT], BF16)
    pxf = scr("pxf", [NF, TOT]); pxb = scr("pxb", [NB, TOT])
    yT = scr("yT", [2, 256, SEQ]); bT = scr("bT", [2, 256, SEQ])
    mixs = scr("mixs", [D, SEQ])
    x1 = scr("x1", [SEQ, D]); acc0 = scr("acc0", [SEQ, D]); x3 = scr("x3", [SEQ, D]); acc1 = scr("acc1", [SEQ, D])
    hrow = scr("hrow", [SEQ, XR]); affT = scr("affT", [16, SEQ]); slot_d = scr("slot_d", [16, SEQ])
    Xg = [scr(f"Xg{e}", [CAP + 128, XR]) for e in range(16)]
    hT1 = scr("hT1", [D, SEQ])

    with ExitStack() as st:
        T = Tracker(nc, st)
        G = Scope(nc, T)
        V = nc.vector; A = nc.scalar; P = nc.gpsimd; PE = nc.tensor

        cs = G.sb("cst_sb", [128, 9, 128])
        T.dma('sp', cs[:], cst, writes=['cst'])
        ident = cs[:, 0, :]; onesbd = cs[:, 1, :]; maskA = cs[:, 2, :]; maskB = cs[:, 3, :]
        bdm = cs[:, 4, :]
        JJ = [cs[:, 5, 0:64], cs[:, 5, 64:128]]
        headsel = cs[:, 6, 0:2]
        triLO = cs[:, 7, :]; triHI = cs[:, 8, :]
        cs2 = G.sb("cst2_sb", [128, 4, 128])
        T.dma('sp', cs2[:], cst2, writes=['cst2'])
        J128 = cs2[:, 0, :]; ones8 = cs2[:, 1, :]; low8 = cs2[:, 2, :]
        onesbf = G.sb("onesbf", [128, 128], BF16)
        T.op('pool', lambda: P.memset(onesbf[:], 1.0), writes=['onesbf'])
        ones64 = G.sb("ones64", [128, 128])
        T.op('dve', lambda: V.tensor_scalar(ones64[:], onesbd, 1.0 / 64, None, ALU.mult), reads=['cst'], writes=['ones64'])
        iota_p = G.sb("iota_p", [128, 1])
        T.op('pool', lambda: P.iota(iota_p[:], pattern=[[0, 1]], base=0, channel_multiplier=1, allow_small_or_imprecise_dtypes=True), writes=['iota_p'])
        vec_sb = G.sb("vec_sb", [128, 32])
        omu = G.sb("omu", [128, 10]); hmu = G.sb("hmu", [128, 10])
        mugd_sb = G.sb("mugd_sb", [128, 2])
        T.dma('sp', mugd_sb[:], mugd, writes=['mugd'])

        def bcast_row(tile_ap, row_ap, name):
            T.dma('sp', tile_ap, row_ap.partition_broadcast(128), reads=[name], writes=[name])

        def rms_tok(S, xin, xn, w_, gname, geff, shrow, out_t, on, eps=NORM_EPS):
            ss = S.sb_once("rms_ss", [128, 1]); sq = S.sb_once("rms_sq", [128, D])
            T.op('act', lambda: A.activation(sq[:], xin[:], AF.Square, accum_out=ss[:]), reads=[xn], writes=['rms_sq', 'rms_ss'])
            T.op('act', lambda: A.activation(ss[:], ss[:], AF.Sqrt, bias=eps, scale=1.0 / D), reads=['rms_ss'], writes=['rms_ss'])
            T.op('dve', lambda: V.reciprocal(ss[:], ss[:]), reads=['rms_ss'], writes=['rms_ss'])
            if shrow is None:
                T.op('dve', lambda: V.scalar_tensor_tensor(out_t[:], xin[:], ss[:, 0:1], geff[:], ALU.mult, ALU.mult), reads=[xn, 'rms_ss', gname], writes=[on])
            else:
                T.op('dve', lambda: V.scalar_tensor_tensor(sq[:], xin[:], ss[:, 0:1], geff[:], ALU.mult, ALU.mult), reads=[xn, 'rms_ss', gname, 'rms_sq'], writes=['rms_sq'])
                T.op('pool', lambda: P.tensor_tensor(out_t[:], sq[:], shrow[:], op=ALU.add), reads=['rms_sq', gname], writes=[on])

        def mod_rows(S, l, who, idx_sc, idx_sh, nrm_i, gname):
            geff = S.sb(gname + "_g", [128, D]); shr = S.sb(gname + "_s", [128, D]); nw = S.sb(gname + "_n", [128, D])
            bcast_row(geff[:], modrow[l, who:who + 1, idx_sc * D:(idx_sc + 1) * D], gname)
            bcast_row(shr[:], modrow[l, who:who + 1, idx_sh * D:(idx_sh + 1) * D], gname)
            bcast_row(nw[:], nrm[nrm_i], gname)
            T.op('dve', lambda: V.scalar_tensor_tensor(geff[:], geff[:], 1.0, nw[:], ALU.add, ALU.mult), reads=[gname], writes=[gname])
            return geff, shr

        def phase_mod():
            S = Scope(nc, T)
            cv = S.sb("cv", [128, KC, 2])
            T.dma('sp', cv[:], cvec, writes=['cv'])
            T.op('act', lambda: A.activation(cv[:], cv[:], AF.Silu), reads=['cv'], writes=['cv'])
            wt = [S.sb(f"adaw{i}", [128, 2048]) for i in range(3)]
            psm = [S.ps(f"psm{i}", [128, 512]) for i in range(4)]
            brow = S.sb("brow", [2, 2048]); mrow = S.sb("mrow", [2, 2048])
            wi = 0
            for l in range(2):
                for cg in range(6):
                    for k in range(KC):
                        w = wt[wi % 3]; wn = f"adaw{wi % 3}"; wi += 1
                        T.dma('sp', w[:], ada_w[l, k * 128:(k + 1) * 128, cg * 2048:(cg + 1) * 2048], reads=[wn], writes=[wn])
                        for j in range(4):
                            T.op('pe', lambda: PE.matmul(psm[j][0:2, :], cv[:, k, :], w[:, j * 512:(j + 1) * 512], start=(k == 0), stop=(k == KC - 1)),
                                 reads=['cv', wn], writes=[f"psm{j}"])
                    for r in range(2):
                        T.dma('sp', brow[r:r + 1, :], ada_b[l, :, cg * 2048:(cg + 1) * 2048], reads=['brow'], writes=['brow'])
                    for j in range(4):
                        T.op('dve', lambda: V.tensor_tensor(mrow[:, j * 512:(j + 1) * 512], psm[j][0:2, :], brow[:, j * 512:(j + 1) * 512], op=ALU.add),
                             reads=[f"psm{j}", 'brow', 'mrow'], writes=['mrow'])
                    T.dma('sp', modrow[l, :, cg * 2048:(cg + 1) * 2048], mrow[:], reads=['mrow'])
            T.barrier()
            S.close()

        def phase_n0():
            S = Scope(nc, T)
            xin = [S.sb(f"n0x{i}", [128, D]) for i in range(2)]
            hh = S.sb("n0h", [128, D])
            hTf = S.sb("hTf", [128, KC, 512], BF16); hTr = S.sb("hTr", [128, KC, 512], BF16)
            pst = [S.ps(f"n0ps{i}", [128, 512]) for i in range(4)]
            pi = 0; xi = 0
            for (src, who, L, base) in ((c_in, 1, CTX, 0), (x_in, 0, SEQ, CTX)):
                geff, shr = mod_rows(S, 0, who, 1, 0, 0, f"n0m{who}")
                for c0 in range(0, L, 512):
                    wd_ = min(512, L - c0)
                    nb = wd_ // 128
                    for j in range(nb):
                        xt = xin[xi % 2]; xn = f"n0x{xi % 2}"; xi += 1
                        T.dma('sp', xt[:], src[c0 + j * 128:c0 + (j + 1) * 128, :], reads=[xn], writes=[xn])
                        rms_tok(S, xt, xn, 128, f"n0m{who}", geff, shr, hh, 'n0h')
                        for kq in range(4):
                            for (dst, dn, idm, jj) in ((hTf, 'hTf', ident, j), (hTr, 'hTr', J128, nb - 1 - j)):
                                pp = pst[pi % 4]; pn = f"n0ps{pi % 4}"; pi += 1
                                for k4 in range(4):
                                    k = kq * 4 + k4
                                    T.op('pe', lambda: PE.matmul(pp[:, k4 * 128:(k4 + 1) * 128], hh[:, k * 128:(k + 1) * 128], idm, start=True, stop=True),
                                         reads=['n0h', 'cst', 'cst2'], writes=[pn])
                                eng = 'act' if pi % 2 == 0 else 'dve'
                                o_ = dst[:, kq * 4:(kq + 1) * 4, jj * 128:(jj + 1) * 128]
                                i_ = pp[:].rearrange("p (k n) -> p k n", n=128)
                                if eng == 'act':
                                    T.op('act', lambda: A.activation(o_, i_, AF.Copy), reads=[pn, dn], writes=[dn])
                                else:
                                    T.op('dve', lambda: V.tensor_copy(o_, i_), reads=[pn, dn], writes=[dn])
                    T.dma('sp', hxf[:, base + c0:base + c0 + wd_].rearrange("(k p) n -> p k n", p=128), hTf[:, :, 0:wd_], reads=['hTf'])
                    r0 = base + (L - c0 - wd_)
                    T.dma('sp', hxr[:, r0:r0 + wd_].rearrange("(k p) n -> p k n", p=128), hTr[:, :, 0:wd_], reads=['hTr'])
            T.barrier()
            S.close()

        def mixer_group(g):
            w2a2 = w2a2_all[g]; g2a = g2a_all[g]; g2b = g2b_all[g]; sinkb = sinkb_all[g]
            mixT_rw = mixs[256 * g:256 * g + 256, :]
            mixT_att = mixs[1024 + 256 * g:1024 + 256 * g + 256, :]
            T.dma('sp', vec_sb[:], vec_all[g], reads=['vec'], writes=['vec'])
            T.op('dve', lambda: V.tensor_scalar(omu[:, 0:8], vec_sb[:, 0:8], -1.0, 1.0, ALU.mult, ALU.add), reads=['vec', 'omu'], writes=['omu'])
            T.op('dve', lambda: V.tensor_scalar(hmu[:, 0:8], vec_sb[:, 0:8], 0.5, None, ALU.mult), reads=['vec', 'hmu'], writes=['hmu'])
            T.op('dve', lambda: V.tensor_scalar(omu[:, 8:10], mugd_sb[:], -1.0, 1.0, ALU.mult, ALU.add), reads=['mugd', 'omu'], writes=['omu'])
            T.op('dve', lambda: V.tensor_scalar(hmu[:, 8:10], mugd_sb[:], 0.5, None, ALU.mult), reads=['mugd', 'hmu'], writes=['hmu'])
            S = Scope(nc, T)
            Wf_sb = S.sb("Wf_sb", [128, KC, NF], BF16)
            Wb_sb = S.sb("Wb_sb", [128, KC, NB], BF16)
            wst = [S.sb(f"wst{i}", [128, NF]) for i in range(2)]
            ci = 0
            for (Wd, Wsb, ncol) in ((Wf_all[g], Wf_sb, NF), (Wb_all[g], Wb_sb, NB)):
                for k in range(KC):
                    w = wst[ci % 2]; wn = f"wst{ci % 2}"
                    T.dma('sp', w[:, 0:ncol], Wd[k * 128:(k + 1) * 128, :], reads=[wn], writes=[wn])
                    eng = ('dve', 'pool')[ci % 2]
                    E = V if eng == 'dve' else P
                    T.op(eng, lambda: E.tensor_copy(Wsb[:, k, :], w[:, 0:ncol]), reads=[wn], writes=[f"W{ncol}_{k}"])
                    ci += 1
            PW = 512
            hx = [S.sb(f"hx{i}", [128, KC, PW], BF16) for i in range(2)]
            ost = [S.sb(f"ost{i}", [128, PW]) for i in range(3)]
            psP = [S.ps(f"psP{i}", [128, PW]) for i in range(4)]
            oi = 0; hi = 0
            for (src, Wsb, ncol, pxo) in ((hxf, Wf_sb, NF, pxf), (hxr, Wb_sb, NB, pxb)):
                for c0 in range(0, TOT, PW):
                    w_ = min(PW, TOT - c0)
                    h_ = hx[hi % 2]; hn = f"hx{hi % 2}"; hi += 1
                    T.dma('sp', h_[:, :, 0:w_], src[:, c0:c0 + w_].rearrange("(k p) n -> p k n", p=128), reads=[hn], writes=[hn])
                    for m0 in range(0, ncol, 128):
                        mw = min(128, ncol - m0)
                        pp = psP[oi % 4]; pn = f"psP{oi % 4}"; oo = ost[oi % 3]; on = f"ost{oi % 3}"
                        for k in range(KC):
                            T.op('pe', lambda: PE.matmul(pp[0:mw, 0:w_], Wsb[:, k, m0:m0 + mw], h_[:, k, 0:w_], start=(k == 0), stop=(k == KC - 1)),
                                 reads=[f"W{ncol}_{k}", hn], writes=[pn])
                        if oi % 2 == 0:
                            T.op('act', lambda: A.activation(oo[0:mw, 0:w_], pp[0:mw, 0:w_], AF.Copy), reads=[pn, on], writes=[on])
                        else:
                            T.op('dve', lambda: V.tensor_copy(oo[0:mw, 0:w_], pp[0:mw, 0:w_]), reads=[pn, on], writes=[on])
                        T.dma('sp', pxo[m0:m0 + mw, c0:c0 + w_], oo[0:mw, 0:w_], reads=[on])
                        oi += 1
            T.barrier()
            S.close()
            TW = 256
            NCH = TW // 64
            tiles = []
            for (s0, L) in ((0, CTX), (CTX, SEQ)):
                for c0 in range(s0, s0 + L, TW):
                    assert c0 + TW <= s0 + L
                    tiles.append((s0, s0 + L, c0, s0 == CTX))

            S = Scope(nc, T)
            psT = [S.ps(f"psT{i}", [128, 512]) for i in range(2)]
            w2a2_sb = S.sb("w2a2_sb", [128, 2, 2, 128])
            T.dma('sp', w2a2_sb[:], w2a2, writes=['w2a2'])
            rmask = S.sb("rmask", [128, TW])
            T.op('pool', lambda: P.memset(rmask[:], 1.0), writes=['rmask'])
            T.op('pool', lambda: P.memset(rmask[:, 0:TW:64], 0.0), reads=['rmask'], writes=['rmask'])

            class Strm:
                pass

            def mk_stream(si):
                s = Strm()
                s.si = si
                n = lambda x: f"{x}_{si}"
                s.n = n
                for nm in ("rl", "kl", "vl", "ll", "tl", "sg", "aa", "t0", "t1", "kkn", "kmod", "bb", "cs_", "epos", "eneg", "eprev", "rk"):
                    setattr(s, nm, S.sb(n(nm), [128, TW]))
                s.raw = S.sb(n("raw"), [128, 4, TW + 2])
                s.AR = S.sb(n("AR"), [128, NCH, 128]); s.BK = S.sb(n("BK"), [128, NCH, 128])
                s.V2 = S.sb(n("V2"), [128, NCH, 128]); s.RK2 = S.sb(n("RK2"), [128, NCH, 128])
                s.Yout = S.sb(n("Yout"), [128, 2, TW])
                s.GB = S.sb(n("GB"), [128, 128]); s.GK = S.sb(n("GK"), [128, 128])
                s.PT = [S.sb(n(f"PT{i}"), [128, 256]) for i in range(2)]
                s.PkT = [S.sb(n(f"PkT{i}"), [128, 128]) for i in range(2)]
                s.Tbd = S.sb(n("Tbd"), [128, 128]); s.BKT = S.sb(n("BKT"), [128, 128])
                s.VV = S.sb(n("VV"), [128, 128]); s.UU = S.sb(n("UU"), [128, 128])
                s.WY = S.sb(n("WY"), [128, 128]); s.Ysb = S.sb(n("Ysb"), [128, 128]); s.Bsb = S.sb(n("Bsb"), [128, 128])
                s.csb = S.sb(n("csb"), [128, 2])
                s.Sbd = [S.sb(n(f"Sbd{i}"), [128, 128]) for i in range(2)]
                s.ps0 = S.ps(n("ps0"), [128, 512])
                s.ps1 = S.ps(n("ps1"), [128, 512])
                s.ps2 = S.ps(n("ps2"), [128, 512])
                for t_, nm in ((s.VV, "VV"), (s.UU, "UU"), (s.Ysb, "Ysb"), (s.Bsb, "Bsb"), (s.Sbd[0], "Sbd0"), (s.Sbd[1], "Sbd1")):
                    T.op('pool', lambda: P.memset(t_[:], 0.0), writes=[n(nm)])
                return s

            def rwkv_stream(s, d, p):
                n = s.n
                px = pxf if d == 0 else pxb
                rows = [p * 128, 256 + p * 128, 512 + p * 128, 768]
                mucol = [3 * p + 0, 3 * p + 1, 3 * p + 2, 6 + d]
                dst = [s.rl, s.kl, s.vl, s.ll]
                dstn = [n("rl"), n("kl"), n("vl"), n("ll")]
                w0c = vec_sb[:, 8 + 2 * d + p: 9 + 2 * d + p]
                a0c = vec_sb[:, 12 + 2 * d + p: 13 + 2 * d + p]
                kkc = vec_sb[:, 16 + p:17 + p]; kac = vec_sb[:, 18 + p:19 + p]; rkc = vec_sb[:, 20 + p:21 + p]
                cur = 0
                for i in range(2):
                    T.op('pool', lambda: P.memset(s.Sbd[i][:], 0.0), reads=[n(f"Sbd{i}")], writes=[n(f"Sbd{i}")])
                for (s0, s1, c0, isx) in tiles:
                    for qi in range(4):
                        lo_ = max(c0 - 1, s0); hi_ = min(c0 + TW + 1, s1)
                        if c0 - 1 < s0:
                            T.op('pool', lambda: P.memset(s.raw[:, qi, 0:1], 0.0), reads=[n(f"raw{qi}")], writes=[n(f"raw{qi}")])
                        if c0 + TW + 1 > s1:
                            T.op('pool', lambda: P.memset(s.raw[:, qi, TW + 1:TW + 2], 0.0), reads=[n(f"raw{qi}")], writes=[n(f"raw{qi}")])
                        T.dma('sp', s.raw[:, qi, lo_ - (c0 - 1): hi_ - (c0 - 1)], px[rows[qi]:rows[qi] + 128, lo_:hi_],
                              reads=[n(f"raw{qi}")], writes=[n(f"raw{qi}")])
                    for qi in range(4):
                        mc = mucol[qi]
                        T.op('dve', lambda: V.tensor_tensor(s.t0[:], s.raw[:, qi, 0:TW], s.raw[:, qi, 2:TW + 2], op=ALU.add),
                             reads=[n(f"raw{qi}")], writes=[n("t0")])
                        T.op('act', lambda: A.activation(s.t1[:], s.raw[:, qi, 1:TW + 1], AF.Copy, scale=omu[:, mc:mc + 1]),
                             reads=[n(f"raw{qi}"), 'omu'], writes=[n("t1")])
                        T.op('dve', lambda: V.scalar_tensor_tensor(dst[qi][:], s.t0[:], hmu[:, mc:mc + 1], s.t1[:], ALU.mult, ALU.add),
                             reads=[n("t0"), n("t1"), 'hmu'], writes=[dstn[qi]])
                    T.op('act', lambda: A.activation(s.tl[0:64, :], s.ll[0:64, :], AF.Tanh), reads=[n("ll")], writes=[n("tl")])
                    T.op('pe', lambda: PE.matmul(psT[0][:, 0:TW], w2a2_sb[0:64, d, p, :], s.tl[0:64, :], start=True, stop=True),
                         reads=['w2a2', n("tl")], writes=['psT0'])
                    T.op('act', lambda: A.activation(s.sg[:], psT[0][:, 0:TW], AF.Sigmoid, bias=w0c), reads=['psT0', 'vec'], writes=[n("sg")])
                    T.op('pe', lambda: PE.matmul(psT[1][:, 0:TW], w2a2_sb[64:128, d, p, :], s.ll[64:128, :], start=True, stop=True),
                         reads=['w2a2', n("ll")], writes=['psT1'])
                    T.op('act', lambda: A.activation(s.aa[:], psT[1][:, 0:TW], AF.Sigmoid, bias=a0c), reads=['psT1', 'vec'], writes=[n("aa")])
                    yield
                    T.op('act', lambda: A.activation(s.t0[:], s.kl[:], AF.Square, scale=kkc), reads=[n("kl"), 'vec'], writes=[n("t0")])
                    T.op('pe', lambda: PE.matmul(psT[0][:, 0:TW], onesbd, s.t0[:], start=True, stop=True), reads=['cst', n("t0")], writes=['psT0'])
                    T.op('dve', lambda: V.tensor_scalar(s.t1[:], psT[0][:, 0:TW], 1e-24, None, ALU.max), reads=['psT0'], writes=[n("t1")])
                    T.op('act', lambda: A.activation(s.t1[:], s.t1[:], AF.Sqrt), reads=[n("t1")], writes=[n("t1")])
                    T.op('dve', lambda: V.reciprocal(s.t1[:], s.t1[:]), reads=[n("t1")], writes=[n("t1")])
                    T.op('dve', lambda: V.scalar_tensor_tensor(s.kkn[:], s.kl[:], kkc, s.t1[:], ALU.mult, ALU.mult),
                         reads=[n("kl"), n("t1"), 'vec'], writes=[n("kkn")])
                    T.op('dve', lambda: V.tensor_scalar(s.t0[:], s.aa[:], -1.0, kac, ALU.add, ALU.mult), reads=[n("aa"), 'vec'], writes=[n("t0")])
                    T.op('dve', lambda: V.scalar_tensor_tensor(s.kmod[:], s.t0[:], 1.0, s.kl[:], ALU.add, ALU.mult),
                         reads=[n("t0"), n("kl")], writes=[n("kmod")])
                    T.op('pool', lambda: P.tensor_tensor(s.bb[:], s.kkn[:], s.aa[:], op=ALU.mult), reads=[n("kkn"), n("aa")], writes=[n("bb")])
                    T.op('dve', lambda: V.tensor_tensor_scan(s.cs_[:], rmask[:], s.sg[:], 0.0, ALU.mult, ALU.add),
                         reads=['rmask', n("sg")], writes=[n("cs_")])
                    T.op('pool', lambda: P.tensor_tensor(s.t1[:], s.cs_[:], s.sg[:], op=ALU.subtract), reads=[n("cs_"), n("sg")], writes=[n("t1")])
                    T.op('act', lambda: A.activation(s.epos[:], s.cs_[:], AF.Exp, scale=-C0), reads=[n("cs_")], writes=[n("epos")])
                    T.op('act', lambda: A.activation(s.eneg[:], s.cs_[:], AF.Exp, scale=C0), reads=[n("cs_")], writes=[n("eneg")])
                    T.op('act', lambda: A.activation(s.eprev[:], s.t1[:], AF.Exp, scale=-C0), reads=[n("t1")], writes=[n("eprev")])
                    c3 = lambda t_, h: t_[64 * h:64 * h + 64, :].rearrange("p (c t) -> p c t", t=64)
                    for h in range(2):
                        lo = 64 * h; ot = 64 * (1 - h)
                        T.op('dve', lambda: V.scalar_tensor_tensor(s.AR[lo:lo + 64, :, lo:lo + 64], c3(s.eprev, h), -1.0, c3(s.kkn, h), ALU.mult, ALU.mult),
                             reads=[n("eprev"), n("kkn"), n("AR")], writes=[n("AR")])
                        T.op('pool', lambda: P.tensor_tensor(s.AR[lo:lo + 64, :, ot:ot + 64], c3(s.epos, h), c3(s.rl, h), op=ALU.mult),
                             reads=[n("epos"), n("rl"), n("AR")], writes=[n("AR")])
                        T.op('dve', lambda: V.tensor_tensor(s.BK[lo:lo + 64, :, lo:lo + 64], c3(s.eneg, h), c3(s.bb, h), op=ALU.mult),
                             reads=[n("eneg"), n("bb"), n("BK")], writes=[n("BK")])
                        T.op('pool', lambda: P.tensor_tensor(s.BK[lo:lo + 64, :, ot:ot + 64], c3(s.eneg, h), c3(s.kmod, h), op=ALU.mult),
                             reads=[n("eneg"), n("kmod"), n("BK")], writes=[n("BK")])
                    vl3 = s.vl[:].rearrange("p (c t) -> p c t", t=64)
                    T.op('pool', lambda: P.tensor_copy(s.V2[:, :, 0:64], vl3), reads=[n("vl"), n("V2")], writes=[n("V2")])
                    T.op('act', lambda: A.activation(s.V2[:, :, 64:128], vl3, AF.Copy), reads=[n("vl"), n("V2")], writes=[n("V2")])
                    T.op('dve', lambda: V.scalar_tensor_tensor(s.rk[:], s.rl[:], rkc, s.kmod[:], ALU.mult, ALU.mult),
                         reads=[n("rl"), n("kmod"), 'vec'], writes=[n("rk")])
                    rk3 = s.rk[:].rearrange("p (c t) -> p c t", t=64)
                    T.op('pool', lambda: P.tensor_copy(s.RK2[:, :, 0:64], rk3), reads=[n("rk"), n("RK2")], writes=[n("RK2")])
                    T.op('act', lambda: A.activation(s.RK2[:, :, 64:128], rk3, AF.Copy), reads=[n("rk"), n("RK2")], writes=[n("RK2")])
                    yield
                    for c in range(NCH):
                        gA = s.ps0[:, 0:128]; gB = s.ps1[:, 0:128]
                        T.op('pe', lambda: PE.matmul(gA, s.BK[0:64, c, :], s.AR[0:64, c, :], start=True, stop=True),
                             reads=[n("BK"), n("AR")], writes=[n("ps0")])
                        T.op('pe', lambda: PE.matmul(gB, s.BK[64:128, c, :], s.AR[64:128, c, :], start=True, stop=True),
                             reads=[n("BK"), n("AR")], writes=[n("ps1")])
                        T.op('dve', lambda: V.tensor_tensor(s.GB[0:64, :], gA[0:64, :], maskA[0:64, :], op=ALU.mult), reads=[n("ps0"), 'cst', n("GB")], writes=[n("GB")])
                        T.op('dve', lambda: V.tensor_tensor(s.GK[64:128, :], gA[64:128, :], maskA[64:128, :], op=ALU.mult), reads=[n("ps0"), 'cst', n("GK")], writes=[n("GK")])
                        T.op('dve', lambda: V.tensor_tensor(s.GK[0:64, :], gB[0:64, :], maskB[0:64, :], op=ALU.mult), reads=[n("ps1"), 'cst', n("GK")], writes=[n("GK")])
                        T.op('dve', lambda: V.tensor_tensor(s.GB[64:128, :], gB[64:128, :], maskB[64:128, :], op=ALU.mult), reads=[n("ps1"), 'cst', n("GB")], writes=[n("GB")])
                        yield
                        pt = s.PT[0]; ptn = n("PT0")
                        T.op('pool', lambda: P.tensor_tensor(pt[:, 0:128], s.GB[:], bdm, op=ALU.mult), reads=[n("GB"), 'cst', ptn], writes=[ptn])
                        T.op('pool', lambda: P.tensor_copy(pt[:, 128:256], ident), reads=['cst', ptn], writes=[ptn])
                        T.op('pe', lambda: PE.transpose(s.ps1[:, 256:384], pt[:, 0:128], ident), reads=[ptn, 'cst'], writes=[n("ps1")])
                        T.op('act', lambda: A.activation(s.PkT[0][:], s.ps1[:, 256:384], AF.Copy), reads=[n("ps1")], writes=[n("PkT0")])
                        yield
                        pi = 0
                        for lvl in range(6):
                            last = lvl == 5
                            pt = s.PT[pi]; ptn = n(f"PT{pi}"); pkt = s.PkT[pi]; pktn = n(f"PkT{pi}")
                            npt = s.PT[1 - pi]; nptn = n(f"PT{1 - pi}"); npkt = s.PkT[1 - pi]; npktn = n(f"PkT{1 - pi}")
                            if not last:
                                T.op('pe', lambda: PE.matmul(s.ps1[:, 0:256], pkt[:], pt[:, 0:256], start=True, stop=True), reads=[pktn, ptn], writes=[n("ps1")])
                                T.op('pe', lambda: PE.matmul(s.ps1[:, 256:384], pt[:, 0:128], pkt[:], start=True, stop=True), reads=[pktn, ptn], writes=[n("ps1")])
                                T.op('act', lambda: A.activation(npt[:, 0:128], s.ps1[:, 0:128], AF.Copy), reads=[n("ps1"), nptn], writes=[nptn])
                                T.op('dve', lambda: V.tensor_tensor(npt[:, 128:256], s.ps1[:, 128:256], pt[:, 128:256], op=ALU.add), reads=[n("ps1"), ptn, nptn], writes=[nptn])
                                T.op('act', lambda: A.activation(npkt[:], s.ps1[:, 256:384], AF.Copy), reads=[n("ps1")], writes=[npktn])
                            else:
                                T.op('pe', lambda: PE.matmul(s.ps1[:, 0:128], pkt[:], pt[:, 128:256], start=True, stop=True), reads=[pktn, ptn], writes=[n("ps1")])
                                T.op('dve', lambda: V.tensor_tensor(s.Tbd[:], s.ps1[:, 0:128], pt[:, 128:256], op=ALU.add), reads=[n("ps1"), ptn], writes=[n("Tbd")])
                            pi = 1 - pi
                            yield
                        T.op('pe', lambda: PE.transpose(s.ps0[:, 256:384], s.BK[:, c, :], ident), reads=[n("BK"), 'cst'], writes=[n("ps0")])
                        T.op('act', lambda: A.activation(s.BKT[:], s.ps0[:, 256:384], AF.Copy), reads=[n("ps0")], writes=[n("BKT")])
                        T.op('pe', lambda: PE.transpose(s.ps0[:, 384:512], s.V2[:, c, :], ident), reads=[n("V2"), 'cst'], writes=[n("ps0")])
                        T.op('dve', lambda: V.tensor_copy(s.VV[64:128, 0:64], s.ps0[64:128, 384:448]), reads=[n("ps0"), n("VV")], writes=[n("VV")])
                        T.op('act', lambda: A.activation(s.VV[0:64, 64:128], s.ps0[0:64, 448:512], AF.Copy), reads=[n("ps0"), n("VV")], writes=[n("VV")])
                        T.op('pe', lambda: PE.matmul(psT[s.si][:, 384:386], s.RK2[:, c, :], headsel, start=True, stop=True), reads=[n("RK2"), 'cst'], writes=[f'psT{s.si}'])
                        T.op('dve', lambda: V.tensor_copy(s.csb[:], psT[s.si][:, 384:386]), reads=[f'psT{s.si}'], writes=[n("csb")])
                        yield
                        Sc = s.Sbd[cur]; Scn = n(f"Sbd{cur}"); Sn = s.Sbd[1 - cur]; Snn = n(f"Sbd{1 - cur}")
                        T.op('pe', lambda: PE.matmul(s.ps2[:, 0:128], s.GK[:], s.VV[:], start=True, stop=False), reads=[n("GK"), n("VV")], writes=[n("ps2")])
                        T.op('pe', lambda: PE.matmul(s.ps2[:, 0:128], s.AR[:, c, :], Sc[:], start=False, stop=True), reads=[n("AR"), Scn], writes=[n("ps2")])
                        T.op('act', lambda: A.activation(s.WY[:], s.ps2[:, 0:128], AF.Copy), reads=[n("ps2")], writes=[n("WY")])
                        yield
                        T.op('pe', lambda: PE.matmul(s.ps2[:, 128:256], s.Tbd[:], s.WY[:], start=True, stop=True), reads=[n("Tbd"), n("WY")], writes=[n("ps2")])
                        T.op('dve', lambda: V.tensor_copy(s.UU[0:64, 0:64], s.ps2[0:64, 128:192]), reads=[n("ps2"), n("UU")], writes=[n("UU")])
                        T.op('act', lambda: A.activation(s.UU[64:128, 64:128], s.ps2[64:128, 192:256], AF.Copy), reads=[n("ps2"), n("UU")], writes=[n("UU")])
                        yield
                        T.op('pe', lambda: PE.matmul(s.ps2[:, 256:384], s.BKT[:], s.VV[:], start=True, stop=False), reads=[n("BKT"), n("VV")], writes=[n("ps2")])
                        T.op('pe', lambda: PE.matmul(s.ps2[:, 256:384], ident, Sc[:], start=False, stop=False), reads=['cst', Scn], writes=[n("ps2")])
                        T.op('pe', lambda: PE.matmul(s.ps2[:, 256:384], s.BKT[:], s.UU[:], start=False, stop=True), reads=[n("BKT"), n("UU")], writes=[n("ps2")])
                        ce = c * 64 + 63
                        T.op('dve', lambda: V.tensor_scalar(Sn[0:64, 0:64], s.ps2[0:64, 256:320], s.epos[0:64, ce:ce + 1], None, ALU.mult),
                             reads=[n("ps2"), n("epos"), Snn], writes=[Snn])
                        T.op('act', lambda: A.activation(Sn[64:128, 64:128], s.ps2[64:128, 320:384], AF.Copy, scale=s.epos[64:128, ce:ce + 1]),
                             reads=[n("ps2"), n("epos"), Snn], writes=[Snn])
                        if isx:
                            T.op('pe', lambda: PE.matmul(s.ps2[:, 384:512], s.GB[:], s.UU[:], start=True, stop=True), reads=[n("GB"), n("UU")], writes=[n("ps2")])
                            T.op('dve', lambda: V.tensor_tensor(s.Ysb[64:128, 0:64], s.ps2[64:128, 384:448], s.WY[64:128, 0:64], op=ALU.add),
                                 reads=[n("ps2"), n("WY"), n("Ysb")], writes=[n("Ysb")])
                            T.op('dve', lambda: V.tensor_tensor(s.Ysb[0:64, 64:128], s.ps2[0:64, 448:512], s.WY[0:64, 64:128], op=ALU.add),
                                 reads=[n("ps2"), n("WY"), n("Ysb")], writes=[n("Ysb")])
                            T.op('pool', lambda: P.tensor_scalar(s.Bsb[64:128, 0:64], s.VV[64:128, 0:64], s.csb[64:128, 0:1], None, ALU.mult),
                                 reads=[n("VV"), n("csb"), n("Bsb")], writes=[n("Bsb")])
                            T.op('pool', lambda: P.tensor_scalar(s.Bsb[0:64, 64:128], s.VV[0:64, 64:128], s.csb[0:64, 1:2], None, ALU.mult),
                                 reads=[n("VV"), n("csb"), n("Bsb")], writes=[n("Bsb")])
                            yield
                            T.op('pe', lambda: PE.matmul(s.ps1[:, 384:448], s.Ysb[:], JJ[d], start=True, stop=True), reads=[n("Ysb"), 'cst'], writes=[n("ps1")])
                            T.op('pe', lambda: PE.matmul(s.ps1[:, 448:512], s.Bsb[:], JJ[d], start=True, stop=True), reads=[n("Bsb"), 'cst'], writes=[n("ps1")])
                            cp = c if d == 0 else NCH - 1 - c
                            T.op('act', lambda: A.activation(s.Yout[:, 0, cp * 64:cp * 64 + 64], s.ps1[:, 384:448], AF.Copy), reads=[n("ps1"), n("Yout")], writes=[n("Yout")])
                            T.op('dve', lambda: V.tensor_copy(s.Yout[:, 1, cp * 64:cp * 64 + 64], s.ps1[:, 448:512]), reads=[n("ps1"), n("Yout")], writes=[n("Yout")])
                        cur = 1 - cur
                        yield
                    if isx:
                        r0 = c0 - CTX
                        f0 = r0 if d == 0 else SEQ - r0 - TW
                        T.dma('sp', yT[d, p * 128:(p + 1) * 128, f0:f0 + TW], s.Yout[:, 0, :], reads=[n("Yout")])
                        T.dma('sp', bT[d, p * 128:(p + 1) * 128, f0:f0 + TW], s.Yout[:, 1, :], reads=[n("Yout")])

            streams = [mk_stream(0), mk_stream(1)]
            for d in range(2):
                gens = [rwkv_stream(streams[p], d, p) for p in range(2)]
                alive = [True, True]
                rounds = 0
                while any(alive):
                    rounds += 1
                    for i, g_ in enumerate(gens):
                        if alive[i]:
                            try:
                                next(g_)
                            except StopIteration:
                                alive[i] = False
            T.barrier()
            S.close()

            S = Scope(nc, T)
            FW = 512 if SEQ % 512 == 0 else 256
            g2a_sb = S.sb("g2a_sb", [128, 256]); g2b_sb = S.sb("g2b_sb", [32, 256])
            T.dma('sp', g2a_sb[:], g2a, writes=['g2a']); T.dma('sp', g2b_sb[:], g2b, writes=['g2b'])
            graw0 = S.sb("graw0", [128, FW + 2]); graw1 = S.sb("graw1", [32, FW + 2])
            sgd0 = S.sb("sgd0", [128, FW]); sgd1 = S.sb("sgd1", [32, FW])
            ft0 = S.sb("ft0", [128, FW]); ft1 = S.sb("ft1", [128, FW])
            yy = [S.sb(f"yy{i}", [128, FW]) for i in range(4)]
            yc = S.sb("yc", [128, FW]); fsq = S.sb("fsq", [128, FW]); frs = S.sb("frs", [128, FW]); fz = S.sb("fz", [128, FW]); fo = S.sb("fo", [128, FW])
            psF = [S.ps(f"psF{i}", [128, 512]) for i in range(3)]
            for c0 in range(0, SEQ, FW):
                for (gr, grn, r0_, nr, mc) in ((graw0, 'graw0', 896, 128, 8), (graw1, 'graw1', 1024, 32, 9)):
                    lo_ = max(c0 - 1, 0); hi_ = min(c0 + FW + 1, SEQ)
                    if c0 == 0:
                        T.op('pool', lambda: P.memset(gr[0:nr, 0:1], 0.0), reads=[grn], writes=[grn])
                    if c0 + FW + 1 > SEQ:
                        T.op('pool', lambda: P.memset(gr[0:nr, FW + 1:FW + 2], 0.0), reads=[grn], writes=[grn])
                    T.dma('sp', gr[0:nr, lo_ - (c0 - 1): hi_ - (c0 - 1)], pxf[r0_:r0_ + nr, CTX + lo_:CTX + hi_], reads=[grn], writes=[grn])
                    sg_ = sgd0 if nr == 128 else sgd1; sgn = 'sgd0' if nr == 128 else 'sgd1'
                    T.op('dve', lambda: V.tensor_tensor(ft0[0:nr, :], gr[0:nr, 0:FW], gr[0:nr, 2:FW + 2], op=ALU.add), reads=[grn, 'ft0'], writes=['ft0'])
                    T.op('act', lambda: A.activation(ft1[0:nr, :], gr[0:nr, 1:FW + 1], AF.Copy, scale=omu[0:nr, mc:mc + 1]), reads=[grn, 'omu', 'ft1'], writes=['ft1'])
                    T.op('dve', lambda: V.scalar_tensor_tensor(ft0[0:nr, :], ft0[0:nr, :], hmu[0:nr, mc:mc + 1], ft1[0:nr, :], ALU.mult, ALU.add),
                         reads=['ft0', 'ft1', 'hmu'], writes=['ft0'])
                    T.op('act', lambda: A.activation(sg_[0:nr, :], ft0[0:nr, :], AF.Sigmoid), reads=['ft0'], writes=[sgn])
                for p in range(2):
                    lnw = vec_sb[:, 22 + p:23 + p]; lnb = vec_sb[:, 24 + p:25 + p]
                    srcs = [yT[0], yT[1], bT[0], bT[1]]
                    for i in range(4):
                        T.dma('sp', yy[i][:], srcs[i][p * 128:(p + 1) * 128, c0:c0 + FW], writes=[f"yy{i}"])
                    T.op('dve', lambda: V.tensor_tensor(yy[0][:], yy[0][:], yy[1][:], op=ALU.add), reads=['yy0', 'yy1'], writes=['yy0'])
                    T.op('pool', lambda: P.tensor_tensor(yy[2][:], yy[2][:], yy[3][:], op=ALU.add), reads=['yy2', 'yy3'], writes=['yy2'])
                    T.op('pe', lambda: PE.matmul(psF[0][:, 0:FW], ones64[:], yy[0][:], start=True, stop=True), reads=['ones64', 'yy0'], writes=['psF0'])
                    T.op('dve', lambda: V.tensor_tensor(yc[:], yy[0][:], psF[0][:, 0:FW], op=ALU.subtract), reads=['yy0', 'psF0'], writes=['yc'])
                    T.op('act', lambda: A.activation(fsq[:], yc[:], AF.Square), reads=['yc'], writes=['fsq'])
                    T.op('pe', lambda: PE.matmul(psF[1][:, 0:FW], ones64[:], fsq[:], start=True, stop=True), reads=['ones64', 'fsq'], writes=['psF1'])
                    T.op('act', lambda: A.activation(frs[:], psF[1][:, 0:FW], AF.Sqrt, bias=GN_EPS), reads=['psF1'], writes=['frs'])
                    T.op('dve', lambda: V.reciprocal(frs[:], frs[:]), reads=['frs'], writes=['frs'])
                    T.op('dve', lambda: V.tensor_tensor(yc[:], yc[:], frs[:], op=ALU.mult), reads=['yc', 'frs'], writes=['yc'])
                    T.op('act', lambda: A.activation(fz[:], yc[:], AF.Identity, bias=lnb, scale=lnw), reads=['yc', 'vec'], writes=['fz'])
                    T.op('pool', lambda: P.tensor_tensor(fz[:], fz[:], yy[2][:], op=ALU.add), reads=['fz', 'yy2'], writes=['fz'])
                    T.op('pe', lambda: PE.matmul(psF[2][:, 0:FW], g2a_sb[:, p * 128:(p + 1) * 128], sgd0[:], start=True, stop=False), reads=['g2a', 'sgd0'], writes=['psF2'])
                    T.op('pe', lambda: PE.matmul(psF[2][:, 0:FW], g2b_sb[0:32, p * 128:(p + 1) * 128], sgd1[0:32, :], start=False, stop=True), reads=['g2b', 'sgd1'], writes=['psF2'])
                    T.op('dve', lambda: V.tensor_tensor(fo[:], fz[:], psF[2][:, 0:FW], op=ALU.mult), reads=['fz', 'psF2'], writes=['fo'])
                    T.dma('sp', mixT_rw[p * 128:(p + 1) * 128, c0:c0 + FW], fo[:], reads=['fo'])
            T.barrier()
            S.close()

            S = Scope(nc, T)
            AW = 512 if SEQ % 512 == 0 else 256
            NBK = SEQ // 128; NCB = CTX // 128
            QR, QPR, KR, KPR, VR = 1056, 1312, 1568, 1632, 1696
            kT = S.sb("kT", [128, SEQ], BF16); kTc = S.sb("kTc", [128, CTX], BF16)
            Vtm = S.sb("Vtm", [128, NBK, 64], BF16); Vtmc = S.sb("Vtmc", [128, NCB, 64], BF16)
            kraw = S.sb("kraw", [128, AW]); kpraw = S.sb("kpraw", [128, AW]); vraw = S.sb("vraw", [64, AW])
            cos_t = S.sb("cos_t", [128, AW]); sin_t = S.sb("sin_t", [128, AW])
            at0 = S.sb("at0", [128, AW]); at1 = S.sb("at1", [128, AW])
            qraw = S.sb("qraw", [64, 4, AW]); qpraw = S.sb("qpraw", [64, 4, AW]); qT = S.sb("qT", [64, 4, AW], BF16)
            cos4 = S.sb("cos4", [64, 4, AW]); sin4 = S.sb("sin4", [64, 4, AW]); aq0 = S.sb("aq0", [64, 4, AW]); aq1 = S.sb("aq1", [64, 4, AW])
            es = S.sb("es", [128, 4])
            T.dma('sp', es[:], sinkb, writes=['es'])
            T.op('act', lambda: A.activation(es[:], es[:], AF.Exp), reads=['es'], writes=['es'])
            mask4 = [S.sb(f"mask4_{i}", [128, 4, 128], BF16) for i in range(2)]
            for i, tri in enumerate((triLO, triHI)):
                for h in range(4):
                    T.op('dve', lambda: V.tensor_copy(mask4[i][:, h, :], tri), reads=['cst', f"mask4_{i}"], writes=[f"mask4_{i}"])
            ones64bf = S.sb("ones64bf", [128, 64], BF16)
            T.op('pool', lambda: P.memset(ones64bf[:], 1.0), writes=['ones64bf'])
            PTr = [S.sb(f"PTr{i}", [128, 512], BF16) for i in range(6)]
            den = S.sb("den", [64, 512]); att = S.sb("att", [64, 512])
            psA_ = [S.ps(f"psA{i}", [128, 512]) for i in range(3)]
            psO_ = S.ps("psAO", [128, 512]); psD_ = S.ps("psAD", [128, 512]); psVt = S.ps("psVt", [128, 512])
            for h in range(2):
                T.dma('sp', kraw[64 * h:64 * h + 64, 0:CTX], pxf[KR:KR + 64, 0:CTX], reads=['kraw'], writes=['kraw'])
            T.op('dve', lambda: V.tensor_copy(kTc[:], kraw[:, 0:CTX]), reads=['kraw'], writes=['kTc'])
            T.dma('sp', vraw[:, 0:CTX], pxf[VR:VR + 64, 0:CTX], writes=['vraw'])
            for j in range(NCB):
                T.op('pe', lambda: PE.transpose(psVt[:, 0:64], vraw[0:64, j * 128:(j + 1) * 128], ident[0:64, 0:64]), reads=['vraw', 'cst'], writes=['psVt'])
                T.op('dve', lambda: V.tensor_copy(Vtmc[:, j, :], psVt[:, 0:64]), reads=['psVt'], writes=['Vtmc'])
            for c0 in range(0, SEQ, AW):
                for h in range(2):
                    T.dma('sp', kraw[64 * h:64 * h + 64, :], pxf[KR:KR + 64, CTX + c0:CTX + c0 + AW], reads=['kraw'], writes=['kraw'])
                    T.dma('sp', kpraw[64 * h:64 * h + 64, :], pxf[KPR:KPR + 64, CTX + c0:CTX + c0 + AW], reads=['kpraw'], writes=['kpraw'])
                T.dma('sp', cos_t[:], cosT[:, c0:c0 + AW], writes=['cos_t'])
                T.dma('sp', sin_t[:], sinT[:, c0:c0 + AW], writes=['sin_t'])
                T.dma('sp', vraw[:, 0:AW], pxf[VR:VR + 64, CTX + c0:CTX + c0 + AW], writes=['vraw'])
                T.op('dve', lambda: V.tensor_tensor(at0[:], kraw[:], cos_t[:], op=ALU.mult), reads=['kraw', 'cos_t'], writes=['at0'])
                T.op('pool', lambda: P.tensor_tensor(at1[:], kpraw[:], sin_t[:], op=ALU.mult), reads=['kpraw', 'sin_t'], writes=['at1'])
                T.op('dve', lambda: V.tensor_tensor(kT[:, c0:c0 + AW], at0[:], at1[:], op=ALU.add), reads=['at0', 'at1'], writes=['kT'])
                for j in range(AW // 128):
                    T.op('pe', lambda: PE.transpose(psVt[:, 0:64], vraw[0:64, j * 128:(j + 1) * 128], ident[0:64, 0:64]), reads=['vraw', 'cst'], writes=['psVt'])
                    T.op('dve', lambda: V.tensor_copy(Vtm[:, c0 // 128 + j, :], psVt[:, 0:64]), reads=['psVt'], writes=['Vtm'])
            pi_ = 0; ai = 0
            for c0 in range(0, SEQ, AW):
                for h in range(4):
                    T.dma('sp', qraw[:, h, :], pxf[QR + h * 64:QR + h * 64 + 64, CTX + c0:CTX + c0 + AW], reads=['qraw'], writes=['qraw'])
                    T.dma('sp', qpraw[:, h, :], pxf[QPR + h * 64:QPR + h * 64 + 64, CTX + c0:CTX + c0 + AW], reads=['qpraw'], writes=['qpraw'])
                    T.dma('sp', cos4[:, h, :], cosT[0:64, c0:c0 + AW], reads=['cos4'], writes=['cos4'])
                    T.dma('sp', sin4[:, h, :], sinT[0:64, c0:c0 + AW], reads=['sin4'], writes=['sin4'])
                T.op('dve', lambda: V.tensor_tensor(aq0[:], qraw[:], cos4[:], op=ALU.mult), reads=['qraw', 'cos4'], writes=['aq0'])
                T.op('pool', lambda: P.tensor_tensor(aq1[:], qpraw[:], sin4[:], op=ALU.mult), reads=['qpraw', 'sin4'], writes=['aq1'])
                T.op('dve', lambda: V.tensor_tensor(qT[:], aq0[:], aq1[:], op=ALU.add), reads=['aq0', 'aq1', 'qT'], writes=['qT'])
                for jb in range(AW // 128):
                    nq = c0 // 128 + jb
                    kbs = [('c', j, None) for j in range(NCB)]
                    if nq - 1 >= 0:
                        kbs.append(('x', nq - 1, 0))
                    kbs.append(('x', nq, None))
                    if nq + 1 < NBK:
                        kbs.append(('x', nq + 1, 1))
                    pts = []
                    for (kind, kb, mk) in kbs:
                        pa = psA_[ai % 3]; pan = f"psA{ai % 3}"; ai += 1
                        for h in range(4):
                            ksrc = kTc if kind == 'c' else kT
                            T.op('pe', lambda: PE.matmul(pa[:, h * 128:(h + 1) * 128], ksrc[0:64, kb * 128:(kb + 1) * 128],
                                                         qT[0:64, h, jb * 128:(jb + 1) * 128], start=True, stop=True),
                                 reads=['kT', 'kTc', 'qT'], writes=[pan])
                        pt = PTr[pi_ % 6]; ptn = f"PTr{pi_ % 6}"; pi_ += 1
                        T.op('act', lambda: A.activation(pt[:], pa[:], AF.Exp, scale=0.125), reads=[pan], writes=[ptn])
                        if mk is not None:
                            T.op('pool', lambda: P.tensor_tensor(pt[:], pt[:], mask4[mk][:].rearrange("p h n -> p (h n)"), op=ALU.mult),
                                 reads=[ptn, f"mask4_{mk}"], writes=[ptn])
                        pts.append((pt, ptn, kind, kb))
                    for i, (pt, ptn, kind, kb) in enumerate(pts):
                        vsrc = Vtmc if kind == 'c' else Vtm
                        T.op('pe', lambda: PE.matmul(psO_[0:64, :], vsrc[:, kb, :], pt[:], start=(i == 0), stop=(i == len(pts) - 1)),
                             reads=['Vtm', 'Vtmc', ptn], writes=['psAO'])
                        T.op('pe', lambda: PE.matmul(psD_[0:64, :], ones64bf[:], pt[:], start=(i == 0), stop=(i == len(pts) - 1)),
                             reads=['ones64bf', ptn], writes=['psAD'])
                    for h in range(4):
                        T.op('dve', lambda: V.tensor_scalar(den[:, h * 128:(h + 1) * 128], psD_[0:64, h * 128:(h + 1) * 128], es[0:64, h:h + 1], None, ALU.add),
                             reads=['psAD', 'es', 'den'], writes=['den'])
                    T.op('dve', lambda: V.reciprocal(den[:], den[:]), reads=['den'], writes=['den'])
                    T.op('dve', lambda: V.tensor_tensor(att[:], psO_[0:64, :], den[:], op=ALU.mult), reads=['psAO', 'den'], writes=['att'])
                    q0 = nq * 128
                    T.dma('sp', mixT_att[0:256, q0:q0 + 128].rearrange("(h p) n -> p h n", p=64), att[:].rearrange("p (h n) -> p h n", n=128), reads=['att'])
            T.barrier()
            S.close()


        def phase_b():
            S = Scope(nc, T)
            wo_sb = S.sb("wo_sb", [128, KC, D], BF16)
            wst = [S.sb(f"bwst{i}", [128, D]) for i in range(2)]
            for k in range(KC):
                w = wst[k % 2]; wn = f"bwst{k % 2}"
                T.dma('sp', w[:], w_out[k * 128:(k + 1) * 128, :], reads=[wn], writes=[wn])
                E = V if k % 2 == 0 else P
                T.op('dve' if k % 2 == 0 else 'pool', lambda: E.tensor_copy(wo_sb[:, k, :], w[:]), reads=[wn], writes=[f"wo{k}"])
            gta = S.sb("gta", [128, D])
            bcast_row(gta[:], modrow[0, 0:1, 2 * D:3 * D], 'gta')
            mt = S.sb("bmt", [128, KC, 512]); mtb = S.sb("bmtb", [128, KC, 512], BF16)
            xt = [S.sb(f"bx{i}", [128, D]) for i in range(2)]
            o1 = [S.sb(f"bo{i}", [128, D]) for i in range(2)]
            ps = [S.ps(f"bps{i}", [128, 512]) for i in range(8)]
            bi = 0
            for c0 in range(0, SEQ, 512):
                T.dma('sp', mt[:], mixs[:, c0:c0 + 512].rearrange("(k p) n -> p k n", p=128), reads=['bmt'], writes=['bmt'])
                T.op('act', lambda: A.activation(mtb[:], mt[:], AF.Copy), reads=['bmt', 'bmtb'], writes=['bmtb'])
                for j in range(4):
                    t0 = c0 + j * 128
                    x_ = xt[bi % 2]; xn = f"bx{bi % 2}"; o_ = o1[bi % 2]; on = f"bo{bi % 2}"
                    T.dma('sp', x_[:], x_in[t0:t0 + 128, :], reads=[xn], writes=[xn])
                    for ct in range(4):
                        pp = ps[(bi * 4 + ct) % 8]; pn = f"bps{(bi * 4 + ct) % 8}"
                        for k in range(KC):
                            T.op('pe', lambda: PE.matmul(pp[:], mtb[:, k, j * 128:(j + 1) * 128], wo_sb[:, k, ct * 512:(ct + 1) * 512], start=(k == 0), stop=(k == KC - 1)),
                                 reads=['bmtb', f"wo{k}"], writes=[pn])
                        T.op('dve', lambda: V.tensor_tensor(o_[:, ct * 512:(ct + 1) * 512], pp[:], gta[:, ct * 512:(ct + 1) * 512], op=ALU.mult),
                             reads=[pn, 'gta', on], writes=[on])
                    T.op('pool', lambda: P.tensor_tensor(o_[:], o_[:], x_[:], op=ALU.add), reads=[on, xn], writes=[on])
                    T.dma('sp', x1[t0:t0 + 128, :], o_[:], reads=[on])
                    bi += 1
            T.barrier()
            S.close()

        def phase_moe(l, xin, acc):
            S = Scope(nc, T)
            geff, shr = mod_rows(S, l, 0, 4, 3, 1 + 2 * l, "mfm")
            rw_sb = S.sb("rw_sb", [128, KC, 16])
            T.dma('sp', rw_sb[:], router_w[l].rearrange("(k p) e -> p k e", p=128), writes=['rw_sb'])
            xt = [S.sb(f"mx{i}", [128, D]) for i in range(2)]
            hr = [S.sb(f"mhr{i}", [128, XR]) for i in range(2)]
            hT = S.sb("mhT", [128, KC, 128])
            Esb = S.sb("mE", [16, 128]); rec = S.sb("mrec", [16, 128]); aff = S.sb("maff", [16, 128])
            pst = [S.ps(f"mps{i}", [128, 512]) for i in range(4)]
            psl = S.ps("mpsl", [128, 512]); pss = S.ps("mpss", [128, 512]); psa = S.ps("mpsa", [128, 512])
            for i in range(2):
                T.op('pool', lambda: P.memset(hr[i][:, D:XR], 0.0), writes=[f"mhr{i}"])
            for b in range(NBLK):
                t0 = b * 128
                x_ = xt[b % 2]; xn = f"mx{b % 2}"; h_ = hr[b % 2]; hn = f"mhr{b % 2}"
                T.dma('sp', x_[:], xin[t0:t0 + 128, :], reads=[xn], writes=[xn])
                T.dma('sp', acc[t0:t0 + 128, :], x_[:], reads=[xn])
                rms_tok(S, x_, xn, 128, "mfm", geff, shr, h_[:, 0:D], hn)
                for kq in range(4):
                    pp = pst[kq]; pn = f"mps{kq}"
                    for k4 in range(4):
                        k = kq * 4 + k4
                        T.op('pe', lambda: PE.transpose(pp[:, k4 * 128:(k4 + 1) * 128], h_[:, k * 128:(k + 1) * 128], ident), reads=[hn, 'cst'], writes=[pn])
                    if kq % 2 == 0:
                        T.op('act', lambda: A.activation(hT[:, kq * 4:(kq + 1) * 4, :], pp[:].rearrange("p (k n) -> p k n", n=128), AF.Copy), reads=[pn, 'mhT'], writes=['mhT'])
                    else:
                        T.op('dve', lambda: V.tensor_copy(hT[:, kq * 4:(kq + 1) * 4, :], pp[:].rearrange("p (k n) -> p k n", n=128)), reads=[pn, 'mhT'], writes=['mhT'])
                for k in range(KC):
                    T.op('pe', lambda: PE.matmul(psl[0:16, 0:128], rw_sb[:, k, :], hT[:, k, :], start=(k == 0), stop=(k == KC - 1)), reads=['rw_sb', 'mhT'], writes=['mpsl'])
                T.op('act', lambda: A.activation(Esb[:], psl[0:16, 0:128], AF.Exp), reads=['mpsl'], writes=['mE'])
                T.op('pe', lambda: PE.matmul(pss[0:16, 0:128], onesbd[0:16, 0:16], Esb[:], start=True, stop=True), reads=['cst', 'mE'], writes=['mpss'])
                T.op('dve', lambda: V.reciprocal(rec[:], pss[0:16, 0:128]), reads=['mpss'], writes=['mrec'])
                T.op('dve', lambda: V.tensor_tensor(aff[:], Esb[:], rec[:], op=ALU.mult), reads=['mE', 'mrec', 'maff'], writes=['maff'])
                T.dma('sp', affT[:, t0:t0 + 128], aff[:], reads=['maff'])
                T.op('pe', lambda: PE.transpose(psa[:, 0:16], aff[:], ident[0:16, 0:16]), reads=['maff', 'cst'], writes=['mpsa'])
                T.op('act', lambda: A.activation(h_[:, D:D + 16], psa[:, 0:16], AF.Copy), reads=['mpsa', hn], writes=[hn])
                T.op('dve', lambda: V.tensor_scalar(h_[:, D + 16:D + 17], iota_p[:], float(t0), None, ALU.add), reads=['iota_p', hn], writes=[hn])
                T.dma('sp', hrow[t0:t0 + 128, :], h_[:], reads=[hn])
            T.barrier()
            S.close()
            S = Scope(nc, T)
            NJ = SEQ // 8
            af = S.sb("taf", [128, NJ]); cmp_ = S.sb("tcmp", [128, NJ]); onesf = S.sb("tones", [128, NJ]); pre = S.sb("tpre", [128, NJ])
            lo = S.sb("tlo", [128, 1]); hi = S.sb("thi", [128, 1]); mid = S.sb("tmid", [128, 1]); cnt = S.sb("tcnt", [128, 2]); ge = S.sb("tge", [128, 1])
            d1 = S.sb("td1", [128, 1])
            psc = S.ps("tpsc", [128, 512])
            T.dma('sp', af[:], affT.rearrange("e (j n) -> (e j) n", j=8), writes=['taf'])
            T.op('pool', lambda: P.memset(lo[:], 0.0), writes=['tlo'])
            T.op('pool', lambda: P.memset(hi[:], 1.0), writes=['thi'])
            T.op('pool', lambda: P.memset(cnt[:], 0.0), writes=['tcnt'])
            T.op('pool', lambda: P.memset(onesf[:], 1.0), writes=['tones'])
            for it in range(34):
                T.op('dve', lambda: V.tensor_scalar(mid[:], lo[:], hi[:, 0:1], 0.5, ALU.add, ALU.mult), reads=['tlo', 'thi'], writes=['tmid'])
                T.op('dve', lambda: V.tensor_scalar(cmp_[:], af[:], mid[:, 0:1], None, ALU.is_ge, ALU.add, accum_out=cnt[:, 0:1]),
                     reads=['taf', 'tmid', 'tcnt'], writes=['tcmp', 'tcnt'])
                T.op('pe', lambda: PE.matmul(psc[:, 0:2], ones8, cnt[:], start=True, stop=True), reads=['cst2', 'tcnt'], writes=['tpsc'])
                T.op('dve', lambda: V.tensor_scalar(ge[:], psc[:, 0:1], float(CAP) - 0.5, None, ALU.is_ge), reads=['tpsc'], writes=['tge'])
                T.op('dve', lambda: V.tensor_tensor(d1[:], mid[:], lo[:], op=ALU.subtract), reads=['tmid', 'tlo'], writes=['td1'])
                T.op('dve', lambda: V.scalar_tensor_tensor(lo[:], d1[:], ge[:, 0:1], lo[:], ALU.mult, ALU.add), reads=['td1', 'tge', 'tlo'], writes=['tlo'])
                T.op('dve', lambda: V.tensor_tensor(d1[:], hi[:], mid[:], op=ALU.subtract), reads=['tmid', 'thi'], writes=['td1'])
                T.op('dve', lambda: V.scalar_tensor_tensor(hi[:], d1[:], ge[:, 0:1], mid[:], ALU.mult, ALU.add), reads=['td1', 'tge', 'tmid', 'thi'], writes=['thi'])
            T.op('dve', lambda: V.tensor_scalar(cmp_[:], af[:], lo[:, 0:1], None, ALU.is_ge), reads=['taf', 'tlo'], writes=['tcmp'])
            T.op('dve', lambda: V.tensor_tensor_scan(pre[:], onesf[:], cmp_[:], 0.0, ALU.mult, ALU.add), reads=['tones', 'tcmp'], writes=['tpre'])
            T.op('dve', lambda: V.tensor_copy(cnt[:, 0:1], pre[:, NJ - 1:NJ]), reads=['tpre', 'tcnt'], writes=['tcnt'])
            T.op('dve', lambda: V.tensor_copy(cnt[:, 1:2], pre[:, NJ - 1:NJ]), reads=['tpre', 'tcnt'], writes=['tcnt'])
            T.op('pe', lambda: PE.matmul(psc[:, 0:2], low8, cnt[:], start=True, stop=True), reads=['cst2', 'tcnt'], writes=['tpsc'])
            T.op('dve', lambda: V.tensor_copy(d1[:], psc[:, 0:1]), reads=['tpsc'], writes=['td1'])
            T.op('dve', lambda: V.scalar_tensor_tensor(pre[:], pre[:], d1[:, 0:1], cmp_[:], ALU.add, ALU.mult), reads=['tpre', 'td1', 'tcmp'], writes=['tpre'])
            T.op('dve', lambda: V.tensor_scalar(pre[:], pre[:], -1.0, None, ALU.add), reads=['tpre'], writes=['tpre'])
            T.dma('sp', slot_d.rearrange("e (j n) -> (e j) n", j=8), pre[:], reads=['tpre'], writes=['slot_d'])
            stm = S.sb("stm", [128, 16, NBLK]); neg = S.sb("sneg", [128, 16, NBLK]); su = S.sb("su", [128, 16, NBLK], U32)
            colp = S.sb("colp", [128, 1])
            T.op('dve', lambda: V.tensor_scalar(colp[:], iota_p[:], float(CAP + 1), None, ALU.add), reads=['iota_p'], writes=['colp'])
            T.dma('sp', stm[:], slot_d.rearrange("e (p b) -> p e b", b=NBLK), reads=['slot_d'], writes=['stm'])
            T.op('dve', lambda: V.tensor_scalar(neg[:], stm[:], 0.0, None, ALU.is_lt), reads=['stm'], writes=['sneg'])
            T.op('dve', lambda: V.scalar_tensor_tensor(stm[:], neg[:], colp[:, 0:1], stm[:], ALU.mult, ALU.add), reads=['sneg', 'colp', 'stm'], writes=['stm'])
            T.op('dve', lambda: V.tensor_copy(su[:], stm[:]), reads=['stm'], writes=['su'])
            ht = [S.sb(f"dht{i}", [128, XR]) for i in range(2)]
            hrow_v = hrow.rearrange("(p b) c -> p b c", b=NBLK)
            for b in range(NBLK):
                h_ = ht[b % 2]; hn = f"dht{b % 2}"
                T.dma('sp', h_[:], hrow_v[:, b, :], reads=[hn], writes=[hn])
                for e in range(16):
                    T.idma(Xg[e], bass.IndirectOffsetOnAxis(ap=su[:, e, b:b + 1], axis=0), h_[:], None, reads=[hn, 'su'])
            T.barrier()
            S.close()
            S = Scope(nc, T)
            SW = min(512, CAP); NSJ = SW // 128
            gtf = S.sb("gtf", [128, D])
            bcast_row(gtf[:], modrow[l, 0:1, 5 * D:6 * D], 'gtf')
            Wg_sb = S.sb("Wg_sb", [128, KC, 1024], BF16); Wu_sb = S.sb("Wu_sb", [128, KC, 1024], BF16); Wd_sb = S.sb("Wd_sb", [128, 8, D], BF16)
            ws = [S.sb(f"ews{i}", [128, 1024]) for i in range(2)]
            xg_t = [S.sb(f"exg{i}", [128, XR]) for i in range(2)]
            XT = S.sb("eXT", [128, KC, SW], BF16); hid = S.sb("ehid", [128, 8, SW], BF16); gs = S.sb("egs", [128, SW], BF16)
            side = S.sb("eside", [128, 4, 17]); tix = S.sb("etix", [128, 4], U32)
            Ysb = [S.sb(f"eY{i}", [128, D]) for i in range(2)]
            pst = [S.ps(f"eps{i}", [128, 512]) for i in range(2)]
            psg = S.ps("epsg", [128, 512]); psu = S.ps("epsu", [128, 512])
            psy = [S.ps(f"epsy{i}", [128, 512]) for i in range(4)]
            wi = 0; xi = 0; yi = 0; ti = 0
            for e in range(16):
                for k in range(KC):
                    for (src, dst, dn) in ((wg, Wg_sb, "Wg"), (wu, Wu_sb, "Wu")):
                        w = ws[wi % 2]; wn = f"ews{wi % 2}"; wi += 1
                        T.dma('sp', w[:], src[l, e, k * 128:(k + 1) * 128, :], reads=[wn], writes=[wn])
                        if wi % 2 == 0:
                            T.op('pool', lambda: P.tensor_copy(dst[:, k, :], w[:]), reads=[wn, dn], writes=[dn])
                        else:
                            T.op('dve', lambda: V.tensor_copy(dst[:, k, :], w[:]), reads=[wn, dn], writes=[dn])
                for f in range(8):
                    for hf in range(2):
                        w = ws[wi % 2]; wn = f"ews{wi % 2}"; wi += 1
                        T.dma('sp', w[:], wd[l, e, f * 128:(f + 1) * 128, hf * 1024:(hf + 1) * 1024], reads=[wn], writes=[wn])
                        if wi % 2 == 0:
                            T.op('pool', lambda: P.tensor_copy(Wd_sb[:, f, hf * 1024:(hf + 1) * 1024], w[:]), reads=[wn, "Wd"], writes=["Wd"])
                        else:
                            T.op('dve', lambda: V.tensor_copy(Wd_sb[:, f, hf * 1024:(hf + 1) * 1024], w[:]), reads=[wn, "Wd"], writes=["Wd"])
                for s0 in range(0, CAP, SW):
                    for j in range(NSJ):
                        xg = xg_t[xi % 2]; xgn = f"exg{xi % 2}"; xi += 1
                        T.dma('sp', xg[:], Xg[e][s0 + j * 128:s0 + (j + 1) * 128, :], reads=[xgn], writes=[xgn])
                        T.op('pool', lambda: P.tensor_copy(side[:, j, :], xg[:, D:D + 17]), reads=[xgn, 'eside'], writes=['eside'])
                        for kq in range(4):
                            pp = pst[ti % 2]; pn = f"eps{ti % 2}"; ti += 1
                            for k4 in range(4):
                                k = kq * 4 + k4
                                T.op('pe', lambda: PE.transpose(pp[:, k4 * 128:(k4 + 1) * 128], xg[:, k * 128:(k + 1) * 128], ident), reads=[xgn, 'cst'], writes=[pn])
                            o_ = XT[:, kq * 4:(kq + 1) * 4, j * 128:(j + 1) * 128]
                            i_ = pp[:].rearrange("p (k n) -> p k n", n=128)
                            if ti % 2 == 0:
                                T.op('act', lambda: A.activation(o_, i_, AF.Copy), reads=[pn, 'eXT'], writes=['eXT'])
                            else:
                                T.op('dve', lambda: V.tensor_copy(o_, i_), reads=[pn, 'eXT'], writes=['eXT'])
                    T.op('dve', lambda: V.tensor_copy(tix[:, 0:NSJ], side[:, 0:NSJ, 16]), reads=['eside', 'etix'], writes=['etix'])
                    for m in range(8):
                        for k in range(KC):
                            T.op('pe', lambda: PE.matmul(psg[:, 0:SW], Wg_sb[:, k, m * 128:(m + 1) * 128], XT[:, k, :], start=(k == 0), stop=(k == KC - 1)),
                                 reads=['Wg', 'eXT'], writes=['epsg'])
                        for k in range(KC):
                            T.op('pe', lambda: PE.matmul(psu[:, 0:SW], Wu_sb[:, k, m * 128:(m + 1) * 128], XT[:, k, :], start=(k == 0), stop=(k == KC - 1)),
                                 reads=['Wu', 'eXT'], writes=['epsu'])
                        T.op('act', lambda: A.activation(gs[:], psg[:, 0:SW], AF.Silu), reads=['epsg', 'egs'], writes=['egs'])
                        T.op('dve', lambda: V.tensor_tensor(hid[:, m, :], gs[:], psu[:, 0:SW], op=ALU.mult), reads=['egs', 'epsu', 'ehid'], writes=['ehid'])
                    for j in range(NSJ):
                        y_ = Ysb[yi % 2]; yn = f"eY{yi % 2}"; yi += 1
                        for ct in range(4):
                            for f in range(8):
                                T.op('pe', lambda: PE.matmul(psy[ct][:], hid[:, f, j * 128:(j + 1) * 128], Wd_sb[:, f, ct * 512:(ct + 1) * 512], start=(f == 0), stop=(f == 7)),
                                     reads=['ehid', 'Wd'], writes=[f"epsy{ct}"])
                            T.op('dve', lambda: V.scalar_tensor_tensor(y_[:, ct * 512:(ct + 1) * 512], psy[ct][:], side[:, j, e:e + 1], gtf[:, ct * 512:(ct + 1) * 512], ALU.mult, ALU.mult),
                                 reads=[f"epsy{ct}", 'eside', 'gtf', yn], writes=[yn])
                        T.idma(acc, bass.IndirectOffsetOnAxis(ap=tix[:, j:j + 1], axis=0), y_[:], None, reads=[yn, 'etix', 'acc'], writes=['acc'], compute_op=ALU.add)
            T.barrier()
            S.close()

        def phase_pool(xin, xout):
            S = Scope(nc, T)
            geff, shr = mod_rows(S, 1, 0, 1, 0, 2, "pm")
            xt = [S.sb(f"px{i}", [128, D]) for i in range(2)]
            hh = S.sb("ph", [128, D]); hTt = S.sb("phT", [128, KC, 512])
            pst = [S.ps(f"pps{i}", [128, 512]) for i in range(4)]
            pi = 0
            for c0 in range(0, SEQ, 512):
                for j in range(4):
                    t0 = c0 + j * 128
                    x_ = xt[j % 2]; xn = f"px{j % 2}"
                    T.dma('sp', x_[:], xin[t0:t0 + 128, :], reads=[xn], writes=[xn])
                    rms_tok(S, x_, xn, 128, "pm", geff, shr, hh, 'ph')
                    for kq in range(4):
                        pp = pst[pi % 4]; pn = f"pps{pi % 4}"; pi += 1
                        for k4 in range(4):
                            k = kq * 4 + k4
                            T.op('pe', lambda: PE.transpose(pp[:, k4 * 128:(k4 + 1) * 128], hh[:, k * 128:(k + 1) * 128], ident), reads=['ph', 'cst'], writes=[pn])
                        o_ = hTt[:, kq * 4:(kq + 1) * 4, j * 128:(j + 1) * 128]
                        i_ = pp[:].rearrange("p (k n) -> p k n", n=128)
                        if pi % 2 == 0:
                            T.op('act', lambda: A.activation(o_, i_, AF.Copy), reads=[pn, 'phT'], writes=['phT'])
                        else:
                            T.op('dve', lambda: V.tensor_copy(o_, i_), reads=[pn, 'phT'], writes=['phT'])
                T.dma('sp', hT1[:, c0:c0 + 512].rearrange("(k p) n -> p k n", p=128), hTt[:], reads=['phT'])
            T.barrier()
            S.close()
            S = Scope(nc, T)
            pw_sb = S.sb("pw_sb", [128, 4, 4, 512], BF16)
            pws = S.sb("pws", [128, 512])
            for cg in range(4):
                for kk in range(4):
                    T.dma('sp', pws[:], pool_w[cg, kk * 128:(kk + 1) * 128, :], reads=['pws'], writes=['pws'])
                    T.op('dve', lambda: V.tensor_copy(pw_sb[:, cg, kk, :], pws[:]), reads=['pws', 'pw_sb'], writes=['pw_sb'])
            sg = S.sb("psg_", [128, D]); sg2 = S.sb("psg2", [128, D])
            bcast_row(sg[:], nrm[5], 'psg_')
            bcast_row(sg2[:], modrow[1, 0:1, 2 * D:3 * D], 'psg2')
            T.op('dve', lambda: V.tensor_tensor(sg[:], sg[:], sg2[:], op=ALU.mult), reads=['psg_', 'psg2'], writes=['psg_'])
            HW_ = 528
            hh2 = S.sb("ph2", [128, KC, HW_]); ic = S.sb("pic", [128, 4, 512])
            sa = S.sb("psa", [128, HW_]); sb_ = S.sb("psb", [128, HW_])
            dT = S.sb("pdT", [128, KC, 512], BF16)
            xt = [S.sb(f"qx{i}", [128, D]) for i in range(2)]
            o1 = [S.sb(f"qo{i}", [128, D]) for i in range(2)]
            ps = [S.ps(f"qps{i}", [128, 512]) for i in range(8)]
            bi = 0
            for c0 in range(0, SEQ, 512):
                lo_ = max(c0 - 8, 0); hi_ = min(c0 + 520, SEQ)
                if c0 == 0:
                    T.op('pool', lambda: P.memset(hh2[:, :, 0:8], 0.0), reads=['ph2'], writes=['ph2'])
                if c0 + 520 > SEQ:
                    T.op('pool', lambda: P.memset(hh2[:, :, 520:528], 0.0), reads=['ph2'], writes=['ph2'])
                T.dma('sp', hh2[:, :, lo_ - (c0 - 8):hi_ - (c0 - 8)], hT1[:, lo_:hi_].rearrange("(k p) n -> p k n", p=128), reads=['ph2'], writes=['ph2'])
                for wi_ in range(4):
                    T.dma('sp', ic[:, wi_, :], invcnt[wi_, :, c0:c0 + 512].partition_broadcast(128), reads=['pic'], writes=['pic'])
                for k in range(KC):
                    cg = k // 4
                    h_ = hh2[:, k, :]
                    T.op('dve', lambda: V.tensor_tensor(sa[:, 1:528], h_[:, 0:527], h_[:, 1:528], op=ALU.add), reads=['ph2', 'psa'], writes=['psa'])
                    cur_, curn, oth, othn = sa, 'psa', sb_, 'psb'
                    if cg >= 1:
                        T.op('pool', lambda: P.tensor_tensor(oth[:, 2:526], cur_[:, 1:525], cur_[:, 3:527], op=ALU.add), reads=[curn, othn], writes=[othn])
                        cur_, curn, oth, othn = oth, othn, cur_, curn
                    if cg >= 2:
                        T.op('dve', lambda: V.tensor_tensor(oth[:, 4:524], cur_[:, 2:522], cur_[:, 6:526], op=ALU.add), reads=[curn, othn], writes=[othn])
                        cur_, curn, oth, othn = oth, othn, cur_, curn
                    if cg >= 3:
                        T.op('pool', lambda: P.tensor_tensor(oth[:, 8:520], cur_[:, 4:516], cur_[:, 12:524], op=ALU.add), reads=[curn, othn], writes=[othn])
                        cur_, curn, oth, othn = oth, othn, cur_, curn
                    T.op('dve', lambda: V.tensor_tensor(oth[:, 8:520], cur_[:, 8:520], ic[:, cg, :], op=ALU.mult), reads=[curn, 'pic', othn], writes=[othn])
                    T.op('pool', lambda: P.tensor_tensor(dT[:, k, :], oth[:, 8:520], h_[:, 8:520], op=ALU.subtract), reads=[othn, 'ph2', 'pdT'], writes=['pdT'])
                for j in range(4):
                    t0 = c0 + j * 128
                    x_ = xt[bi % 2]; xn = f"qx{bi % 2}"; o_ = o1[bi % 2]; on = f"qo{bi % 2}"
                    T.dma('sp', x_[:], xin[t0:t0 + 128, :], reads=[xn], writes=[xn])
                    for cg in range(4):
                        pp = ps[(bi * 4 + cg) % 8]; pn = f"qps{(bi * 4 + cg) % 8}"
                        for kk in range(4):
                            T.op('pe', lambda: PE.matmul(pp[:], dT[:, cg * 4 + kk, j * 128:(j + 1) * 128], pw_sb[:, cg, kk, :], start=(kk == 0), stop=(kk == 3)),
                                 reads=['pdT', 'pw_sb'], writes=[pn])
                        T.op('dve', lambda: V.tensor_tensor(o_[:, cg * 512:(cg + 1) * 512], pp[:], sg[:, cg * 512:(cg + 1) * 512], op=ALU.mult),
                             reads=[pn, 'psg_', on], writes=[on])
                    T.op('pool', lambda: P.tensor_tensor(o_[:], o_[:], x_[:], op=ALU.add), reads=[on, xn], writes=[on])
                    T.dma('sp', xout[t0:t0 + 128, :], o_[:], reads=[on])
                    bi += 1
            T.barrier()
            S.close()

        def phase_final(xin):
            S = Scope(nc, T)
            nf = S.sb("fnf", [128, D])
            bcast_row(nf[:], nrm[4], 'fnf')
            xt = [S.sb(f"fx{i}", [128, D]) for i in range(2)]
            ot = [S.sb(f"fo{i}", [128, D]) for i in range(2)]
            for b in range(NBLK):
                t0 = b * 128
                x_ = xt[b % 2]; xn = f"fx{b % 2}"; o_ = ot[b % 2]; on = f"fo{b % 2}"
                T.dma('sp', x_[:], xin[t0:t0 + 128, :], reads=[xn], writes=[xn])
                rms_tok(S, x_, xn, 128, 'fnf', nf, None, o_, on)
                T.dma('sp', out[t0:t0 + 128, :], o_[:], reads=[on], writes=['out'])
            T.barrier()
            S.close()

        stages = [("mod", phase_mod), ("n0", phase_n0)] + [(f"mix{g}", (lambda g=g: mixer_group(g))) for g in range(4)] + [
            ("b", phase_b), ("moe0", lambda: phase_moe(0, x1, acc0)), ("pool", lambda: phase_pool(acc0, x3)),
            ("moe1", lambda: phase_moe(1, x3, acc1)), ("final", lambda: phase_final(acc1))]
        for name, fn in stages:
            fn()
            if upto == name:
                break
        T.finish()
    return nc


def _pcol(v):
    v = np.asarray(v, np.float32)
    return np.ascontiguousarray(v.reshape(-1, 128).T)


def _partner_perm():
    d = np.arange(64)
    return np.where((d % 32) < 16, d + 16, d - 16)


def _consts():
    c = np.zeros((128, 9, 128), np.float32)
    r = np.arange(128)[:, None]; q = np.arange(128)[None, :]
    s = r % 64; t = q % 64
    c[:, 0, :] = (r == q)
    c[:, 1, :] = ((r // 64) == (q // 64))
    c[:, 2, :] = np.where(q < 64, s < t, s <= t)
    c[:, 3, :] = np.where(q < 64, s <= t, s < t)
    c[:, 4, :] = ((r // 64) == (q // 64))
    n = np.arange(64)[None, :]
    c[:, 5, 0:64] = (s == n)
    c[:, 5, 64:128] = (s == 63 - n)
    c[:, 6, 0] = (np.arange(128) < 64)
    c[:, 6, 1] = (np.arange(128) >= 64)
    c[:, 7, :] = (r >= q)
    c[:, 8, :] = (r <= q)
    return c


def _rope_tables(SEQ, grid_w=64, theta=10000.0):
    rows = SEQ // grid_w
    row = np.repeat(np.arange(rows, dtype=np.float32), grid_w)
    col = np.tile(np.arange(grid_w, dtype=np.float32), rows)
    n_freq = 16
    inv = (np.float32(theta) ** (-np.arange(n_freq, dtype=np.float32) / np.float32(n_freq))).astype(np.float32)
    ar = (row[:, None] * inv).astype(np.float32); ac = (col[:, None] * inv).astype(np.float32)
    cosT = np.zeros((128, SEQ), np.float32); sinT = np.zeros((128, SEQ), np.float32)
    for p in range(128):
        d = p % 64
        ang = ar[:, d % 16] if d < 32 else ac[:, d % 16]
        cosT[p] = np.cos(ang)
        sinT[p] = np.sin(ang) * (-1.0 if (d % 32) < 16 else 1.0)
    return cosT, sinT


def prep_stageA(inp, mod_x, mod_c, b, g, SEQ, CTX, weights_only=False):
    f = np.float32
    m = {}
    if not weights_only:
        x = np.asarray(inp["x"][b], f); ctx = np.asarray(inp["ctx"][b], f)
        m["xT"] = np.ascontiguousarray(x.T); m["xTr"] = np.ascontiguousarray(x[::-1].T)
        m["cT"] = np.ascontiguousarray(ctx.T); m["cTr"] = np.ascontiguousarray(ctx[::-1].T)
        mx = np.asarray(mod_x, f).reshape(6, D); mc = np.asarray(mod_c, f).reshape(6, D)
        m["modv"] = np.ascontiguousarray(np.stack([_pcol(inp["norm_mix"][0]), _pcol(mx[1]), _pcol(mx[0]), _pcol(mc[1]), _pcol(mc[0])], axis=1))
    w = np.asarray(inp["w_in"][0], f)
    hs = slice(256 * g, 256 * g + 256)
    perm = _partner_perm()
    qcols = 3488 + 256 * g + np.arange(256)
    qpcols = 3488 + 256 * g + (np.arange(4)[:, None] * 64 + perm[None, :]).reshape(-1)
    kcols = 4512 + 64 * g + np.arange(64); kpcols = 4512 + 64 * g + perm
    vcols = 4768 + 64 * g + np.arange(64)
    rkv = np.concatenate([np.arange(0, 1024)[hs], np.arange(1024, 2048)[hs], np.arange(2048, 3072)[hs]])
    colsf = np.concatenate([rkv, np.arange(3072, 3136), np.arange(3200, 3264), np.arange(3328, 3488), qcols, qpcols, kcols, kpcols, vcols])
    colsb = np.concatenate([rkv, np.arange(3136, 3200), np.arange(3264, 3328)])
    assert len(colsf) == NF and len(colsb) == NB
    m["Wf"] = np.ascontiguousarray(w[:, colsf]); m["Wb"] = np.ascontiguousarray(w[:, colsb])
    mu = np.asarray(inp["shift_mu"][0], f)
    vec = np.zeros((128, 32), f)
    for p in range(2):
        sl = slice(256 * g + 128 * p, 256 * g + 128 * p + 128)
        for qi in range(3):
            vec[:, 3 * p + qi] = mu[qi * 1024:(qi + 1) * 1024][sl]
        for d in range(2):
            vec[:, 8 + 2 * d + p] = np.asarray(inp["decay_w0"][0][d], f)[sl]
            vec[:, 12 + 2 * d + p] = np.asarray(inp["iclr_a0"][0][d], f)[sl]
        vec[:, 16 + p] = np.asarray(inp["k_k"][0], f)[sl]; vec[:, 18 + p] = np.asarray(inp["k_a"][0], f)[sl]
        vec[:, 20 + p] = np.asarray(inp["r_k"][0], f)[sl]; vec[:, 22 + p] = np.asarray(inp["ln_w"][0], f)[sl]
        vec[:, 24 + p] = np.asarray(inp["ln_b"][0], f)[sl]
    for d in range(2):
        vec[0:64, 6 + d] = mu[3072 + 64 * d:3072 + 64 * d + 64]
        vec[64:128, 6 + d] = mu[3200 + 64 * d:3200 + 64 * d + 64]
    m["vec"] = vec
    w2a2 = np.zeros((128, 2, 2, 128), f)
    for d in range(2):
        for p in range(2):
            sl = slice(256 * g + 128 * p, 256 * g + 128 * p + 128)
            w2a2[0:64, d, p, :] = np.asarray(inp["decay_w2"][0][d], f)[:, sl]
            w2a2[64:128, d, p, :] = np.asarray(inp["iclr_a2"][0][d], f)[:, sl]
    m["w2a2"] = w2a2
    g2 = np.asarray(inp["gate_g2"][0], f)
    m["g2a"] = np.ascontiguousarray(g2[0:128, hs]); m["g2b"] = np.ascontiguousarray(g2[128:160, hs])
    mg = np.zeros((128, 2), f); mg[:, 0] = mu[3328:3456]; mg[0:32, 1] = mu[3456:3488]
    m["mugd"] = mg
    if not weights_only:
        cosT, sinT = _rope_tables(SEQ)
        m["cosT"] = cosT; m["sinT"] = sinT
    m["sinkb"] = np.ascontiguousarray(np.broadcast_to(np.asarray(inp["sink"][0], f)[4 * g:4 * g + 4][None, :], (128, 4)))
    m["cst"] = _consts()
    return m


def _consts2():
    c = np.zeros((128, 4, 128), np.float32)
    r = np.arange(128)[:, None]; q = np.arange(128)[None, :]
    c[:, 0, :] = (r == 127 - q)
    c[:, 1, :] = ((r // 8) == (q // 8))
    c[:, 2, :] = ((r // 8) == (q // 8)) & (r < q)
    return c


def prep_all(inp, b, SEQ, CTX):
    f = np.float32
    m = {}
    m["x"] = np.ascontiguousarray(np.asarray(inp["x"][b], f)); m["ctx"] = np.ascontiguousarray(np.asarray(inp["ctx"][b], f))
    m["cvec"] = np.ascontiguousarray(np.stack([_pcol(inp["c"][b]), _pcol(inp["c_ctx"])], axis=2))
    m["ada_w"] = np.asarray(inp["ada_w"], f); m["ada_b"] = np.asarray(inp["ada_b"], f).reshape(2, 1, 6 * D)
    m["nrm"] = np.stack([np.asarray(inp["norm_mix"][0], f), np.asarray(inp["norm_ffn"][0], f), np.asarray(inp["norm_mix"][1], f),
                         np.asarray(inp["norm_ffn"][1], f), np.asarray(inp["norm_final"], f), np.asarray(inp["pool_scale"][0], f)]).reshape(6, 1, D)
    zero_mod = np.zeros(6 * D, f)
    parts = [prep_stageA(inp, zero_mod, zero_mod, b, g, SEQ, CTX, weights_only=True) for g in range(4)]
    m["Wf_all"] = np.stack([p["Wf"] for p in parts]); m["Wb_all"] = np.stack([p["Wb"] for p in parts])
    m["vec_all"] = np.stack([p["vec"] for p in parts]); m["w2a2_all"] = np.stack([p["w2a2"] for p in parts])
    m["g2a_all"] = np.stack([p["g2a"] for p in parts]); m["g2b_all"] = np.stack([p["g2b"] for p in parts])
    m["mugd"] = parts[0]["mugd"]; m["sinkb_all"] = np.stack([p["sinkb"] for p in parts])
    m["cosT"], m["sinT"] = _rope_tables(SEQ)
    m["cst"] = _consts(); m["cst2"] = _consts2()
    m["w_out"] = np.asarray(inp["w_out"][0], f)
    m["pool_w"] = np.asarray(inp["pool_w"][0], f)
    pos = np.arange(SEQ)
    ic = np.zeros((4, 1, SEQ), f)
    for i, w in enumerate((2, 4, 8, 16)):
        lo = np.clip(pos - w // 2, 0, SEQ); hi = np.clip(pos + w // 2, 0, SEQ)
        ic[i, 0] = 1.0 / (hi - lo).astype(f)
    m["invcnt"] = ic
    m["router_w"] = np.asarray(inp["router_w"], f)
    m["exp_w_gate"] = np.asarray(inp["exp_w_gate"], f); m["exp_w_up"] = np.asarray(inp["exp_w_up"], f); m["exp_w_down"] = np.asarray(inp["exp_w_down"], f)
    return m


def kernel(**inputs):
    SEQ, CTX = 16384, 256
    nc = build_all(SEQ, CTX)
    maps = [prep_all(inputs, b, SEQ, CTX) for b in range(2)]
    res = run_bass_kernel_spmd(nc, maps, core_ids=[0, 1])
    return np.stack([np.asarray(res.results[b]["out"], np.float32) for b in range(2)])
```

```python
import numpy as np
from contextlib import ExitStack
import concourse.bass as bass
import concourse.mybir as mybir
from concourse.bass_utils import run_bass_kernel_spmd

F32 = mybir.dt.float32
BF16 = mybir.dt.bfloat16
I32 = mybir.dt.int32
U32 = mybir.dt.uint32
AF = mybir.ActivationFunctionType
ALU = mybir.AluOpType
AX = mybir.AxisListType

D = 2048
KC = D // 128
NORM_EPS = 1e-6
GN_EPS = 64e-5
C0 = float(np.exp(-0.5))
NF = 1760
NB = 896
RSTOP = None
RVAR = 0
SKIPP = False
NSTREAM = 2


class Tracker:
    EPOCH = 30000
    NDMA = 24

    def __init__(self, nc, stack):
        self.nc = nc
        self.stack = stack
        self.engines = {'pe': nc.tensor, 'act': nc.scalar, 'dve': nc.vector,
                        'pool': nc.gpsimd, 'sp': nc.sync}
        self.cur = {}
        self.nsem = 0
        self.seen = {e: {} for e in self.engines}
        self.regs = {}
        self.dsems = []
        self.dnext = 0
        self.n_inst = 0
        self.rr = 0

    def _newsem(self, tag):
        self.nsem += 1
        return self.stack.enter_context(self.nc.semaphore(f"s{self.nsem}_{tag}"))

    def sb(self, name, shape, dtype):
        return self.stack.enter_context(self.nc.sbuf_tensor(name, shape, dtype))

    def ps(self, name, shape, dtype):
        return self.stack.enter_context(self.nc.psum_tensor(name, shape, dtype))

    def _tick(self, e):
        c = self.cur.get(e)
        if c is None or c[2] >= self.EPOCH:
            ep = 0 if c is None else c[3] + 1
            c = [(e, ep), self._newsem(f"{e}{ep}"), 0, ep]
            self.cur[e] = c
        c[2] += 1
        return (c[0], c[1], c[2])

    def _wait(self, e, dep):
        key, sem, cnt = dep
        if e == 'pe' and key[0] == 'pe':
            return
        if self.seen[e].get(key, 0) >= cnt:
            return
        self.engines[e].wait_ge(sem, cnt)
        self.seen[e][key] = cnt

    def _deps(self, e, reads, writes):
        for r in reads:
            info = self.regs.get(r)
            if info and info['w']:
                self._wait(e, info['w'])
            if info and r.startswith('ps'):
                for rd in info['r']:
                    if rd[0][0] != e:
                        self._wait(e, rd)
        for w in writes:
            info = self.regs.get(w)
            if info:
                if info['w']:
                    self._wait(e, info['w'])
                for rd in info['r']:
                    self._wait(e, rd)

    def _record(self, tok, reads, writes):
        for r in reads:
            info = self.regs.setdefault(r, {'w': None, 'r': []})
            info['r'] = [x for x in info['r'] if x[0] != tok[0]] + [tok]
        for w in writes:
            self.regs[w] = {'w': tok, 'r': []}

    def op(self, e, fn, reads=(), writes=()):
        self._deps(e, reads, writes)
        tok = self._tick(e)
        inst = fn()
        inst.then_inc(tok[1], 1)
        self._record(tok, reads, writes)
        self.n_inst += 1
        return inst

    def dma(self, e, out, in_, reads=(), writes=(), **kw):
        self._deps(e, reads, writes)
        if len(self.dsems) < self.NDMA:
            self.dsems.append([('d', len(self.dsems)), self._newsem(f"d{len(self.dsems)}"), 0])
            d = self.dsems[-1]
        else:
            d = self.dsems[self.dnext % self.NDMA]
        self.dnext += 1
        if d[2] > 0:
            self._wait(e, (d[0], d[1], d[2]))
        d[2] += 16
        tok = (d[0], d[1], d[2])
        inst = self.engines[e].dma_start(out=out, in_=in_, **kw)
        inst.then_inc(d[1], 16)
        self._record(tok, reads, writes)
        self.n_inst += 1
        return inst

    def idma(self, out, out_offset, in_, in_offset, reads=(), writes=(), **kw):
        e = 'pool'
        self._deps(e, reads, writes)
        if not hasattr(self, 'isems'):
            self.isems = []
            self.inext = 0
        if len(self.isems) < 8:
            self.isems.append([('i', len(self.isems)), self._newsem(f"i{len(self.isems)}"), 0])
            d = self.isems[-1]
        else:
            d = self.isems[self.inext % 8]
        self.inext += 1
        if d[2] > 0:
            self._wait(e, (d[0], d[1], d[2]))
        d[2] += 16
        tok = (d[0], d[1], d[2])
        inst = self.nc.gpsimd.indirect_dma_start(out=out, out_offset=out_offset, in_=in_, in_offset=in_offset, **kw)
        inst.then_inc(d[1], 16)
        self._record(tok, reads, writes)
        self.n_inst += 1
        return inst

    def finish(self):
        e = 'sp'
        for d in self.dsems + getattr(self, 'isems', []):
            if d[2] > 0:
                self._wait(e, (d[0], d[1], d[2]))
        for k, c in self.cur.items():
            if k != e:
                self._wait(e, (c[0], c[1], c[2]))


class Scope:
    UID = 0
    def __init__(self, nc, T):
        self.nc = nc
        self.T = T
        self.st = ExitStack()
        self.n = 0

    def sb(self, name, shape, dtype=F32):
        self.n += 1
        Scope.UID += 1
        return self.st.enter_context(self.nc.sbuf_tensor(f"{name}__{Scope.UID}", shape, dtype))

    def ps(self, name, shape, dtype=F32):
        Scope.UID += 1
        return self.st.enter_context(self.nc.psum_tensor(f"{name}__{Scope.UID}", shape, dtype))

    def sb_once(self, name, shape, dtype=F32):
        if not hasattr(self, "_once"):
            self._once = {}
        if name not in self._once:
            self._once[name] = self.sb(name, shape, dtype)
        return self._once[name]

    def close(self):
        self.st.close()


def _barrier(T):
    toks = [(c[0], c[1], c[2]) for c in T.cur.values()]
    dtoks = [(d[0], d[1], d[2]) for d in T.dsems + getattr(T, 'isems', []) if d[2] > 0]
    for e in T.engines:
        for t in toks + dtoks:
            if t[0][0] != e:
                T._wait(e, t)
            elif e != 'pe':
                T._wait(e, t)
    T.regs = {}


Tracker.barrier = _barrier


def build_all(SEQ, CTX, dbg=False, upto=None):
    nc = bass.Bass("TRN2", target_bir_lowering=False)
    TOT = CTX + SEQ
    CAP = 2 * SEQ // 16
    NBLK = SEQ // 128
    XR = 2080
    assert CAP % 128 == 0 and SEQ % 512 == 0 and CTX % 256 == 0

    def din(name, shape, dt=F32):
        return nc.dram_tensor(name, shape, dt, kind="ExternalInput").ap()

    def scr(name, shape, dt=F32):
        return nc.dram_tensor(name, shape, dt, kind=("ExternalOutput" if dbg else "Internal")).ap()

    x_in = din("x", [SEQ, D]); c_in = din("ctx", [CTX, D])
    cvec = din("cvec", [128, KC, 2])
    ada_w = din("ada_w", [2, D, 6 * D]); ada_b = din("ada_b", [2, 1, 6 * D])
    nrm = din("nrm", [6, 1, D])
    Wf_all = din("Wf_all", [4, D, NF]); Wb_all = din("Wb_all", [4, D, NB])
    vec_all = din("vec_all", [4, 128, 32]); w2a2_all = din("w2a2_all", [4, 128, 2, 2, 128])
    g2a_all = din("g2a_all", [4, 128, 256]); g2b_all = din("g2b_all", [4, 32, 256])
    mugd = din("mugd", [128, 2]); sinkb_all = din("sinkb_all", [4, 128, 4])
    cosT = din("cosT", [128, SEQ]); sinT = din("sinT", [128, SEQ])
    cst = din("cst", [128, 9, 128]); cst2 = din("cst2", [128, 4, 128])
    w_out = din("w_out", [D, D])
    pool_w = din("pool_w", [4, 512, 512]); invcnt = din("invcnt", [4, 1, SEQ])
    router_w = din("router_w", [2, D, 16])
    wg = din("exp_w_gate", [2, 16, D, 1024]); wu = din("exp_w_up", [2, 16, D, 1024]); wd = din("exp_w_down", [2, 16, 1024, D])
    out = nc.dram_tensor("out", [SEQ, D], F32, kind="ExternalOutput").ap()

    modrow = scr("modrow", [2, 2, 6 * D])
    hxf = scr("hxf", [D, TOT], BF16); hxr = scr("hxr", [D, TOT], BF16)
    pxf = scr("pxf", [NF, TOT]); pxb = scr("pxb", [NB, TOT])
    yT = scr("yT", [2, 256, SEQ]); bT = scr("bT", [2, 256, SEQ])
    mixs = scr("mixs", [D, SEQ])
    x1 = scr("x1", [SEQ, D]); acc0 = scr("acc0", [SEQ, D]); x3 = scr("x3", [SEQ, D]); acc1 = scr("acc1", [SEQ, D])
    hrow = scr("hrow", [SEQ, XR]); affT = scr("affT", [16, SEQ]); slot_d = scr("slot_d", [16, SEQ])
    Xg = [scr(f"Xg{e}", [CAP + 128, XR]) for e in range(16)]
    hT1 = scr("hT1", [D, SEQ])

    with ExitStack() as st:
        T = Tracker(nc, st)
        G = Scope(nc, T)
        V = nc.vector; A = nc.scalar; P = nc.gpsimd; PE = nc.tensor

        cs = G.sb("cst_sb", [128, 9, 128])
        T.dma('sp', cs[:], cst, writes=['cst'])
        ident = cs[:, 0, :]; onesbd = cs[:, 1, :]; maskA = cs[:, 2, :]; maskB = cs[:, 3, :]
        bdm = cs[:, 4, :]
        JJ = [cs[:, 5, 0:64], cs[:, 5, 64:128]]
        headsel = cs[:, 6, 0:2]
        triLO = cs[:, 7, :]; triHI = cs[:, 8, :]
        cs2 = G.sb("cst2_sb", [128, 4, 128])
        T.dma('sp', cs2[:], cst2, writes=['cst2'])
        J128 = cs2[:, 0, :]; ones8 = cs2[:, 1, :]; low8 = cs2[:, 2, :]
        onesbf = G.sb("onesbf", [128, 128], BF16)
        T.op('pool', lambda: P.memset(onesbf[:], 1.0), writes=['onesbf'])
        ones64 = G.sb("ones64", [128, 128])
        T.op('dve', lambda: V.tensor_scalar(ones64[:], onesbd, 1.0 / 64, None, ALU.mult), reads=['cst'], writes=['ones64'])
        iota_p = G.sb("iota_p", [128, 1])
        T.op('pool', lambda: P.iota(iota_p[:], pattern=[[0, 1]], base=0, channel_multiplier=1, allow_small_or_imprecise_dtypes=True), writes=['iota_p'])
        vec_sb = G.sb("vec_sb", [128, 32])
        omu = G.sb("omu", [128, 10]); hmu = G.sb("hmu", [128, 10])
        mugd_sb = G.sb("mugd_sb", [128, 2])
        T.dma('sp', mugd_sb[:], mugd, writes=['mugd'])

        def bcast_row(tile_ap, row_ap, name):
            T.dma('sp', tile_ap, row_ap.partition_broadcast(128), reads=[name], writes=[name])

        def rms_tok(S, xin, xn, w_, gname, geff, shrow, out_t, on, eps=NORM_EPS):
            ss = S.sb_once("rms_ss", [128, 1]); sq = S.sb_once("rms_sq", [128, D])
            T.op('act', lambda: A.activation(sq[:], xin[:], AF.Square, accum_out=ss[:]), reads=[xn], writes=['rms_sq', 'rms_ss'])
            T.op('act', lambda: A.activation(ss[:], ss[:], AF.Sqrt, bias=eps, scale=1.0 / D), reads=['rms_ss'], writes=['rms_ss'])
            T.op('dve', lambda: V.reciprocal(ss[:], ss[:]), reads=['rms_ss'], writes=['rms_ss'])
            if shrow is None:
                T.op('dve', lambda: V.scalar_tensor_tensor(out_t[:], xin[:], ss[:, 0:1], geff[:], ALU.mult, ALU.mult), reads=[xn, 'rms_ss', gname], writes=[on])
            else:
                T.op('dve', lambda: V.scalar_tensor_tensor(sq[:], xin[:], ss[:, 0:1], geff[:], ALU.mult, ALU.mult), reads=[xn, 'rms_ss', gname, 'rms_sq'], writes=['rms_sq'])
                T.op('pool', lambda: P.tensor_tensor(out_t[:], sq[:], shrow[:], op=ALU.add), reads=['rms_sq', gname], writes=[on])

        def mod_rows(S, l, who, idx_sc, idx_sh, nrm_i, gname):
            geff = S.sb(gname + "_g", [128, D]); shr = S.sb(gname + "_s", [128, D]); nw = S.sb(gname + "_n", [128, D])
            bcast_row(geff[:], modrow[l, who:who + 1, idx_sc * D:(idx_sc + 1) * D], gname)
            bcast_row(shr[:], modrow[l, who:who + 1, idx_sh * D:(idx_sh + 1) * D], gname)
            bcast_row(nw[:], nrm[nrm_i], gname)
            T.op('dve', lambda: V.scalar_tensor_tensor(geff[:], geff[:], 1.0, nw[:], ALU.add, ALU.mult), reads=[gname], writes=[gname])
            return geff, shr

        def phase_mod():
            S = Scope(nc, T)
            cv = S.sb("cv", [128, KC, 2])
            T.dma('sp', cv[:], cvec, writes=['cv'])
            T.op('act', lambda: A.activation(cv[:], cv[:], AF.Silu), reads=['cv'], writes=['cv'])
            wt = [S.sb(f"adaw{i}", [128, 2048]) for i in range(3)]
            psm = [S.ps(f"psm{i}", [128, 512]) for i in range(4)]
            brow = S.sb("brow", [2, 2048]); mrow = S.sb("mrow", [2, 2048])
            wi = 0
            for l in range(2):
                for cg in range(6):
                    for k in range(KC):
                        w = wt[wi % 3]; wn = f"adaw{wi % 3}"; wi += 1
                        T.dma('sp', w[:], ada_w[l, k * 128:(k + 1) * 128, cg * 2048:(cg + 1) * 2048], reads=[wn], writes=[wn])
                        for j in range(4):
                            T.op('pe', lambda: PE.matmul(psm[j][0:2, :], cv[:, k, :], w[:, j * 512:(j + 1) * 512], start=(k == 0), stop=(k == KC - 1)),
                                 reads=['cv', wn], writes=[f"psm{j}"])
                    for r in range(2):
                        T.dma('sp', brow[r:r + 1, :], ada_b[l, :, cg * 2048:(cg + 1) * 2048], reads=['brow'], writes=['brow'])
                    for j in range(4):
                        T.op('dve', lambda: V.tensor_tensor(mrow[:, j * 512:(j + 1) * 512], psm[j][0:2, :], brow[:, j * 512:(j + 1) * 512], op=ALU.add),
                             reads=[f"psm{j}", 'brow', 'mrow'], writes=['mrow'])
                    T.dma('sp', modrow[l, :, cg * 2048:(cg + 1) * 2048], mrow[:], reads=['mrow'])
            T.barrier()
            S.close()

        def phase_n0():
            S = Scope(nc, T)
            xin = [S.sb(f"n0x{i}", [128, D]) for i in range(2)]
            hh = S.sb("n0h", [128, D])
            hTf = S.sb("hTf", [128, KC, 512], BF16); hTr = S.sb("hTr", [128, KC, 512], BF16)
            pst = [S.ps(f"n0ps{i}", [128, 512]) for i in range(4)]
            pi = 0; xi = 0
            for (src, who, L, base) in ((c_in, 1, CTX, 0), (x_in, 0, SEQ, CTX)):
                geff, shr = mod_rows(S, 0, who, 1, 0, 0, f"n0m{who}")
                for c0 in range(0, L, 512):
                    wd_ = min(512, L - c0)
                    nb = wd_ // 128
                    for j in range(nb):
                        xt = xin[xi % 2]; xn = f"n0x{xi % 2}"; xi += 1
                        T.dma('sp', xt[:], src[c0 + j * 128:c0 + (j + 1) * 128, :], reads=[xn], writes=[xn])
                        rms_tok(S, xt, xn, 128, f"n0m{who}", geff, shr, hh, 'n0h')
                        for kq in range(4):
                            for (dst, dn, idm, jj) in ((hTf, 'hTf', ident, j), (hTr, 'hTr', J128, nb - 1 - j)):
                                pp = pst[pi % 4]; pn = f"n0ps{pi % 4}"; pi += 1
                                for k4 in range(4):
                                    k = kq * 4 + k4
                                    T.op('pe', lambda: PE.matmul(pp[:, k4 * 128:(k4 + 1) * 128], hh[:, k * 128:(k + 1) * 128], idm, start=True, stop=True),
                                         reads=['n0h', 'cst', 'cst2'], writes=[pn])
                                eng = 'act' if pi % 2 == 0 else 'dve'
                                o_ = dst[:, kq * 4:(kq + 1) * 4, jj * 128:(jj + 1) * 128]
                                i_ = pp[:].rearrange("p (k n) -> p k n", n=128)
                                if eng == 'act':
                                    T.op('act', lambda: A.activation(o_, i_, AF.Copy), reads=[pn, dn], writes=[dn])
                                else:
                                    T.op('dve', lambda: V.tensor_copy(o_, i_), reads=[pn, dn], writes=[dn])
                    T.dma('sp', hxf[:, base + c0:base + c0 + wd_].rearrange("(k p) n -> p k n", p=128), hTf[:, :, 0:wd_], reads=['hTf'])
                    r0 = base + (L - c0 - wd_)
                    T.dma('sp', hxr[:, r0:r0 + wd_].rearrange("(k p) n -> p k n", p=128), hTr[:, :, 0:wd_], reads=['hTr'])
            T.barrier()
            S.close()

        def mixer_group(g):
            w2a2 = w2a2_all[g]; g2a = g2a_all[g]; g2b = g2b_all[g]; sinkb = sinkb_all[g]
            mixT_rw = mixs[256 * g:256 * g + 256, :]
            mixT_att = mixs[1024 + 256 * g:1024 + 256 * g + 256, :]
            T.dma('sp', vec_sb[:], vec_all[g], reads=['vec'], writes=['vec'])
            T.op('dve', lambda: V.tensor_scalar(omu[:, 0:8], vec_sb[:, 0:8], -1.0, 1.0, ALU.mult, ALU.add), reads=['vec', 'omu'], writes=['omu'])
            T.op('dve', lambda: V.tensor_scalar(hmu[:, 0:8], vec_sb[:, 0:8], 0.5, None, ALU.mult), reads=['vec', 'hmu'], writes=['hmu'])
            T.op('dve', lambda: V.tensor_scalar(omu[:, 8:10], mugd_sb[:], -1.0, 1.0, ALU.mult, ALU.add), reads=['mugd', 'omu'], writes=['omu'])
            T.op('dve', lambda: V.tensor_scalar(hmu[:, 8:10], mugd_sb[:], 0.5, None, ALU.mult), reads=['mugd', 'hmu'], writes=['hmu'])
            S = Scope(nc, T)
            Wf_sb = S.sb("Wf_sb", [128, KC, NF], BF16)
            Wb_sb = S.sb("Wb_sb", [128, KC, NB], BF16)
            wst = [S.sb(f"wst{i}", [128, NF]) for i in range(2)]
            ci = 0
            for (Wd, Wsb, ncol) in ((Wf_all[g], Wf_sb, NF), (Wb_all[g], Wb_sb, NB)):
                for k in range(KC):
                    w = wst[ci % 2]; wn = f"wst{ci % 2}"
                    T.dma('sp', w[:, 0:ncol], Wd[k * 128:(k + 1) * 128, :], reads=[wn], writes=[wn])
                    eng = ('dve', 'pool')[ci % 2]
                    E = V if eng == 'dve' else P
                    T.op(eng, lambda: E.tensor_copy(Wsb[:, k, :], w[:, 0:ncol]), reads=[wn], writes=[f"W{ncol}_{k}"])
                    ci += 1
            PW = 512
            hx = [S.sb(f"hx{i}", [128, KC, PW], BF16) for i in range(2)]
            ost = [S.sb(f"ost{i}", [128, PW]) for i in range(3)]
            psP = [S.ps(f"psP{i}", [128, PW]) for i in range(4)]
            oi = 0; hi = 0
            for (src, Wsb, ncol, pxo) in ((hxf, Wf_sb, NF, pxf), (hxr, Wb_sb, NB, pxb)):
                for c0 in range(0, TOT, PW):
                    w_ = min(PW, TOT - c0)
                    h_ = hx[hi % 2]; hn = f"hx{hi % 2}"; hi += 1
                    T.dma('sp', h_[:, :, 0:w_], src[:, c0:c0 + w_].rearrange("(k p) n -> p k n", p=128), reads=[hn], writes=[hn])
                    for m0 in range(0, ncol, 128):
                        mw = min(128, ncol - m0)
                        pp = psP[oi % 4]; pn = f"psP{oi % 4}"; oo = ost[oi % 3]; on = f"ost{oi % 3}"
                        for k in range(KC):
                            T.op('pe', lambda: PE.matmul(pp[0:mw, 0:w_], Wsb[:, k, m0:m0 + mw], h_[:, k, 0:w_], start=(k == 0), stop=(k == KC - 1)),
                                 reads=[f"W{ncol}_{k}", hn], writes=[pn])
                        if oi % 2 == 0:
                            T.op('act', lambda: A.activation(oo[0:mw, 0:w_], pp[0:mw, 0:w_], AF.Copy), reads=[pn, on], writes=[on])
                        else:
                            T.op('dve', lambda: V.tensor_copy(oo[0:mw, 0:w_], pp[0:mw, 0:w_]), reads=[pn, on], writes=[on])
                        T.dma('sp', pxo[m0:m0 + mw, c0:c0 + w_], oo[0:mw, 0:w_], reads=[on])
                        oi += 1
            T.barrier()
            S.close()
            TW = 256
            NCH = TW // 64
            tiles = []
            for (s0, L) in ((0, CTX), (CTX, SEQ)):
                for c0 in range(s0, s0 + L, TW):
                    assert c0 + TW <= s0 + L
                    tiles.append((s0, s0 + L, c0, s0 == CTX))

            S = Scope(nc, T)
            w2a2_sb = S.sb("w2a2_sb", [128, 2, 2, 128])
            T.dma('sp', w2a2_sb[:], w2a2, writes=['w2a2'])
            rmask = S.sb("rmask", [128, TW])
            T.op('pool', lambda: P.memset(rmask[:], 1.0), writes=['rmask'])
            T.op('pool', lambda: P.memset(rmask[:, 0:TW:64], 0.0), reads=['rmask'], writes=['rmask'])

            class Strm:
                pass

            def mk_stream(si):
                s = Strm()
                s.si = si
                n = lambda x: f"{x}_{si}"
                s.n = n
                for nm in ("rl", "kl", "vl", "ll", "tl", "sg", "aa", "t0", "t1", "kkn", "kmod", "bb", "cs_", "epos", "eneg", "eprev", "rk"):
                    setattr(s, nm, S.sb(n(nm), [128, TW]))
                s.raw = S.sb(n("raw"), [128, 4, TW + 2])
                s.AR = S.sb(n("AR"), [128, NCH, 128]); s.BK = S.sb(n("BK"), [128, NCH, 128])
                s.V2 = S.sb(n("V2"), [128, NCH, 128]); s.RK2 = S.sb(n("RK2"), [128, NCH, 128])
                s.Yout = S.sb(n("Yout"), [128, 2, TW])
                s.GB = S.sb(n("GB"), [128, 128]); s.GK = S.sb(n("GK"), [128, 128])
                s.PT = [S.sb(n(f"PT{i}"), [128, 256]) for i in range(2)]
                s.PkT = [S.sb(n(f"PkT{i}"), [128, 128]) for i in range(2)]
                s.Tbd = S.sb(n("Tbd"), [128, 128]); s.BKT = S.sb(n("BKT"), [128, 128])
                s.VV = S.sb(n("VV"), [128, 128]); s.UU = S.sb(n("UU"), [128, 128])
                s.WY = S.sb(n("WY"), [128, 128]); s.Ysb = S.sb(n("Ysb"), [128, 128]); s.Bsb = S.sb(n("Bsb"), [128, 128])
                s.csb = S.sb(n("csb"), [128, 2])
                s.Sbd = [S.sb(n(f"Sbd{i}"), [128, 128]) for i in range(2)]
                s.psa = S.ps(n("psa"), [128, 512])
                s.psb = S.ps(n("psb"), [128, 512])
                for t_, nm in ((s.VV, "VV"), (s.UU, "UU"), (s.Ysb, "Ysb"), (s.Bsb, "Bsb"), (s.Sbd[0], "Sbd0"), (s.Sbd[1], "Sbd1")):
                    T.op('pool', lambda: P.memset(t_[:], 0.0), writes=[n(nm)])
                return s

            def rwkv_stream(s, d, p):
                n = s.n
                px = pxf if d == 0 else pxb
                rows = [p * 128, 256 + p * 128, 512 + p * 128, 768]
                mucol = [3 * p + 0, 3 * p + 1, 3 * p + 2, 6 + d]
                dst = [s.rl, s.kl, s.vl, s.ll]
                dstn = [n("rl"), n("kl"), n("vl"), n("ll")]
                w0c = vec_sb[:, 8 + 2 * d + p: 9 + 2 * d + p]
                a0c = vec_sb[:, 12 + 2 * d + p: 13 + 2 * d + p]
                kkc = vec_sb[:, 16 + p:17 + p]; kac = vec_sb[:, 18 + p:19 + p]; rkc = vec_sb[:, 20 + p:21 + p]
                cur = 0
                for i in range(2):
                    T.op('pool', lambda: P.memset(s.Sbd[i][:], 0.0), reads=[n(f"Sbd{i}")], writes=[n(f"Sbd{i}")])
                for (s0, s1, c0, isx) in tiles:
                    for qi in range(4):
                        lo_ = max(c0 - 1, s0); hi_ = min(c0 + TW + 1, s1)
                        if c0 - 1 < s0:
                            T.op('pool', lambda: P.memset(s.raw[:, qi, 0:1], 0.0), reads=[n(f"raw{qi}")], writes=[n(f"raw{qi}")])
                        if c0 + TW + 1 > s1:
                            T.op('pool', lambda: P.memset(s.raw[:, qi, TW + 1:TW + 2], 0.0), reads=[n(f"raw{qi}")], writes=[n(f"raw{qi}")])
                        T.dma('sp', s.raw[:, qi, lo_ - (c0 - 1): hi_ - (c0 - 1)], px[rows[qi]:rows[qi] + 128, lo_:hi_],
                              reads=[n(f"raw{qi}")], writes=[n(f"raw{qi}")])
                    for qi in range(4):
                        mc = mucol[qi]
                        T.op('dve', lambda: V.tensor_tensor(s.t0[:], s.raw[:, qi, 0:TW], s.raw[:, qi, 2:TW + 2], op=ALU.add),
                             reads=[n(f"raw{qi}")], writes=[n("t0")])
                        T.op('act', lambda: A.activation(s.t1[:], s.raw[:, qi, 1:TW + 1], AF.Copy, scale=omu[:, mc:mc + 1]),
                             reads=[n(f"raw{qi}"), 'omu'], writes=[n("t1")])
                        T.op('dve', lambda: V.scalar_tensor_tensor(dst[qi][:], s.t0[:], hmu[:, mc:mc + 1], s.t1[:], ALU.mult, ALU.add),
                             reads=[n("t0"), n("t1"), 'hmu'], writes=[dstn[qi]])
                    T.op('act', lambda: A.activation(s.tl[0:64, :], s.ll[0:64, :], AF.Tanh), reads=[n("ll")], writes=[n("tl")])
                    T.op('pe', lambda: PE.matmul(s.psa[:, 0:TW], w2a2_sb[0:64, d, p, :], s.tl[0:64, :], start=True, stop=True),
                         reads=['w2a2', n("tl")], writes=[n("psa")])
                    T.op('act', lambda: A.activation(s.sg[:], s.psa[:, 0:TW], AF.Sigmoid, bias=w0c), reads=[n("psa"), 'vec'], writes=[n("sg")])
                    T.op('pe', lambda: PE.matmul(s.psb[:, 0:TW], w2a2_sb[64:128, d, p, :], s.ll[64:128, :], start=True, stop=True),
                         reads=['w2a2', n("ll")], writes=[n("psb")])
                    T.op('act', lambda: A.activation(s.aa[:], s.psb[:, 0:TW], AF.Sigmoid, bias=a0c), reads=[n("psb"), 'vec'], writes=[n("aa")])
                    yield
                    T.op('act', lambda: A.activation(s.t0[:], s.kl[:], AF.Square, scale=kkc), reads=[n("kl"), 'vec'], writes=[n("t0")])
                    T.op('pe', lambda: PE.matmul(s.psa[:, 0:TW], onesbd, s.t0[:], start=True, stop=True), reads=['cst', n("t0")], writes=[n("psa")])
                    T.op('dve', lambda: V.tensor_scalar(s.t1[:], s.psa[:, 0:TW], 1e-24, None, ALU.max), reads=[n("psa")], writes=[n("t1")])
                    T.op('act', lambda: A.activation(s.t1[:], s.t1[:], AF.Sqrt), reads=[n("t1")], writes=[n("t1")])
                    T.op('dve', lambda: V.reciprocal(s.t1[:], s.t1[:]), reads=[n("t1")], writes=[n("t1")])
                    T.op('dve', lambda: V.scalar_tensor_tensor(s.kkn[:], s.kl[:], kkc, s.t1[:], ALU.mult, ALU.mult),
                         reads=[n("kl"), n("t1"), 'vec'], writes=[n("kkn")])
                    T.op('dve', lambda: V.tensor_scalar(s.t0[:], s.aa[:], -1.0, kac, ALU.add, ALU.mult), reads=[n("aa"), 'vec'], writes=[n("t0")])
                    T.op('dve', lambda: V.scalar_tensor_tensor(s.kmod[:], s.t0[:], 1.0, s.kl[:], ALU.add, ALU.mult),
                         reads=[n("t0"), n("kl")], writes=[n("kmod")])
                    T.op('pool', lambda: P.tensor_tensor(s.bb[:], s.kkn[:], s.aa[:], op=ALU.mult), reads=[n("kkn"), n("aa")], writes=[n("bb")])
                    T.op('dve', lambda: V.tensor_tensor_scan(s.cs_[:], rmask[:], s.sg[:], 0.0, ALU.mult, ALU.add),
                         reads=['rmask', n("sg")], writes=[n("cs_")])
                    T.op('pool', lambda: P.tensor_tensor(s.t1[:], s.cs_[:], s.sg[:], op=ALU.subtract), reads=[n("cs_"), n("sg")], writes=[n("t1")])
                    T.op('act', lambda: A.activation(s.epos[:], s.cs_[:], AF.Exp, scale=-C0), reads=[n("cs_")], writes=[n("epos")])
                    T.op('act', lambda: A.activation(s.eneg[:], s.cs_[:], AF.Exp, scale=C0), reads=[n("cs_")], writes=[n("eneg")])
                    T.op('act', lambda: A.activation(s.eprev[:], s.t1[:], AF.Exp, scale=-C0), reads=[n("t1")], writes=[n("eprev")])
                    c3 = lambda t_, h: t_[64 * h:64 * h + 64, :].rearrange("p (c t) -> p c t", t=64)
                    for h in range(2):
                        lo = 64 * h; ot = 64 * (1 - h)
                        T.op('dve', lambda: V.scalar_tensor_tensor(s.AR[lo:lo + 64, :, lo:lo + 64], c3(s.eprev, h), -1.0, c3(s.kkn, h), ALU.mult, ALU.mult),
                             reads=[n("eprev"), n("kkn"), n("AR")], writes=[n("AR")])
                        T.op('pool', lambda: P.tensor_tensor(s.AR[lo:lo + 64, :, ot:ot + 64], c3(s.epos, h), c3(s.rl, h), op=ALU.mult),
                             reads=[n("epos"), n("rl"), n("AR")], writes=[n("AR")])
                        T.op('dve', lambda: V.tensor_tensor(s.BK[lo:lo + 64, :, lo:lo + 64], c3(s.eneg, h), c3(s.bb, h), op=ALU.mult),
                             reads=[n("eneg"), n("bb"), n("BK")], writes=[n("BK")])
                        T.op('pool', lambda: P.tensor_tensor(s.BK[lo:lo + 64, :, ot:ot + 64], c3(s.eneg, h), c3(s.kmod, h), op=ALU.mult),
                             reads=[n("eneg"), n("kmod"), n("BK")], writes=[n("BK")])
                    vl3 = s.vl[:].rearrange("p (c t) -> p c t", t=64)
                    T.op('pool', lambda: P.tensor_copy(s.V2[:, :, 0:64], vl3), reads=[n("vl"), n("V2")], writes=[n("V2")])
                    T.op('act', lambda: A.activation(s.V2[:, :, 64:128], vl3, AF.Copy), reads=[n("vl"), n("V2")], writes=[n("V2")])
                    T.op('dve', lambda: V.scalar_tensor_tensor(s.rk[:], s.rl[:], rkc, s.kmod[:], ALU.mult, ALU.mult),
                         reads=[n("rl"), n("kmod"), 'vec'], writes=[n("rk")])
                    rk3 = s.rk[:].rearrange("p (c t) -> p c t", t=64)
                    T.op('pool', lambda: P.tensor_copy(s.RK2[:, :, 0:64], rk3), reads=[n("rk"), n("RK2")], writes=[n("RK2")])
                    T.op('act', lambda: A.activation(s.RK2[:, :, 64:128], rk3, AF.Copy), reads=[n("rk"), n("RK2")], writes=[n("RK2")])
                    yield
                    for c in range(NCH):
                        gA = s.psa[:, 0:128]; gB = s.psb[:, 0:128]
                        T.op('pe', lambda: PE.matmul(gA, s.BK[0:64, c, :], s.AR[0:64, c, :], start=True, stop=True),
                             reads=[n("BK"), n("AR")], writes=[n("psa")])
                        T.op('pe', lambda: PE.matmul(gB, s.BK[64:128, c, :], s.AR[64:128, c, :], start=True, stop=True),
                             reads=[n("BK"), n("AR")], writes=[n("psb")])
                        T.op('dve', lambda: V.tensor_tensor(s.GB[0:64, :], gA[0:64, :], maskA[0:64, :], op=ALU.mult), reads=[n("psa"), 'cst', n("GB")], writes=[n("GB")])
                        T.op('dve', lambda: V.tensor_tensor(s.GK[64:128, :], gA[64:128, :], maskA[64:128, :], op=ALU.mult), reads=[n("psa"), 'cst', n("GK")], writes=[n("GK")])
                        T.op('dve', lambda: V.tensor_tensor(s.GK[0:64, :], gB[0:64, :], maskB[0:64, :], op=ALU.mult), reads=[n("psb"), 'cst', n("GK")], writes=[n("GK")])
                        T.op('dve', lambda: V.tensor_tensor(s.GB[64:128, :], gB[64:128, :], maskB[64:128, :], op=ALU.mult), reads=[n("psb"), 'cst', n("GB")], writes=[n("GB")])
                        yield
                        pt = s.PT[0]; ptn = n("PT0")
                        T.op('pool', lambda: P.tensor_tensor(pt[:, 0:128], s.GB[:], bdm, op=ALU.mult), reads=[n("GB"), 'cst', ptn], writes=[ptn])
                        T.op('pool', lambda: P.tensor_copy(pt[:, 128:256], ident), reads=['cst', ptn], writes=[ptn])
                        T.op('pe', lambda: PE.transpose(s.psb[:, 256:384], pt[:, 0:128], ident), reads=[ptn, 'cst'], writes=[n("psb")])
                        T.op('act', lambda: A.activation(s.PkT[0][:], s.psb[:, 256:384], AF.Copy), reads=[n("psb")], writes=[n("PkT0")])
                        yield
                        pi = 0
                        for lvl in range(6):
                            last = lvl == 5
                            pt = s.PT[pi]; ptn = n(f"PT{pi}"); pkt = s.PkT[pi]; pktn = n(f"PkT{pi}")
                            npt = s.PT[1 - pi]; nptn = n(f"PT{1 - pi}"); npkt = s.PkT[1 - pi]; npktn = n(f"PkT{1 - pi}")
                            if not last:
                                T.op('pe', lambda: PE.matmul(s.psb[:, 0:256], pkt[:], pt[:, 0:256], start=True, stop=True), reads=[pktn, ptn], writes=[n("psb")])
                                T.op('pe', lambda: PE.matmul(s.psb[:, 256:384], pt[:, 0:128], pkt[:], start=True, stop=True), reads=[pktn, ptn], writes=[n("psb")])
                                T.op('act', lambda: A.activation(npt[:, 0:128], s.psb[:, 0:128], AF.Copy), reads=[n("psb"), nptn], writes=[nptn])
                                T.op('dve', lambda: V.tensor_tensor(npt[:, 128:256], s.psb[:, 128:256], pt[:, 128:256], op=ALU.add), reads=[n("psb"), ptn, nptn], writes=[nptn])
                                T.op('act', lambda: A.activation(npkt[:], s.psb[:, 256:384], AF.Copy), reads=[n("psb")], writes=[npktn])
                            else:
                                T.op('pe', lambda: PE.matmul(s.psb[:, 0:128], pkt[:], pt[:, 128:256], start=True, stop=True), reads=[pktn, ptn], writes=[n("psb")])
                                T.op('dve', lambda: V.tensor_tensor(s.Tbd[:], s.psb[:, 0:128], pt[:, 128:256], op=ALU.add), reads=[n("psb"), ptn], writes=[n("Tbd")])
                            pi = 1 - pi
                            yield
                        T.op('pe', lambda: PE.transpose(s.psa[:, 256:384], s.BK[:, c, :], ident), reads=[n("BK"), 'cst'], writes=[n("psa")])
                        T.op('act', lambda: A.activation(s.BKT[:], s.psa[:, 256:384], AF.Copy), reads=[n("psa")], writes=[n("BKT")])
                        T.op('pe', lambda: PE.transpose(s.psa[:, 384:512], s.V2[:, c, :], ident), reads=[n("V2"), 'cst'], writes=[n("psa")])
                        T.op('dve', lambda: V.tensor_copy(s.VV[64:128, 0:64], s.psa[64:128, 384:448]), reads=[n("psa"), n("VV")], writes=[n("VV")])
                        T.op('act', lambda: A.activation(s.VV[0:64, 64:128], s.psa[0:64, 448:512], AF.Copy), reads=[n("psa"), n("VV")], writes=[n("VV")])
                        T.op('pe', lambda: PE.matmul(s.psa[:, 128:130], s.RK2[:, c, :], headsel, start=True, stop=True), reads=[n("RK2"), 'cst'], writes=[n("psa")])
                        T.op('dve', lambda: V.tensor_copy(s.csb[:], s.psa[:, 128:130]), reads=[n("psa")], writes=[n("csb")])
                        yield
                        Sc = s.Sbd[cur]; Scn = n(f"Sbd{cur}"); Sn = s.Sbd[1 - cur]; Snn = n(f"Sbd{1 - cur}")
                        T.op('pe', lambda: PE.matmul(s.psa[:, 0:128], s.GK[:], s.VV[:], start=True, stop=False), reads=[n("GK"), n("VV")], writes=[n("psa")])
                        T.op('pe', lambda: PE.matmul(s.psa[:, 0:128], s.AR[:, c, :], Sc[:], start=False, stop=True), reads=[n("AR"), Scn], writes=[n("psa")])
                        T.op('act', lambda: A.activation(s.WY[:], s.psa[:, 0:128], AF.Copy), reads=[n("psa")], writes=[n("WY")])
                        yield
                        T.op('pe', lambda: PE.matmul(s.psa[:, 128:256], s.Tbd[:], s.WY[:], start=True, stop=True), reads=[n("Tbd"), n("WY")], writes=[n("psa")])
                        T.op('dve', lambda: V.tensor_copy(s.UU[0:64, 0:64], s.psa[0:64, 128:192]), reads=[n("psa"), n("UU")], writes=[n("UU")])
                        T.op('act', lambda: A.activation(s.UU[64:128, 64:128], s.psa[64:128, 192:256], AF.Copy), reads=[n("psa"), n("UU")], writes=[n("UU")])
                        yield
                        T.op('pe', lambda: PE.matmul(s.psa[:, 256:384], s.BKT[:], s.VV[:], start=True, stop=False), reads=[n("BKT"), n("VV")], writes=[n("psa")])
                        T.op('pe', lambda: PE.matmul(s.psa[:, 256:384], ident, Sc[:], start=False, stop=False), reads=['cst', Scn], writes=[n("psa")])
                        T.op('pe', lambda: PE.matmul(s.psa[:, 256:384], s.BKT[:], s.UU[:], start=False, stop=True), reads=[n("BKT"), n("UU")], writes=[n("psa")])
                        ce = c * 64 + 63
                        T.op('dve', lambda: V.tensor_scalar(Sn[0:64, 0:64], s.psa[0:64, 256:320], s.epos[0:64, ce:ce + 1], None, ALU.mult),
                             reads=[n("psa"), n("epos"), Snn], writes=[Snn])
                        T.op('act', lambda: A.activation(Sn[64:128, 64:128], s.psa[64:128, 320:384], AF.Copy, scale=s.epos[64:128, ce:ce + 1]),
                             reads=[n("psa"), n("epos"), Snn], writes=[Snn])
                        if isx:
                            T.op('pe', lambda: PE.matmul(s.psa[:, 384:512], s.GB[:], s.UU[:], start=True, stop=True), reads=[n("GB"), n("UU")], writes=[n("psa")])
                            T.op('dve', lambda: V.tensor_tensor(s.Ysb[64:128, 0:64], s.psa[64:128, 384:448], s.WY[64:128, 0:64], op=ALU.add),
                                 reads=[n("psa"), n("WY"), n("Ysb")], writes=[n("Ysb")])
                            T.op('dve', lambda: V.tensor_tensor(s.Ysb[0:64, 64:128], s.psa[0:64, 448:512], s.WY[0:64, 64:128], op=ALU.add),
                                 reads=[n("psa"), n("WY"), n("Ysb")], writes=[n("Ysb")])
                            T.op('pool', lambda: P.tensor_scalar(s.Bsb[64:128, 0:64], s.VV[64:128, 0:64], s.csb[64:128, 0:1], None, ALU.mult),
                                 reads=[n("VV"), n("csb"), n("Bsb")], writes=[n("Bsb")])
                            T.op('pool', lambda: P.tensor_scalar(s.Bsb[0:64, 64:128], s.VV[0:64, 64:128], s.csb[0:64, 1:2], None, ALU.mult),
                                 reads=[n("VV"), n("csb"), n("Bsb")], writes=[n("Bsb")])
                            yield
                            T.op('pe', lambda: PE.matmul(s.psb[:, 384:448], s.Ysb[:], JJ[d], start=True, stop=True), reads=[n("Ysb"), 'cst'], writes=[n("psb")])
                            T.op('pe', lambda: PE.matmul(s.psb[:, 448:512], s.Bsb[:], JJ[d], start=True, stop=True), reads=[n("Bsb"), 'cst'], writes=[n("psb")])
                            cp = c if d == 0 else NCH - 1 - c
                            T.op('act', lambda: A.activation(s.Yout[:, 0, cp * 64:cp * 64 + 64], s.psb[:, 384:448], AF.Copy), reads=[n("psb"), n("Yout")], writes=[n("Yout")])
                            T.op('dve', lambda: V.tensor_copy(s.Yout[:, 1, cp * 64:cp * 64 + 64], s.psb[:, 448:512]), reads=[n("psb"), n("Yout")], writes=[n("Yout")])
                        cur = 1 - cur
                        yield
                    if isx:
                        r0 = c0 - CTX
                        f0 = r0 if d == 0 else SEQ - r0 - TW
                        T.dma('sp', yT[d, p * 128:(p + 1) * 128, f0:f0 + TW], s.Yout[:, 0, :], reads=[n("Yout")])
                        T.dma('sp', bT[d, p * 128:(p + 1) * 128, f0:f0 + TW], s.Yout[:, 1, :], reads=[n("Yout")])

            streams = [mk_stream(i) for i in range(4)]
            gens = [rwkv_stream(streams[2 * d + p], d, p) for d in range(2) for p in range(2)]
            alive = [True] * 4
            while any(alive):
                for i, g_ in enumerate(gens):
                    if alive[i]:
                        try:
                            next(g_)
                        except StopIteration:
                            alive[i] = False
            T.barrier()
            S.close()

            S = Scope(nc, T)
            FW = 512 if SEQ % 512 == 0 else 256
            g2a_sb = S.sb("g2a_sb", [128, 256]); g2b_sb = S.sb("g2b_sb", [32, 256])
            T.dma('sp', g2a_sb[:], g2a, writes=['g2a']); T.dma('sp', g2b_sb[:], g2b, writes=['g2b'])
            graw0 = S.sb("graw0", [128, FW + 2]); graw1 = S.sb("graw1", [32, FW + 2])
            sgd0 = S.sb("sgd0", [128, FW]); sgd1 = S.sb("sgd1", [32, FW])
            ft0 = S.sb("ft0", [128, FW]); ft1 = S.sb("ft1", [128, FW])
            yy = [S.sb(f"yy{i}", [128, FW]) for i in range(4)]
            yc = S.sb("yc", [128, FW]); fsq = S.sb("fsq", [128, FW]); frs = S.sb("frs", [128, FW]); fz = S.sb("fz", [128, FW]); fo = S.sb("fo", [128, FW])
            psF = [S.ps(f"psF{i}", [128, 512]) for i in range(3)]
            for c0 in range(0, SEQ, FW):
                for (gr, grn, r0_, nr, mc) in ((graw0, 'graw0', 896, 128, 8), (graw1, 'graw1', 1024, 32, 9)):
                    lo_ = max(c0 - 1, 0); hi_ = min(c0 + FW + 1, SEQ)
                    if c0 == 0:
                        T.op('pool', lambda: P.memset(gr[0:nr, 0:1], 0.0), reads=[grn], writes=[grn])
                    if c0 + FW + 1 > SEQ:
                        T.op('pool', lambda: P.memset(gr[0:nr, FW + 1:FW + 2], 0.0), reads=[grn], writes=[grn])
                    T.dma('sp', gr[0:nr, lo_ - (c0 - 1): hi_ - (c0 - 1)], pxf[r0_:r0_ + nr, CTX + lo_:CTX + hi_], reads=[grn], writes=[grn])
                    sg_ = sgd0 if nr == 128 else sgd1; sgn = 'sgd0' if nr == 128 else 'sgd1'
                    T.op('dve', lambda: V.tensor_tensor(ft0[0:nr, :], gr[0:nr, 0:FW], gr[0:nr, 2:FW + 2], op=ALU.add), reads=[grn, 'ft0'], writes=['ft0'])
                    T.op('act', lambda: A.activation(ft1[0:nr, :], gr[0:nr, 1:FW + 1], AF.Copy, scale=omu[0:nr, mc:mc + 1]), reads=[grn, 'omu', 'ft1'], writes=['ft1'])
                    T.op('dve', lambda: V.scalar_tensor_tensor(ft0[0:nr, :], ft0[0:nr, :], hmu[0:nr, mc:mc + 1], ft1[0:nr, :], ALU.mult, ALU.add),
                         reads=['ft0', 'ft1', 'hmu'], writes=['ft0'])
                    T.op('act', lambda: A.activation(sg_[0:nr, :], ft0[0:nr, :], AF.Sigmoid), reads=['ft0'], writes=[sgn])
                for p in range(2):
                    lnw = vec_sb[:, 22 + p:23 + p]; lnb = vec_sb[:, 24 + p:25 + p]
                    srcs = [yT[0], yT[1], bT[0], bT[1]]
                    for i in range(4):
                        T.dma('sp', yy[i][:], srcs[i][p * 128:(p + 1) * 128, c0:c0 + FW], writes=[f"yy{i}"])
                    T.op('dve', lambda: V.tensor_tensor(yy[0][:], yy[0][:], yy[1][:], op=ALU.add), reads=['yy0', 'yy1'], writes=['yy0'])
                    T.op('pool', lambda: P.tensor_tensor(yy[2][:], yy[2][:], yy[3][:], op=ALU.add), reads=['yy2', 'yy3'], writes=['yy2'])
                    T.op('pe', lambda: PE.matmul(psF[0][:, 0:FW], ones64[:], yy[0][:], start=True, stop=True), reads=['ones64', 'yy0'], writes=['psF0'])
                    T.op('dve', lambda: V.tensor_tensor(yc[:], yy[0][:], psF[0][:, 0:FW], op=ALU.subtract), reads=['yy0', 'psF0'], writes=['yc'])
                    T.op('act', lambda: A.activation(fsq[:], yc[:], AF.Square), reads=['yc'], writes=['fsq'])
                    T.op('pe', lambda: PE.matmul(psF[1][:, 0:FW], ones64[:], fsq[:], start=True, stop=True), reads=['ones64', 'fsq'], writes=['psF1'])
                    T.op('act', lambda: A.activation(frs[:], psF[1][:, 0:FW], AF.Sqrt, bias=GN_EPS), reads=['psF1'], writes=['frs'])
                    T.op('dve', lambda: V.reciprocal(frs[:], frs[:]), reads=['frs'], writes=['frs'])
                    T.op('dve', lambda: V.tensor_tensor(yc[:], yc[:], frs[:], op=ALU.mult), reads=['yc', 'frs'], writes=['yc'])
                    T.op('act', lambda: A.activation(fz[:], yc[:], AF.Identity, bias=lnb, scale=lnw), reads=['yc', 'vec'], writes=['fz'])
                    T.op('pool', lambda: P.tensor_tensor(fz[:], fz[:], yy[2][:], op=ALU.add), reads=['fz', 'yy2'], writes=['fz'])
                    T.op('pe', lambda: PE.matmul(psF[2][:, 0:FW], g2a_sb[:, p * 128:(p + 1) * 128], sgd0[:], start=True, stop=False), reads=['g2a', 'sgd0'], writes=['psF2'])
                    T.op('pe', lambda: PE.matmul(psF[2][:, 0:FW], g2b_sb[0:32, p * 128:(p + 1) * 128], sgd1[0:32, :], start=False, stop=True), reads=['g2b', 'sgd1'], writes=['psF2'])
                    T.op('dve', lambda: V.tensor_tensor(fo[:], fz[:], psF[2][:, 0:FW], op=ALU.mult), reads=['fz', 'psF2'], writes=['fo'])
                    T.dma('sp', mixT_rw[p * 128:(p + 1) * 128, c0:c0 + FW], fo[:], reads=['fo'])
            T.barrier()
            S.close()

            S = Scope(nc, T)
            AW = 512 if SEQ % 512 == 0 else 256
            NBK = SEQ // 128; NCB = CTX // 128
            QR, QPR, KR, KPR, VR = 1056, 1312, 1568, 1632, 1696
            kT = S.sb("kT", [128, SEQ], BF16); kTc = S.sb("kTc", [128, CTX], BF16)
            Vtm = S.sb("Vtm", [128, NBK, 64], BF16); Vtmc = S.sb("Vtmc", [128, NCB, 64], BF16)
            kraw = S.sb("kraw", [128, AW]); kpraw = S.sb("kpraw", [128, AW]); vraw = S.sb("vraw", [64, AW])
            cos_t = S.sb("cos_t", [128, AW]); sin_t = S.sb("sin_t", [128, AW])
            at0 = S.sb("at0", [128, AW]); at1 = S.sb("at1", [128, AW])
            qraw = S.sb("qraw", [64, 4, AW]); qpraw = S.sb("qpraw", [64, 4, AW]); qT = S.sb("qT", [64, 4, AW], BF16)
            cos4 = S.sb("cos4", [64, 4, AW]); sin4 = S.sb("sin4", [64, 4, AW]); aq0 = S.sb("aq0", [64, 4, AW]); aq1 = S.sb("aq1", [64, 4, AW])
            es = S.sb("es", [128, 4])
            T.dma('sp', es[:], sinkb, writes=['es'])
            T.op('act', lambda: A.activation(es[:], es[:], AF.Exp), reads=['es'], writes=['es'])
            mask4 = [S.sb(f"mask4_{i}", [128, 4, 128], BF16) for i in range(2)]
            for i, tri in enumerate((triLO, triHI)):
                for h in range(4):
                    T.op('dve', lambda: V.tensor_copy(mask4[i][:, h, :], tri), reads=['cst', f"mask4_{i}"], writes=[f"mask4_{i}"])
            ones64bf = S.sb("ones64bf", [128, 64], BF16)
            T.op('pool', lambda: P.memset(ones64bf[:], 1.0), writes=['ones64bf'])
            PTr = [S.sb(f"PTr{i}", [128, 512], BF16) for i in range(6)]
            den = S.sb("den", [64, 512]); att = S.sb("att", [64, 512])
            psA_ = [S.ps(f"psA{i}", [128, 512]) for i in range(3)]
            psO_ = S.ps("psAO", [128, 512]); psD_ = S.ps("psAD", [128, 512]); psVt = S.ps("psVt", [128, 512])
            for h in range(2):
                T.dma('sp', kraw[64 * h:64 * h + 64, 0:CTX], pxf[KR:KR + 64, 0:CTX], reads=['kraw'], writes=['kraw'])
            T.op('dve', lambda: V.tensor_copy(kTc[:], kraw[:, 0:CTX]), reads=['kraw'], writes=['kTc'])
            T.dma('sp', vraw[:, 0:CTX], pxf[VR:VR + 64, 0:CTX], writes=['vraw'])
            for j in range(NCB):
                T.op('pe', lambda: PE.transpose(psVt[:, 0:64], vraw[0:64, j * 128:(j + 1) * 128], ident[0:64, 0:64]), reads=['vraw', 'cst'], writes=['psVt'])
                T.op('dve', lambda: V.tensor_copy(Vtmc[:, j, :], psVt[:, 0:64]), reads=['psVt'], writes=['Vtmc'])
            for c0 in range(0, SEQ, AW):
                for h in range(2):
                    T.dma('sp', kraw[64 * h:64 * h + 64, :], pxf[KR:KR + 64, CTX + c0:CTX + c0 + AW], reads=['kraw'], writes=['kraw'])
                    T.dma('sp', kpraw[64 * h:64 * h + 64, :], pxf[KPR:KPR + 64, CTX + c0:CTX + c0 + AW], reads=['kpraw'], writes=['kpraw'])
                T.dma('sp', cos_t[:], cosT[:, c0:c0 + AW], writes=['cos_t'])
                T.dma('sp', sin_t[:], sinT[:, c0:c0 + AW], writes=['sin_t'])
                T.dma('sp', vraw[:, 0:AW], pxf[VR:VR + 64, CTX + c0:CTX + c0 + AW], writes=['vraw'])
                T.op('dve', lambda: V.tensor_tensor(at0[:], kraw[:], cos_t[:], op=ALU.mult), reads=['kraw', 'cos_t'], writes=['at0'])
                T.op('pool', lambda: P.tensor_tensor(at1[:], kpraw[:], sin_t[:], op=ALU.mult), reads=['kpraw', 'sin_t'], writes=['at1'])
                T.op('dve', lambda: V.tensor_tensor(kT[:, c0:c0 + AW], at0[:], at1[:], op=ALU.add), reads=['at0', 'at1'], writes=['kT'])
                for j in range(AW // 128):
                    T.op('pe', lambda: PE.transpose(psVt[:, 0:64], vraw[0:64, j * 128:(j + 1) * 128], ident[0:64, 0:64]), reads=['vraw', 'cst'], writes=['psVt'])
                    T.op('dve', lambda: V.tensor_copy(Vtm[:, c0 // 128 + j, :], psVt[:, 0:64]), reads=['psVt'], writes=['Vtm'])
            pi_ = 0; ai = 0
            for c0 in range(0, SEQ, AW):
                for h in range(4):
                    T.dma('sp', qraw[:, h, :], pxf[QR + h * 64:QR + h * 64 + 64, CTX + c0:CTX + c0 + AW], reads=['qraw'], writes=['qraw'])
                    T.dma('sp', qpraw[:, h, :], pxf[QPR + h * 64:QPR + h * 64 + 64, CTX + c0:CTX + c0 + AW], reads=['qpraw'], writes=['qpraw'])
                    T.dma('sp', cos4[:, h, :], cosT[0:64, c0:c0 + AW], reads=['cos4'], writes=['cos4'])
                    T.dma('sp', sin4[:, h, :], sinT[0:64, c0:c0 + AW], reads=['sin4'], writes=['sin4'])
                T.op('dve', lambda: V.tensor_tensor(aq0[:], qraw[:], cos4[:], op=ALU.mult), reads=['qraw', 'cos4'], writes=['aq0'])
                T.op('pool', lambda: P.tensor_tensor(aq1[:], qpraw[:], sin4[:], op=ALU.mult), reads=['qpraw', 'sin4'], writes=['aq1'])
                T.op('dve', lambda: V.tensor_tensor(qT[:], aq0[:], aq1[:], op=ALU.add), reads=['aq0', 'aq1', 'qT'], writes=['qT'])
                for jb in range(AW // 128):
                    nq = c0 // 128 + jb
                    kbs = [('c', j, None) for j in range(NCB)]
                    if nq - 1 >= 0:
                        kbs.append(('x', nq - 1, 0))
                    kbs.append(('x', nq, None))
                    if nq + 1 < NBK:
                        kbs.append(('x', nq + 1, 1))
                    pts = []
                    for (kind, kb, mk) in kbs:
                        pa = psA_[ai % 3]; pan = f"psA{ai % 3}"; ai += 1
                        for h in range(4):
                            ksrc = kTc if kind == 'c' else kT
                            T.op('pe', lambda: PE.matmul(pa[:, h * 128:(h + 1) * 128], ksrc[0:64, kb * 128:(kb + 1) * 128],
                                                         qT[0:64, h, jb * 128:(jb + 1) * 128], start=True, stop=True),
                                 reads=['kT', 'kTc', 'qT'], writes=[pan])
                        pt = PTr[pi_ % 6]; ptn = f"PTr{pi_ % 6}"; pi_ += 1
                        T.op('act', lambda: A.activation(pt[:], pa[:], AF.Exp, scale=0.125), reads=[pan], writes=[ptn])
                        if mk is not None:
                            T.op('pool', lambda: P.tensor_tensor(pt[:], pt[:], mask4[mk][:].rearrange("p h n -> p (h n)"), op=ALU.mult),
                                 reads=[ptn, f"mask4_{mk}"], writes=[ptn])
                        pts.append((pt, ptn, kind, kb))
                    for i, (pt, ptn, kind, kb) in enumerate(pts):
                        vsrc = Vtmc if kind == 'c' else Vtm
                        T.op('pe', lambda: PE.matmul(psO_[0:64, :], vsrc[:, kb, :], pt[:], start=(i == 0), stop=(i == len(pts) - 1)),
                             reads=['Vtm', 'Vtmc', ptn], writes=['psAO'])
                        T.op('pe', lambda: PE.matmul(psD_[0:64, :], ones64bf[:], pt[:], start=(i == 0), stop=(i == len(pts) - 1)),
                             reads=['ones64bf', ptn], writes=['psAD'])
                    for h in range(4):
                        T.op('dve', lambda: V.tensor_scalar(den[:, h * 128:(h + 1) * 128], psD_[0:64, h * 128:(h + 1) * 128], es[0:64, h:h + 1], None, ALU.add),
                             reads=['psAD', 'es', 'den'], writes=['den'])
                    T.op('dve', lambda: V.reciprocal(den[:], den[:]), reads=['den'], writes=['den'])
                    T.op('dve', lambda: V.tensor_tensor(att[:], psO_[0:64, :], den[:], op=ALU.mult), reads=['psAO', 'den'], writes=['att'])
                    q0 = nq * 128
                    T.dma('sp', mixT_att[0:256, q0:q0 + 128].rearrange("(h p) n -> p h n", p=64), att[:].rearrange("p (h n) -> p h n", n=128), reads=['att'])
            T.barrier()
            S.close()


        def phase_b():
            S = Scope(nc, T)
            wo_sb = S.sb("wo_sb", [128, KC, D], BF16)
            wst = [S.sb(f"bwst{i}", [128, D]) for i in range(2)]
            for k in range(KC):
                w = wst[k % 2]; wn = f"bwst{k % 2}"
                T.dma('sp', w[:], w_out[k * 128:(k + 1) * 128, :], reads=[wn], writes=[wn])
                E = V if k % 2 == 0 else P
                T.op('dve' if k % 2 == 0 else 'pool', lambda: E.tensor_copy(wo_sb[:, k, :], w[:]), reads=[wn], writes=[f"wo{k}"])
            gta = S.sb("gta", [128, D])
            bcast_row(gta[:], modrow[0, 0:1, 2 * D:3 * D], 'gta')
            mt = S.sb("bmt", [128, KC, 512]); mtb = S.sb("bmtb", [128, KC, 512], BF16)
            xt = [S.sb(f"bx{i}", [128, D]) for i in range(2)]
            o1 = [S.sb(f"bo{i}", [128, D]) for i in range(2)]
            ps = [S.ps(f"bps{i}", [128, 512]) for i in range(8)]
            bi = 0
            for c0 in range(0, SEQ, 512):
                T.dma('sp', mt[:], mixs[:, c0:c0 + 512].rearrange("(k p) n -> p k n", p=128), reads=['bmt'], writes=['bmt'])
                T.op('act', lambda: A.activation(mtb[:], mt[:], AF.Copy), reads=['bmt', 'bmtb'], writes=['bmtb'])
                for j in range(4):
                    t0 = c0 + j * 128
                    x_ = xt[bi % 2]; xn = f"bx{bi % 2}"; o_ = o1[bi % 2]; on = f"bo{bi % 2}"
                    T.dma('sp', x_[:], x_in[t0:t0 + 128, :], reads=[xn], writes=[xn])
                    for ct in range(4):
                        pp = ps[(bi * 4 + ct) % 8]; pn = f"bps{(bi * 4 + ct) % 8}"
                        for k in range(KC):
                            T.op('pe', lambda: PE.matmul(pp[:], mtb[:, k, j * 128:(j + 1) * 128], wo_sb[:, k, ct * 512:(ct + 1) * 512], start=(k == 0), stop=(k == KC - 1)),
                                 reads=['bmtb', f"wo{k}"], writes=[pn])
                        T.op('dve', lambda: V.tensor_tensor(o_[:, ct * 512:(ct + 1) * 512], pp[:], gta[:, ct * 512:(ct + 1) * 512], op=ALU.mult),
                             reads=[pn, 'gta', on], writes=[on])
                    T.op('pool', lambda: P.tensor_tensor(o_[:], o_[:], x_[:], op=ALU.add), reads=[on, xn], writes=[on])
                    T.dma('sp', x1[t0:t0 + 128, :], o_[:], reads=[on])
                    bi += 1
            T.barrier()
            S.close()

        def phase_moe(l, xin, acc):
            S = Scope(nc, T)
            geff, shr = mod_rows(S, l, 0, 4, 3, 1 + 2 * l, "mfm")
            rw_sb = S.sb("rw_sb", [128, KC, 16])
            T.dma('sp', rw_sb[:], router_w[l].rearrange("(k p) e -> p k e", p=128), writes=['rw_sb'])
            xt = [S.sb(f"mx{i}", [128, D]) for i in range(2)]
            hr = [S.sb(f"mhr{i}", [128, XR]) for i in range(2)]
            hT = S.sb("mhT", [128, KC, 128])
            Esb = S.sb("mE", [16, 128]); rec = S.sb("mrec", [16, 128]); aff = S.sb("maff", [16, 128])
            pst = [S.ps(f"mps{i}", [128, 512]) for i in range(4)]
            psl = S.ps("mpsl", [128, 512]); pss = S.ps("mpss", [128, 512]); psa = S.ps("mpsa", [128, 512])
            for i in range(2):
                T.op('pool', lambda: P.memset(hr[i][:, D:XR], 0.0), writes=[f"mhr{i}"])
            for b in range(NBLK):
                t0 = b * 128
                x_ = xt[b % 2]; xn = f"mx{b % 2}"; h_ = hr[b % 2]; hn = f"mhr{b % 2}"
                T.dma('sp', x_[:], xin[t0:t0 + 128, :], reads=[xn], writes=[xn])
                T.dma('sp', acc[t0:t0 + 128, :], x_[:], reads=[xn])
                rms_tok(S, x_, xn, 128, "mfm", geff, shr, h_[:, 0:D], hn)
                for kq in range(4):
                    pp = pst[kq]; pn = f"mps{kq}"
                    for k4 in range(4):
                        k = kq * 4 + k4
                        T.op('pe', lambda: PE.transpose(pp[:, k4 * 128:(k4 + 1) * 128], h_[:, k * 128:(k + 1) * 128], ident), reads=[hn, 'cst'], writes=[pn])
                    if kq % 2 == 0:
                        T.op('act', lambda: A.activation(hT[:, kq * 4:(kq + 1) * 4, :], pp[:].rearrange("p (k n) -> p k n", n=128), AF.Copy), reads=[pn, 'mhT'], writes=['mhT'])
                    else:
                        T.op('dve', lambda: V.tensor_copy(hT[:, kq * 4:(kq + 1) * 4, :], pp[:].rearrange("p (k n) -> p k n", n=128)), reads=[pn, 'mhT'], writes=['mhT'])
                for k in range(KC):
                    T.op('pe', lambda: PE.matmul(psl[0:16, 0:128], rw_sb[:, k, :], hT[:, k, :], start=(k == 0), stop=(k == KC - 1)), reads=['rw_sb', 'mhT'], writes=['mpsl'])
                T.op('act', lambda: A.activation(Esb[:], psl[0:16, 0:128], AF.Exp), reads=['mpsl'], writes=['mE'])
                T.op('pe', lambda: PE.matmul(pss[0:16, 0:128], onesbd[0:16, 0:16], Esb[:], start=True, stop=True), reads=['cst', 'mE'], writes=['mpss'])
                T.op('dve', lambda: V.reciprocal(rec[:], pss[0:16, 0:128]), reads=['mpss'], writes=['mrec'])
                T.op('dve', lambda: V.tensor_tensor(aff[:], Esb[:], rec[:], op=ALU.mult), reads=['mE', 'mrec', 'maff'], writes=['maff'])
                T.dma('sp', affT[:, t0:t0 + 128], aff[:], reads=['maff'])
                T.op('pe', lambda: PE.transpose(psa[:, 0:16], aff[:], ident[0:16, 0:16]), reads=['maff', 'cst'], writes=['mpsa'])
                T.op('act', lambda: A.activation(h_[:, D:D + 16], psa[:, 0:16], AF.Copy), reads=['mpsa', hn], writes=[hn])
                T.op('dve', lambda: V.tensor_scalar(h_[:, D + 16:D + 17], iota_p[:], float(t0), None, ALU.add), reads=['iota_p', hn], writes=[hn])
                T.dma('sp', hrow[t0:t0 + 128, :], h_[:], reads=[hn])
            T.barrier()
            S.close()
            S = Scope(nc, T)
            NJ = SEQ // 8
            af = S.sb("taf", [128, NJ]); cmp_ = S.sb("tcmp", [128, NJ]); onesf = S.sb("tones", [128, NJ]); pre = S.sb("tpre", [128, NJ])
            lo = S.sb("tlo", [128, 1]); hi = S.sb("thi", [128, 1]); mid = S.sb("tmid", [128, 1]); cnt = S.sb("tcnt", [128, 2]); ge = S.sb("tge", [128, 1])
            d1 = S.sb("td1", [128, 1])
            psc = S.ps("tpsc", [128, 512])
            T.dma('sp', af[:], affT.rearrange("e (j n) -> (e j) n", j=8), writes=['taf'])
            T.op('pool', lambda: P.memset(lo[:], 0.0), writes=['tlo'])
            T.op('pool', lambda: P.memset(hi[:], 1.0), writes=['thi'])
            T.op('pool', lambda: P.memset(cnt[:], 0.0), writes=['tcnt'])
            T.op('pool', lambda: P.memset(onesf[:], 1.0), writes=['tones'])
            for it in range(34):
                T.op('dve', lambda: V.tensor_scalar(mid[:], lo[:], hi[:, 0:1], 0.5, ALU.add, ALU.mult), reads=['tlo', 'thi'], writes=['tmid'])
                T.op('dve', lambda: V.tensor_scalar(cmp_[:], af[:], mid[:, 0:1], None, ALU.is_ge, ALU.add, accum_out=cnt[:, 0:1]),
                     reads=['taf', 'tmid', 'tcnt'], writes=['tcmp', 'tcnt'])
                T.op('pe', lambda: PE.matmul(psc[:, 0:2], ones8, cnt[:], start=True, stop=True), reads=['cst2', 'tcnt'], writes=['tpsc'])
                T.op('dve', lambda: V.tensor_scalar(ge[:], psc[:, 0:1], float(CAP) - 0.5, None, ALU.is_ge), reads=['tpsc'], writes=['tge'])
                T.op('dve', lambda: V.tensor_tensor(d1[:], mid[:], lo[:], op=ALU.subtract), reads=['tmid', 'tlo'], writes=['td1'])
                T.op('dve', lambda: V.scalar_tensor_tensor(lo[:], d1[:], ge[:, 0:1], lo[:], ALU.mult, ALU.add), reads=['td1', 'tge', 'tlo'], writes=['tlo'])
                T.op('dve', lambda: V.tensor_tensor(d1[:], hi[:], mid[:], op=ALU.subtract), reads=['tmid', 'thi'], writes=['td1'])
                T.op('dve', lambda: V.scalar_tensor_tensor(hi[:], d1[:], ge[:, 0:1], mid[:], ALU.mult, ALU.add), reads=['td1', 'tge', 'tmid', 'thi'], writes=['thi'])
            T.op('dve', lambda: V.tensor_scalar(cmp_[:], af[:], lo[:, 0:1], None, ALU.is_ge), reads=['taf', 'tlo'], writes=['tcmp'])
            T.op('dve', lambda: V.tensor_tensor_scan(pre[:], onesf[:], cmp_[:], 0.0, ALU.mult, ALU.add), reads=['tones', 'tcmp'], writes=['tpre'])
            T.op('dve', lambda: V.tensor_copy(cnt[:, 0:1], pre[:, NJ - 1:NJ]), reads=['tpre', 'tcnt'], writes=['tcnt'])
            T.op('dve', lambda: V.tensor_copy(cnt[:, 1:2], pre[:, NJ - 1:NJ]), reads=['tpre', 'tcnt'], writes=['tcnt'])
            T.op('pe', lambda: PE.matmul(psc[:, 0:2], low8, cnt[:], start=True, stop=True), reads=['cst2', 'tcnt'], writes=['tpsc'])
            T.op('dve', lambda: V.tensor_copy(d1[:], psc[:, 0:1]), reads=['tpsc'], writes=['td1'])
            T.op('dve', lambda: V.scalar_tensor_tensor(pre[:], pre[:], d1[:, 0:1], cmp_[:], ALU.add, ALU.mult), reads=['tpre', 'td1', 'tcmp'], writes=['tpre'])
            T.op('dve', lambda: V.tensor_scalar(pre[:], pre[:], -1.0, None, ALU.add), reads=['tpre'], writes=['tpre'])
            T.dma('sp', slot_d.rearrange("e (j n) -> (e j) n", j=8), pre[:], reads=['tpre'], writes=['slot_d'])
            stm = S.sb("stm", [128, 16, NBLK]); neg = S.sb("sneg", [128, 16, NBLK]); su = S.sb("su", [128, 16, NBLK], U32)
            colp = S.sb("colp", [128, 1])
            T.op('dve', lambda: V.tensor_scalar(colp[:], iota_p[:], float(CAP + 1), None, ALU.add), reads=['iota_p'], writes=['colp'])
            T.dma('sp', stm[:], slot_d.rearrange("e (p b) -> p e b", b=NBLK), reads=['slot_d'], writes=['stm'])
            T.op('dve', lambda: V.tensor_scalar(neg[:], stm[:], 0.0, None, ALU.is_lt), reads=['stm'], writes=['sneg'])
            T.op('dve', lambda: V.scalar_tensor_tensor(stm[:], neg[:], colp[:, 0:1], stm[:], ALU.mult, ALU.add), reads=['sneg', 'colp', 'stm'], writes=['stm'])
            T.op('dve', lambda: V.tensor_copy(su[:], stm[:]), reads=['stm'], writes=['su'])
            ht = [S.sb(f"dht{i}", [128, XR]) for i in range(2)]
            hrow_v = hrow.rearrange("(p b) c -> p b c", b=NBLK)
            bc_reg = nc.gpsimd.to_reg(CAP - 1)
            for b in range(NBLK):
                h_ = ht[b % 2]; hn = f"dht{b % 2}"
                T.dma('sp', h_[:], hrow_v[:, b, :], reads=[hn], writes=[hn])
                for e in range(16):
                    T.idma(Xg[e], bass.IndirectOffsetOnAxis(ap=su[:, e, b:b + 1], axis=0), h_[:], None, reads=[hn, 'su'], bounds_check=bc_reg, oob_is_err=False)
            T.barrier()
            S.close()
            S = Scope(nc, T)
            SW = min(512, CAP); NSJ = SW // 128
            gtf = S.sb("gtf", [128, D])
            bcast_row(gtf[:], modrow[l, 0:1, 5 * D:6 * D], 'gtf')
            Wg_sb = S.sb("Wg_sb", [128, KC, 1024], BF16); Wu_sb = S.sb("Wu_sb", [128, KC, 1024], BF16); Wd_sb = S.sb("Wd_sb", [128, 8, D], BF16)
            ws = [S.sb(f"ews{i}", [128, 1024]) for i in range(2)]
            xg_t = [S.sb(f"exg{i}", [128, XR]) for i in range(2)]
            XT = S.sb("eXT", [128, KC, SW], BF16); hid = S.sb("ehid", [128, 8, SW], BF16); gs = S.sb("egs", [128, SW], BF16)
            side = S.sb("eside", [128, 4, 17]); tix = S.sb("etix", [128, 4], U32)
            Ysb = [S.sb(f"eY{i}", [128, D]) for i in range(2)]
            pst = [S.ps(f"eps{i}", [128, 512]) for i in range(2)]
            psg = S.ps("epsg", [128, 512]); psu = S.ps("epsu", [128, 512])
            psy = [S.ps(f"epsy{i}", [128, 512]) for i in range(4)]
            wi = 0; xi = 0; yi = 0; ti = 0
            for e in range(16):
                for k in range(KC):
                    for (src, dst, dn) in ((wg, Wg_sb, "Wg"), (wu, Wu_sb, "Wu")):
                        w = ws[wi % 2]; wn = f"ews{wi % 2}"; wi += 1
                        T.dma('sp', w[:], src[l, e, k * 128:(k + 1) * 128, :], reads=[wn], writes=[wn])
                        if wi % 2 == 0:
                            T.op('pool', lambda: P.tensor_copy(dst[:, k, :], w[:]), reads=[wn, dn], writes=[dn])
                        else:
                            T.op('dve', lambda: V.tensor_copy(dst[:, k, :], w[:]), reads=[wn, dn], writes=[dn])
                for f in range(8):
                    for hf in range(2):
                        w = ws[wi % 2]; wn = f"ews{wi % 2}"; wi += 1
                        T.dma('sp', w[:], wd[l, e, f * 128:(f + 1) * 128, hf * 1024:(hf + 1) * 1024], reads=[wn], writes=[wn])
                        if wi % 2 == 0:
                            T.op('pool', lambda: P.tensor_copy(Wd_sb[:, f, hf * 1024:(hf + 1) * 1024], w[:]), reads=[wn, "Wd"], writes=["Wd"])
                        else:
                            T.op('dve', lambda: V.tensor_copy(Wd_sb[:, f, hf * 1024:(hf + 1) * 1024], w[:]), reads=[wn, "Wd"], writes=["Wd"])
                for s0 in range(0, CAP, SW):
                    for j in range(NSJ):
                        xg = xg_t[xi % 2]; xgn = f"exg{xi % 2}"; xi += 1
                        T.dma('sp', xg[:], Xg[e][s0 + j * 128:s0 + (j + 1) * 128, :], reads=[xgn], writes=[xgn])
                        T.op('pool', lambda: P.tensor_copy(side[:, j, :], xg[:, D:D + 17]), reads=[xgn, 'eside'], writes=['eside'])
                        for kq in range(4):
                            pp = pst[ti % 2]; pn = f"eps{ti % 2}"; ti += 1
                            for k4 in range(4):
                                k = kq * 4 + k4
                                T.op('pe', lambda: PE.transpose(pp[:, k4 * 128:(k4 + 1) * 128], xg[:, k * 128:(k + 1) * 128], ident), reads=[xgn, 'cst'], writes=[pn])
                            o_ = XT[:, kq * 4:(kq + 1) * 4, j * 128:(j + 1) * 128]
                            i_ = pp[:].rearrange("p (k n) -> p k n", n=128)
                            if ti % 2 == 0:
                                T.op('act', lambda: A.activation(o_, i_, AF.Copy), reads=[pn, 'eXT'], writes=['eXT'])
                            else:
                                T.op('dve', lambda: V.tensor_copy(o_, i_), reads=[pn, 'eXT'], writes=['eXT'])
                    T.op('dve', lambda: V.tensor_copy(tix[:, 0:NSJ], side[:, 0:NSJ, 16]), reads=['eside', 'etix'], writes=['etix'])
                    for m in range(8):
                        for k in range(KC):
                            T.op('pe', lambda: PE.matmul(psg[:, 0:SW], Wg_sb[:, k, m * 128:(m + 1) * 128], XT[:, k, :], start=(k == 0), stop=(k == KC - 1)),
                                 reads=['Wg', 'eXT'], writes=['epsg'])
                        for k in range(KC):
                            T.op('pe', lambda: PE.matmul(psu[:, 0:SW], Wu_sb[:, k, m * 128:(m + 1) * 128], XT[:, k, :], start=(k == 0), stop=(k == KC - 1)),
                                 reads=['Wu', 'eXT'], writes=['epsu'])
                        T.op('act', lambda: A.activation(gs[:], psg[:, 0:SW], AF.Silu), reads=['epsg', 'egs'], writes=['egs'])
                        T.op('dve', lambda: V.tensor_tensor(hid[:, m, :], gs[:], psu[:, 0:SW], op=ALU.mult), reads=['egs', 'epsu', 'ehid'], writes=['ehid'])
                    for j in range(NSJ):
                        y_ = Ysb[yi % 2]; yn = f"eY{yi % 2}"; yi += 1
                        for ct in range(4):
                            for f in range(8):
                                T.op('pe', lambda: PE.matmul(psy[ct][:], hid[:, f, j * 128:(j + 1) * 128], Wd_sb[:, f, ct * 512:(ct + 1) * 512], start=(f == 0), stop=(f == 7)),
                                     reads=['ehid', 'Wd'], writes=[f"epsy{ct}"])
                            T.op('dve', lambda: V.scalar_tensor_tensor(y_[:, ct * 512:(ct + 1) * 512], psy[ct][:], side[:, j, e:e + 1], gtf[:, ct * 512:(ct + 1) * 512], ALU.mult, ALU.mult),
                                 reads=[f"epsy{ct}", 'eside', 'gtf', yn], writes=[yn])
                        T.idma(acc, bass.IndirectOffsetOnAxis(ap=tix[:, j:j + 1], axis=0), y_[:], None, reads=[yn, 'etix', 'acc'], writes=['acc'], compute_op=ALU.add)
            T.barrier()
            S.close()

        def phase_pool(xin, xout):
            S = Scope(nc, T)
            geff, shr = mod_rows(S, 1, 0, 1, 0, 2, "pm")
            xt = [S.sb(f"px{i}", [128, D]) for i in range(2)]
            hh = S.sb("ph", [128, D]); hTt = S.sb("phT", [128, KC, 512])
            pst = [S.ps(f"pps{i}", [128, 512]) for i in range(4)]
            pi = 0
            for c0 in range(0, SEQ, 512):
                for j in range(4):
                    t0 = c0 + j * 128
                    x_ = xt[j % 2]; xn = f"px{j % 2}"
                    T.dma('sp', x_[:], xin[t0:t0 + 128, :], reads=[xn], writes=[xn])
                    rms_tok(S, x_, xn, 128, "pm", geff, shr, hh, 'ph')
                    for kq in range(4):
                        pp = pst[pi % 4]; pn = f"pps{pi % 4}"; pi += 1
                        for k4 in range(4):
                            k = kq * 4 + k4
                            T.op('pe', lambda: PE.transpose(pp[:, k4 * 128:(k4 + 1) * 128], hh[:, k * 128:(k + 1) * 128], ident), reads=['ph', 'cst'], writes=[pn])
                        o_ = hTt[:, kq * 4:(kq + 1) * 4, j * 128:(j + 1) * 128]
                        i_ = pp[:].rearrange("p (k n) -> p k n", n=128)
                        if pi % 2 == 0:
                            T.op('act', lambda: A.activation(o_, i_, AF.Copy), reads=[pn, 'phT'], writes=['phT'])
                        else:
                            T.op('dve', lambda: V.tensor_copy(o_, i_), reads=[pn, 'phT'], writes=['phT'])
                T.dma('sp', hT1[:, c0:c0 + 512].rearrange("(k p) n -> p k n", p=128), hTt[:], reads=['phT'])
            T.barrier()
            S.close()
            S = Scope(nc, T)
            pw_sb = S.sb("pw_sb", [128, 4, 4, 512], BF16)
            pws = S.sb("pws", [128, 512])
            for cg in range(4):
                for kk in range(4):
                    T.dma('sp', pws[:], pool_w[cg, kk * 128:(kk + 1) * 128, :], reads=['pws'], writes=['pws'])
                    T.op('dve', lambda: V.tensor_copy(pw_sb[:, cg, kk, :], pws[:]), reads=['pws', 'pw_sb'], writes=['pw_sb'])
            sg = S.sb("psg_", [128, D]); sg2 = S.sb("psg2", [128, D])
            bcast_row(sg[:], nrm[5], 'psg_')
            bcast_row(sg2[:], modrow[1, 0:1, 2 * D:3 * D], 'psg2')
            T.op('dve', lambda: V.tensor_tensor(sg[:], sg[:], sg2[:], op=ALU.mult), reads=['psg_', 'psg2'], writes=['psg_'])
            HW_ = 528
            hh2 = S.sb("ph2", [128, KC, HW_]); ic = S.sb("pic", [128, 4, 512])
            sa = S.sb("psa", [128, HW_]); sb_ = S.sb("psb", [128, HW_])
            dT = S.sb("pdT", [128, KC, 512], BF16)
            xt = [S.sb(f"qx{i}", [128, D]) for i in range(2)]
            o1 = [S.sb(f"qo{i}", [128, D]) for i in range(2)]
            ps = [S.ps(f"qps{i}", [128, 512]) for i in range(8)]
            bi = 0
            for c0 in range(0, SEQ, 512):
                lo_ = max(c0 - 8, 0); hi_ = min(c0 + 520, SEQ)
                if c0 == 0:
                    T.op('pool', lambda: P.memset(hh2[:, :, 0:8], 0.0), reads=['ph2'], writes=['ph2'])
                if c0 + 520 > SEQ:
                    T.op('pool', lambda: P.memset(hh2[:, :, 520:528], 0.0), reads=['ph2'], writes=['ph2'])
                T.dma('sp', hh2[:, :, lo_ - (c0 - 8):hi_ - (c0 - 8)], hT1[:, lo_:hi_].rearrange("(k p) n -> p k n", p=128), reads=['ph2'], writes=['ph2'])
                for wi_ in range(4):
                    T.dma('sp', ic[:, wi_, :], invcnt[wi_, :, c0:c0 + 512].partition_broadcast(128), reads=['pic'], writes=['pic'])
                for k in range(KC):
                    cg = k // 4
                    h_ = hh2[:, k, :]
                    T.op('dve', lambda: V.tensor_tensor(sa[:, 1:528], h_[:, 0:527], h_[:, 1:528], op=ALU.add), reads=['ph2', 'psa'], writes=['psa'])
                    cur_, curn, oth, othn = sa, 'psa', sb_, 'psb'
                    if cg >= 1:
                        T.op('pool', lambda: P.tensor_tensor(oth[:, 2:526], cur_[:, 1:525], cur_[:, 3:527], op=ALU.add), reads=[curn, othn], writes=[othn])
                        cur_, curn, oth, othn = oth, othn, cur_, curn
                    if cg >= 2:
                        T.op('dve', lambda: V.tensor_tensor(oth[:, 4:524], cur_[:, 2:522], cur_[:, 6:526], op=ALU.add), reads=[curn, othn], writes=[othn])
                        cur_, curn, oth, othn = oth, othn, cur_, curn
                    if cg >= 3:
                        T.op('pool', lambda: P.tensor_tensor(oth[:, 8:520], cur_[:, 4:516], cur_[:, 12:524], op=ALU.add), reads=[curn, othn], writes=[othn])
                        cur_, curn, oth, othn = oth, othn, cur_, curn
                    T.op('dve', lambda: V.tensor_tensor(oth[:, 8:520], cur_[:, 8:520], ic[:, cg, :], op=ALU.mult), reads=[curn, 'pic', othn], writes=[othn])
                    T.op('pool', lambda: P.tensor_tensor(dT[:, k, :], oth[:, 8:520], h_[:, 8:520], op=ALU.subtract), reads=[othn, 'ph2', 'pdT'], writes=['pdT'])
                for j in range(4):
                    t0 = c0 + j * 128
                    x_ = xt[bi % 2]; xn = f"qx{bi % 2}"; o_ = o1[bi % 2]; on = f"qo{bi % 2}"
                    T.dma('sp', x_[:], xin[t0:t0 + 128, :], reads=[xn], writes=[xn])
                    for cg in range(4):
                        pp = ps[(bi * 4 + cg) % 8]; pn = f"qps{(bi * 4 + cg) % 8}"
                        for kk in range(4):
                            T.op('pe', lambda: PE.matmul(pp[:], dT[:, cg * 4 + kk, j * 128:(j + 1) * 128], pw_sb[:, cg, kk, :], start=(kk == 0), stop=(kk == 3)),
                                 reads=['pdT', 'pw_sb'], writes=[pn])
                        T.op('dve', lambda: V.tensor_tensor(o_[:, cg * 512:(cg + 1) * 512], pp[:], sg[:, cg * 512:(cg + 1) * 512], op=ALU.mult),
                             reads=[pn, 'psg_', on], writes=[on])
                    T.op('pool', lambda: P.tensor_tensor(o_[:], o_[:], x_[:], op=ALU.add), reads=[on, xn], writes=[on])
                    T.dma('sp', xout[t0:t0 + 128, :], o_[:], reads=[on])
                    bi += 1
            T.barrier()
            S.close()

        def phase_final(xin):
            S = Scope(nc, T)
            nf = S.sb("fnf", [128, D])
            bcast_row(nf[:], nrm[4], 'fnf')
            xt = [S.sb(f"fx{i}", [128, D]) for i in range(2)]
            ot = [S.sb(f"fo{i}", [128, D]) for i in range(2)]
            for b in range(NBLK):
                t0 = b * 128
                x_ = xt[b % 2]; xn = f"fx{b % 2}"; o_ = ot[b % 2]; on = f"fo{b % 2}"
                T.dma('sp', x_[:], xin[t0:t0 + 128, :], reads=[xn], writes=[xn])
                rms_tok(S, x_, xn, 128, 'fnf', nf, None, o_, on)
                T.dma('sp', out[t0:t0 + 128, :], o_[:], reads=[on], writes=['out'])
            T.barrier()
            S.close()

        stages = [("mod", phase_mod), ("n0", phase_n0)] + [(f"mix{g}", (lambda g=g: mixer_group(g))) for g in range(4)] + [
            ("b", phase_b), ("moe0", lambda: phase_moe(0, x1, acc0)), ("pool", lambda: phase_pool(acc0, x3)),
            ("moe1", lambda: phase_moe(1, x3, acc1)), ("final", lambda: phase_final(acc1))]
        for name, fn in stages:
            fn()
            if upto == name:
                break
        T.finish()
    return nc


def _pcol(v):
    v = np.asarray(v, np.float32)
    return np.ascontiguousarray(v.reshape(-1, 128).T)


def _partner_perm():
    d = np.arange(64)
    return np.where((d % 32) < 16, d + 16, d - 16)


def _consts():
    c = np.zeros((128, 9, 128), np.float32)
    r = np.arange(128)[:, None]; q = np.arange(128)[None, :]
    s = r % 64; t = q % 64
    c[:, 0, :] = (r == q)
    c[:, 1, :] = ((r // 64) == (q // 64))
    c[:, 2, :] = np.where(q < 64, s < t, s <= t)
    c[:, 3, :] = np.where(q < 64, s <= t, s < t)
    c[:, 4, :] = ((r // 64) == (q // 64))
    n = np.arange(64)[None, :]
    c[:, 5, 0:64] = (s == n)
    c[:, 5, 64:128] = (s == 63 - n)
    c[:, 6, 0] = (np.arange(128) < 64)
    c[:, 6, 1] = (np.arange(128) >= 64)
    c[:, 7, :] = (r >= q)
    c[:, 8, :] = (r <= q)
    return c


def _rope_tables(SEQ, grid_w=64, theta=10000.0):
    rows = SEQ // grid_w
    row = np.repeat(np.arange(rows, dtype=np.float32), grid_w)
    col = np.tile(np.arange(grid_w, dtype=np.float32), rows)
    n_freq = 16
    inv = (np.float32(theta) ** (-np.arange(n_freq, dtype=np.float32) / np.float32(n_freq))).astype(np.float32)
    ar = (row[:, None] * inv).astype(np.float32); ac = (col[:, None] * inv).astype(np.float32)
    cosT = np.zeros((128, SEQ), np.float32); sinT = np.zeros((128, SEQ), np.float32)
    for p in range(128):
        d = p % 64
        ang = ar[:, d % 16] if d < 32 else ac[:, d % 16]
        cosT[p] = np.cos(ang)
        sinT[p] = np.sin(ang) * (-1.0 if (d % 32) < 16 else 1.0)
    return cosT, sinT


def prep_stageA(inp, mod_x, mod_c, b, g, SEQ, CTX, weights_only=False):
    f = np.float32
    m = {}
    if not weights_only:
        x = np.asarray(inp["x"][b], f); ctx = np.asarray(inp["ctx"][b], f)
        m["xT"] = np.ascontiguousarray(x.T); m["xTr"] = np.ascontiguousarray(x[::-1].T)
        m["cT"] = np.ascontiguousarray(ctx.T); m["cTr"] = np.ascontiguousarray(ctx[::-1].T)
        mx = np.asarray(mod_x, f).reshape(6, D); mc = np.asarray(mod_c, f).reshape(6, D)
        m["modv"] = np.ascontiguousarray(np.stack([_pcol(inp["norm_mix"][0]), _pcol(mx[1]), _pcol(mx[0]), _pcol(mc[1]), _pcol(mc[0])], axis=1))
    w = np.asarray(inp["w_in"][0], f)
    hs = slice(256 * g, 256 * g + 256)
    perm = _partner_perm()
    qcols = 3488 + 256 * g + np.arange(256)
    qpcols = 3488 + 256 * g + (np.arange(4)[:, None] * 64 + perm[None, :]).reshape(-1)
    kcols = 4512 + 64 * g + np.arange(64); kpcols = 4512 + 64 * g + perm
    vcols = 4768 + 64 * g + np.arange(64)
    rkv = np.concatenate([np.arange(0, 1024)[hs], np.arange(1024, 2048)[hs], np.arange(2048, 3072)[hs]])
    colsf = np.concatenate([rkv, np.arange(3072, 3136), np.arange(3200, 3264), np.arange(3328, 3488), qcols, qpcols, kcols, kpcols, vcols])
    colsb = np.concatenate([rkv, np.arange(3136, 3200), np.arange(3264, 3328)])
    assert len(colsf) == NF and len(colsb) == NB
    m["Wf"] = np.ascontiguousarray(w[:, colsf]); m["Wb"] = np.ascontiguousarray(w[:, colsb])
    mu = np.asarray(inp["shift_mu"][0], f)
    vec = np.zeros((128, 32), f)
    for p in range(2):
        sl = slice(256 * g + 128 * p, 256 * g + 128 * p + 128)
        for qi in range(3):
            vec[:, 3 * p + qi] = mu[qi * 1024:(qi + 1) * 1024][sl]
        for d in range(2):
            vec[:, 8 + 2 * d + p] = np.asarray(inp["decay_w0"][0][d], f)[sl]
            vec[:, 12 + 2 * d + p] = np.asarray(inp["iclr_a0"][0][d], f)[sl]
        vec[:, 16 + p] = np.asarray(inp["k_k"][0], f)[sl]; vec[:, 18 + p] = np.asarray(inp["k_a"][0], f)[sl]
        vec[:, 20 + p] = np.asarray(inp["r_k"][0], f)[sl]; vec[:, 22 + p] = np.asarray(inp["ln_w"][0], f)[sl]
        vec[:, 24 + p] = np.asarray(inp["ln_b"][0], f)[sl]
    for d in range(2):
        vec[0:64, 6 + d] = mu[3072 + 64 * d:3072 + 64 * d + 64]
        vec[64:128, 6 + d] = mu[3200 + 64 * d:3200 + 64 * d + 64]
    m["vec"] = vec
    w2a2 = np.zeros((128, 2, 2, 128), f)
    for d in range(2):
        for p in range(2):
            sl = slice(256 * g + 128 * p, 256 * g + 128 * p + 128)
            w2a2[0:64, d, p, :] = np.asarray(inp["decay_w2"][0][d], f)[:, sl]
            w2a2[64:128, d, p, :] = np.asarray(inp["iclr_a2"][0][d], f)[:, sl]
    m["w2a2"] = w2a2
    g2 = np.asarray(inp["gate_g2"][0], f)
    m["g2a"] = np.ascontiguousarray(g2[0:128, hs]); m["g2b"] = np.ascontiguousarray(g2[128:160, hs])
    mg = np.zeros((128, 2), f); mg[:, 0] = mu[3328:3456]; mg[0:32, 1] = mu[3456:3488]
    m["mugd"] = mg
    if not weights_only:
        cosT, sinT = _rope_tables(SEQ)
        m["cosT"] = cosT; m["sinT"] = sinT
    m["sinkb"] = np.ascontiguousarray(np.broadcast_to(np.asarray(inp["sink"][0], f)[4 * g:4 * g + 4][None, :], (128, 4)))
    m["cst"] = _consts()
    return m


def _consts2():
    c = np.zeros((128, 4, 128), np.float32)
    r = np.arange(128)[:, None]; q = np.arange(128)[None, :]
    c[:, 0, :] = (r == 127 - q)
    c[:, 1, :] = ((r // 8) == (q // 8))
    c[:, 2, :] = ((r // 8) == (q // 8)) & (r < q)
    return c


def prep_all(inp, b, SEQ, CTX):
    f = np.float32
    m = {}
    m["x"] = np.ascontiguousarray(np.asarray(inp["x"][b], f)); m["ctx"] = np.ascontiguousarray(np.asarray(inp["ctx"][b], f))
    m["cvec"] = np.ascontiguousarray(np.stack([_pcol(inp["c"][b]), _pcol(inp["c_ctx"])], axis=2))
    m["ada_w"] = np.asarray(inp["ada_w"], f); m["ada_b"] = np.asarray(inp["ada_b"], f).reshape(2, 1, 6 * D)
    m["nrm"] = np.stack([np.asarray(inp["norm_mix"][0], f), np.asarray(inp["norm_ffn"][0], f), np.asarray(inp["norm_mix"][1], f),
                         np.asarray(inp["norm_ffn"][1], f), np.asarray(inp["norm_final"], f), np.asarray(inp["pool_scale"][0], f)]).reshape(6, 1, D)
    zero_mod = np.zeros(6 * D, f)
    parts = [prep_stageA(inp, zero_mod, zero_mod, b, g, SEQ, CTX, weights_only=True) for g in range(4)]
    m["Wf_all"] = np.stack([p["Wf"] for p in parts]); m["Wb_all"] = np.stack([p["Wb"] for p in parts])
    m["vec_all"] = np.stack([p["vec"] for p in parts]); m["w2a2_all"] = np.stack([p["w2a2"] for p in parts])
    m["g2a_all"] = np.stack([p["g2a"] for p in parts]); m["g2b_all"] = np.stack([p["g2b"] for p in parts])
    m["mugd"] = parts[0]["mugd"]; m["sinkb_all"] = np.stack([p["sinkb"] for p in parts])
    m["cosT"], m["sinT"] = _rope_tables(SEQ)
    m["cst"] = _consts(); m["cst2"] = _consts2()
    m["w_out"] = np.asarray(inp["w_out"][0], f)
    m["pool_w"] = np.asarray(inp["pool_w"][0], f)
    pos = np.arange(SEQ)
    ic = np.zeros((4, 1, SEQ), f)
    for i, w in enumerate((2, 4, 8, 16)):
        lo = np.clip(pos - w // 2, 0, SEQ); hi = np.clip(pos + w // 2, 0, SEQ)
        ic[i, 0] = 1.0 / (hi - lo).astype(f)
    m["invcnt"] = ic
    m["router_w"] = np.asarray(inp["router_w"], f)
    m["exp_w_gate"] = np.asarray(inp["exp_w_gate"], f); m["exp_w_up"] = np.asarray(inp["exp_w_up"], f); m["exp_w_down"] = np.asarray(inp["exp_w_down"], f)
    return m


def kernel(**inputs):
    SEQ, CTX = 16384, 256
    nc = build_all(SEQ, CTX)
    maps = [prep_all(inputs, b, SEQ, CTX) for b in range(2)]
    res = run_bass_kernel_spmd(nc, maps, core_ids=[0, 1])
    return np.stack([np.asarray(res.results[b]["out"], np.float32) for b in range(2)])
```

```python
import numpy as np
from contextlib import ExitStack
import concourse.bass as bass
import concourse.mybir as mybir
from concourse.bass_utils import run_bass_kernel_spmd

F32 = mybir.dt.float32
BF16 = mybir.dt.bfloat16
I32 = mybir.dt.int32
U32 = mybir.dt.uint32
AF = mybir.ActivationFunctionType
ALU = mybir.AluOpType
AX = mybir.AxisListType

D = 2048
KC = D // 128
NORM_EPS = 1e-6
GN_EPS = 64e-5
C0 = float(np.exp(-0.5))
NF = 1760
NB = 896
RSTOP = None
RVAR = 0
SKIPP = False
NSTREAM = 2


class Tracker:
    EPOCH = 30000
    NDMA = 24

    def __init__(self, nc, stack):
        self.nc = nc
        self.stack = stack
        self.engines = {'pe': nc.tensor, 'act': nc.scalar, 'dve': nc.vector,
                        'pool': nc.gpsimd, 'sp': nc.sync}
        self.cur = {}
        self.nsem = 0
        self.seen = {e: {} for e in self.engines}
        self.regs = {}
        self.dsems = []
        self.dnext = 0
        self.n_inst = 0
        self.rr = 0

    def _newsem(self, tag):
        self.nsem += 1
        return self.stack.enter_context(self.nc.semaphore(f"s{self.nsem}_{tag}"))

    def sb(self, name, shape, dtype):
        return self.stack.enter_context(self.nc.sbuf_tensor(name, shape, dtype))

    def ps(self, name, shape, dtype):
        return self.stack.enter_context(self.nc.psum_tensor(name, shape, dtype))

    def _tick(self, e):
        c = self.cur.get(e)
        if c is None or c[2] >= self.EPOCH:
            ep = 0 if c is None else c[3] + 1
            c = [(e, ep), self._newsem(f"{e}{ep}"), 0, ep]
            self.cur[e] = c
        c[2] += 1
        return (c[0], c[1], c[2])

    def _wait(self, e, dep):
        key, sem, cnt = dep
        if e == 'pe' and key[0] == 'pe':
            return
        if self.seen[e].get(key, 0) >= cnt:
            return
        self.engines[e].wait_ge(sem, cnt)
        self.seen[e][key] = cnt

    def _deps(self, e, reads, writes):
        for r in reads:
            info = self.regs.get(r)
            if info and info['w']:
                self._wait(e, info['w'])
            if info and r.startswith('ps'):
                for rd in info['r']:
                    if rd[0][0] != e:
                        self._wait(e, rd)
        for w in writes:
            info = self.regs.get(w)
            if info:
                if info['w']:
                    self._wait(e, info['w'])
                for rd in info['r']:
                    self._wait(e, rd)

    def _record(self, tok, reads, writes):
        for r in reads:
            info = self.regs.setdefault(r, {'w': None, 'r': []})
            info['r'] = [x for x in info['r'] if x[0] != tok[0]] + [tok]
        for w in writes:
            self.regs[w] = {'w': tok, 'r': []}

    def op(self, e, fn, reads=(), writes=()):
        self._deps(e, reads, writes)
        tok = self._tick(e)
        inst = fn()
        inst.then_inc(tok[1], 1)
        self._record(tok, reads, writes)
        self.n_inst += 1
        return inst

    def dma(self, e, out, in_, reads=(), writes=(), **kw):
        self._deps(e, reads, writes)
        if len(self.dsems) < self.NDMA:
            self.dsems.append([('d', len(self.dsems)), self._newsem(f"d{len(self.dsems)}"), 0])
            d = self.dsems[-1]
        else:
            d = self.dsems[self.dnext % self.NDMA]
        self.dnext += 1
        if d[2] > 0:
            self._wait(e, (d[0], d[1], d[2]))
        d[2] += 16
        tok = (d[0], d[1], d[2])
        inst = self.engines[e].dma_start(out=out, in_=in_, **kw)
        inst.then_inc(d[1], 16)
        self._record(tok, reads, writes)
        self.n_inst += 1
        return inst

    def idma(self, out, out_offset, in_, in_offset, reads=(), writes=(), **kw):
        e = 'pool'
        self._deps(e, reads, writes)
        if not hasattr(self, 'isems'):
            self.isems = []
            self.inext = 0
        if len(self.isems) < 8:
            self.isems.append([('i', len(self.isems)), self._newsem(f"i{len(self.isems)}"), 0])
            d = self.isems[-1]
        else:
            d = self.isems[self.inext % 8]
        self.inext += 1
        if d[2] > 0:
            self._wait(e, (d[0], d[1], d[2]))
        d[2] += 16
        tok = (d[0], d[1], d[2])
        inst = self.nc.gpsimd.indirect_dma_start(out=out, out_offset=out_offset, in_=in_, in_offset=in_offset, **kw)
        inst.then_inc(d[1], 16)
        self._record(tok, reads, writes)
        self.n_inst += 1
        return inst

    def finish(self):
        e = 'sp'
        for d in self.dsems + getattr(self, 'isems', []):
            if d[2] > 0:
                self._wait(e, (d[0], d[1], d[2]))
        for k, c in self.cur.items():
            if k != e:
                self._wait(e, (c[0], c[1], c[2]))


class Scope:
    UID = 0
    def __init__(self, nc, T):
        self.nc = nc
        self.T = T
        self.st = ExitStack()
        self.n = 0

    def sb(self, name, shape, dtype=F32):
        self.n += 1
        Scope.UID += 1
        return self.st.enter_context(self.nc.sbuf_tensor(f"{name}__{Scope.UID}", shape, dtype))

    def ps(self, name, shape, dtype=F32):
        Scope.UID += 1
        return self.st.enter_context(self.nc.psum_tensor(f"{name}__{Scope.UID}", shape, dtype))

    def sb_once(self, name, shape, dtype=F32):
        if not hasattr(self, "_once"):
            self._once = {}
        if name not in self._once:
            self._once[name] = self.sb(name, shape, dtype)
        return self._once[name]

    def close(self):
        self.st.close()


def _barrier(T):
    toks = [(c[0], c[1], c[2]) for c in T.cur.values()]
    dtoks = [(d[0], d[1], d[2]) for d in T.dsems + getattr(T, 'isems', []) if d[2] > 0]
    for e in T.engines:
        for t in toks + dtoks:
            if t[0][0] != e:
                T._wait(e, t)
            elif e != 'pe':
                T._wait(e, t)
    T.regs = {}


Tracker.barrier = _barrier


def build_all(SEQ, CTX, dbg=False, upto=None):
    nc = bass.Bass("TRN2", target_bir_lowering=False)
    TOT = CTX + SEQ
    CAP = 2 * SEQ // 16
    NBLK = SEQ // 128
    XR = 2080
    assert CAP % 128 == 0 and SEQ % 512 == 0 and CTX % 256 == 0

    def din(name, shape, dt=F32):
        return nc.dram_tensor(name, shape, dt, kind="ExternalInput").ap()

    def scr(name, shape, dt=F32):
        return nc.dram_tensor(name, shape, dt, kind=("ExternalOutput" if dbg else "Internal")).ap()

    x_in = din("x", [SEQ, D]); c_in = din("ctx", [CTX, D])
    cvec = din("cvec", [128, KC, 2])
    ada_w = din("ada_w", [2, D, 6 * D]); ada_b = din("ada_b", [2, 1, 6 * D])
    nrm = din("nrm", [6, 1, D])
    Wf_all = din("Wf_all", [4, D, NF]); Wb_all = din("Wb_all", [4, D, NB])
    vec_all = din("vec_all", [4, 128, 32]); w2a2_all = din("w2a2_all", [4, 128, 2, 2, 128])
    g2a_all = din("g2a_all", [4, 128, 256]); g2b_all = din("g2b_all", [4, 32, 256])
    mugd = din("mugd", [128, 2]); sinkb_all = din("sinkb_all", [4, 128, 4])
    cosT = din("cosT", [128, SEQ]); sinT = din("sinT", [128, SEQ])
    cst = din("cst", [128, 9, 128]); cst2 = din("cst2", [128, 4, 128])
    w_out = din("w_out", [D, D])
    pool_w = din("pool_w", [4, 512, 512]); invcnt = din("invcnt", [4, 1, SEQ])
    router_w = din("router_w", [2, D, 16])
    wg = din("exp_w_gate", [2, 16, D, 1024]); wu = din("exp_w_up", [2, 16, D, 1024]); wd = din("exp_w_down", [2, 16, 1024, D])
    out = nc.dram_tensor("out", [SEQ, D], F32, kind="ExternalOutput").ap()

    modrow = scr("modrow", [2, 2, 6 * D])
    hxf = scr("hxf", [D, TOT], BF16); hxr = scr("hxr", [D, TOT], BF16)
    pxf = scr("pxf", [NF, TOT]); pxb = scr("pxb", [NB, TOT])
    yT = scr("yT", [2, 256, SEQ]); bT = scr("bT", [2, 256, SEQ])
    mixs = scr("mixs", [D, SEQ])
    x1 = scr("x1", [SEQ, D]); acc0 = scr("acc0", [SEQ, D]); x3 = scr("x3", [SEQ, D]); acc1 = scr("acc1", [SEQ, D])
    hrow = scr("hrow", [SEQ, XR]); affT = scr("affT", [16, SEQ]); slot_d = scr("slot_d", [16, SEQ])
    Xg = [scr(f"Xg{e}", [CAP + 128, XR]) for e in range(16)]
    hT1 = scr("hT1", [D, SEQ])

    with ExitStack() as st:
        T = Tracker(nc, st)
        G = Scope(nc, T)
        V = nc.vector; A = nc.scalar; P = nc.gpsimd; PE = nc.tensor

        cs = G.sb("cst_sb", [128, 9, 128])
        T.dma('sp', cs[:], cst, writes=['cst'])
        ident = cs[:, 0, :]; onesbd = cs[:, 1, :]; maskA = cs[:, 2, :]; maskB = cs[:, 3, :]
        bdm = cs[:, 4, :]
        JJ = [cs[:, 5, 0:64], cs[:, 5, 64:128]]
        headsel = cs[:, 6, 0:2]
        triLO = cs[:, 7, :]; triHI = cs[:, 8, :]
        cs2 = G.sb("cst2_sb", [128, 4, 128])
        T.dma('sp', cs2[:], cst2, writes=['cst2'])
        J128 = cs2[:, 0, :]; ones8 = cs2[:, 1, :]; low8 = cs2[:, 2, :]
        onesbf = G.sb("onesbf", [128, 128], BF16)
        T.op('pool', lambda: P.memset(onesbf[:], 1.0), writes=['onesbf'])
        ones64 = G.sb("ones64", [128, 128])
        T.op('dve', lambda: V.tensor_scalar(ones64[:], onesbd, 1.0 / 64, None, ALU.mult), reads=['cst'], writes=['ones64'])
        iota_p = G.sb("iota_p", [128, 1])
        T.op('pool', lambda: P.iota(iota_p[:], pattern=[[0, 1]], base=0, channel_multiplier=1, allow_small_or_imprecise_dtypes=True), writes=['iota_p'])
        vec_sb = G.sb("vec_sb", [128, 32])
        omu = G.sb("omu", [128, 10]); hmu = G.sb("hmu", [128, 10])
        mugd_sb = G.sb("mugd_sb", [128, 2])
        T.dma('sp', mugd_sb[:], mugd, writes=['mugd'])

        _gbf = {}

        def S_glob_bf(name, src_ap):
            if name not in _gbf:
                t_ = G.sb(name, [128, src_ap.shape[-1]], BF16)
                T.op('dve', lambda: V.tensor_copy(t_[:], src_ap), reads=['cst'], writes=[name if not name.startswith('JJb') else 'JJb'])
                _gbf[name] = t_
            return _gbf[name][:]

        class Pre:
            def __init__(self, items):
                self.items = items
                self.issued = -1

            def get(self, i):
                while self.issued < min(i + 1, len(self.items) - 1):
                    self.issued += 1
                    dst, name, src = self.items[self.issued]
                    T.dma('sp', dst, src, reads=[name], writes=[name])

        def bcast_row(tile_ap, row_ap, name):
            T.dma('sp', tile_ap, row_ap.partition_broadcast(128), reads=[name], writes=[name])

        def rms_tok(S, xin, xn, w_, gname, geff, shrow, out_t, on, eps=NORM_EPS):
            ss = S.sb_once("rms_ss", [128, 1]); sq = S.sb_once("rms_sq", [128, D])
            T.op('act', lambda: A.activation(sq[:], xin[:], AF.Square, accum_out=ss[:]), reads=[xn], writes=['rms_sq', 'rms_ss'])
            T.op('act', lambda: A.activation(ss[:], ss[:], AF.Sqrt, bias=eps, scale=1.0 / D), reads=['rms_ss'], writes=['rms_ss'])
            T.op('dve', lambda: V.reciprocal(ss[:], ss[:]), reads=['rms_ss'], writes=['rms_ss'])
            if shrow is None:
                T.op('dve', lambda: V.scalar_tensor_tensor(out_t[:], xin[:], ss[:, 0:1], geff[:], ALU.mult, ALU.mult), reads=[xn, 'rms_ss', gname], writes=[on])
            else:
                T.op('dve', lambda: V.scalar_tensor_tensor(sq[:], xin[:], ss[:, 0:1], geff[:], ALU.mult, ALU.mult), reads=[xn, 'rms_ss', gname, 'rms_sq'], writes=['rms_sq'])
                T.op('pool', lambda: P.tensor_tensor(out_t[:], sq[:], shrow[:], op=ALU.add), reads=['rms_sq', gname], writes=[on])

        def mod_rows(S, l, who, idx_sc, idx_sh, nrm_i, gname):
            geff = S.sb(gname + "_g", [128, D]); shr = S.sb(gname + "_s", [128, D]); nw = S.sb(gname + "_n", [128, D])
            bcast_row(geff[:], modrow[l, who:who + 1, idx_sc * D:(idx_sc + 1) * D], gname)
            bcast_row(shr[:], modrow[l, who:who + 1, idx_sh * D:(idx_sh + 1) * D], gname)
            bcast_row(nw[:], nrm[nrm_i], gname)
            T.op('dve', lambda: V.scalar_tensor_tensor(geff[:], geff[:], 1.0, nw[:], ALU.add, ALU.mult), reads=[gname], writes=[gname])
            return geff, shr

        def phase_mod():
            S = Scope(nc, T)
            cv = S.sb("cv", [128, KC, 2])
            T.dma('sp', cv[:], cvec, writes=['cv'])
            T.op('act', lambda: A.activation(cv[:], cv[:], AF.Silu), reads=['cv'], writes=['cv'])
            wt = [S.sb(f"adaw{i}", [128, 2048]) for i in range(3)]
            psm = [S.ps(f"psm{i}", [128, 512]) for i in range(4)]
            brow = S.sb("brow", [2, 2048]); mrow = S.sb("mrow", [2, 2048])
            wi = 0
            for l in range(2):
                for cg in range(6):
                    for k in range(KC):
                        w = wt[wi % 3]; wn = f"adaw{wi % 3}"; wi += 1
                        T.dma('sp', w[:], ada_w[l, k * 128:(k + 1) * 128, cg * 2048:(cg + 1) * 2048], reads=[wn], writes=[wn])
                        for j in range(4):
                            T.op('pe', lambda: PE.matmul(psm[j][0:2, :], cv[:, k, :], w[:, j * 512:(j + 1) * 512], start=(k == 0), stop=(k == KC - 1)),
                                 reads=['cv', wn], writes=[f"psm{j}"])
                    for r in range(2):
                        T.dma('sp', brow[r:r + 1, :], ada_b[l, :, cg * 2048:(cg + 1) * 2048], reads=['brow'], writes=['brow'])
                    for j in range(4):
                        T.op('dve', lambda: V.tensor_tensor(mrow[:, j * 512:(j + 1) * 512], psm[j][0:2, :], brow[:, j * 512:(j + 1) * 512], op=ALU.add),
                             reads=[f"psm{j}", 'brow', 'mrow'], writes=['mrow'])
                    T.dma('sp', modrow[l, :, cg * 2048:(cg + 1) * 2048], mrow[:], reads=['mrow'])
            T.barrier()
            S.close()

        def phase_n0():
            S = Scope(nc, T)
            xin = [S.sb(f"n0x{i}", [128, D]) for i in range(2)]
            hh = S.sb("n0h", [128, D])
            hTf = S.sb("hTf", [128, KC, 512], BF16); hTr = S.sb("hTr", [128, KC, 512], BF16)
            pst = [S.ps(f"n0ps{i}", [128, 512]) for i in range(4)]
            pi = 0; xi = 0
            for (src, who, L, base) in ((c_in, 1, CTX, 0), (x_in, 0, SEQ, CTX)):
                geff, shr = mod_rows(S, 0, who, 1, 0, 0, f"n0m{who}")
                for c0 in range(0, L, 512):
                    wd_ = min(512, L - c0)
                    nb = wd_ // 128
                    for j in range(nb):
                        xt = xin[xi % 2]; xn = f"n0x{xi % 2}"; xi += 1
                        T.dma('sp', xt[:], src[c0 + j * 128:c0 + (j + 1) * 128, :], reads=[xn], writes=[xn])
                        rms_tok(S, xt, xn, 128, f"n0m{who}", geff, shr, hh, 'n0h')
                        for kq in range(4):
                            for (dst, dn, idm, jj) in ((hTf, 'hTf', ident, j), (hTr, 'hTr', J128, nb - 1 - j)):
                                pp = pst[pi % 4]; pn = f"n0ps{pi % 4}"; pi += 1
                                for k4 in range(4):
                                    k = kq * 4 + k4
                                    T.op('pe', lambda: PE.matmul(pp[:, k4 * 128:(k4 + 1) * 128], hh[:, k * 128:(k + 1) * 128], idm, start=True, stop=True),
                                         reads=['n0h', 'cst', 'cst2'], writes=[pn])
                                eng = 'act' if pi % 2 == 0 else 'dve'
                                o_ = dst[:, kq * 4:(kq + 1) * 4, jj * 128:(jj + 1) * 128]
                                i_ = pp[:].rearrange("p (k n) -> p k n", n=128)
                                if eng == 'act':
                                    T.op('act', lambda: A.activation(o_, i_, AF.Copy), reads=[pn, dn], writes=[dn])
                                else:
                                    T.op('dve', lambda: V.tensor_copy(o_, i_), reads=[pn, dn], writes=[dn])
                    T.dma('sp', hxf[:, base + c0:base + c0 + wd_].rearrange("(k p) n -> p k n", p=128), hTf[:, :, 0:wd_], reads=['hTf'])
                    r0 = base + (L - c0 - wd_)
                    T.dma('sp', hxr[:, r0:r0 + wd_].rearrange("(k p) n -> p k n", p=128), hTr[:, :, 0:wd_], reads=['hTr'])
            T.barrier()
            S.close()

        def mixer_group(g):
            w2a2 = w2a2_all[g]; g2a = g2a_all[g]; g2b = g2b_all[g]; sinkb = sinkb_all[g]
            mixT_rw = mixs[256 * g:256 * g + 256, :]
            mixT_att = mixs[1024 + 256 * g:1024 + 256 * g + 256, :]
            T.dma('sp', vec_sb[:], vec_all[g], reads=['vec'], writes=['vec'])
            T.op('dve', lambda: V.tensor_scalar(omu[:, 0:8], vec_sb[:, 0:8], -1.0, 1.0, ALU.mult, ALU.add), reads=['vec', 'omu'], writes=['omu'])
            T.op('dve', lambda: V.tensor_scalar(hmu[:, 0:8], vec_sb[:, 0:8], 0.5, None, ALU.mult), reads=['vec', 'hmu'], writes=['hmu'])
            T.op('dve', lambda: V.tensor_scalar(omu[:, 8:10], mugd_sb[:], -1.0, 1.0, ALU.mult, ALU.add), reads=['mugd', 'omu'], writes=['omu'])
            T.op('dve', lambda: V.tensor_scalar(hmu[:, 8:10], mugd_sb[:], 0.5, None, ALU.mult), reads=['mugd', 'hmu'], writes=['hmu'])
            S = Scope(nc, T)
            Wf_sb = S.sb("Wf_sb", [128, KC, NF], BF16)
            Wb_sb = S.sb("Wb_sb", [128, KC, NB], BF16)
            wst = [S.sb(f"wst{i}", [128, NF]) for i in range(2)]
            ci = 0
            for (Wd, Wsb, ncol) in ((Wf_all[g], Wf_sb, NF), (Wb_all[g], Wb_sb, NB)):
                for k in range(KC):
                    w = wst[ci % 2]; wn = f"wst{ci % 2}"
                    T.dma('sp', w[:, 0:ncol], Wd[k * 128:(k + 1) * 128, :], reads=[wn], writes=[wn])
                    eng = ('dve', 'pool')[ci % 2]
                    E = V if eng == 'dve' else P
                    T.op(eng, lambda: E.tensor_copy(Wsb[:, k, :], w[:, 0:ncol]), reads=[wn], writes=[f"W{ncol}_{k}"])
                    ci += 1
            PW = 512
            hx = [S.sb(f"hx{i}", [128, KC, PW], BF16) for i in range(2)]
            ost = [S.sb(f"ost{i}", [128, PW]) for i in range(3)]
            psP = [S.ps(f"psP{i}", [128, PW]) for i in range(4)]
            oi = 0; hi = 0
            for (src, Wsb, ncol, pxo) in ((hxf, Wf_sb, NF, pxf), (hxr, Wb_sb, NB, pxb)):
                for c0 in range(0, TOT, PW):
                    w_ = min(PW, TOT - c0)
                    h_ = hx[hi % 2]; hn = f"hx{hi % 2}"; hi += 1
                    T.dma('sp', h_[:, :, 0:w_], src[:, c0:c0 + w_].rearrange("(k p) n -> p k n", p=128), reads=[hn], writes=[hn])
                    for m0 in range(0, ncol, 128):
                        mw = min(128, ncol - m0)
                        pp = psP[oi % 4]; pn = f"psP{oi % 4}"; oo = ost[oi % 3]; on = f"ost{oi % 3}"
                        for k in range(KC):
                            T.op('pe', lambda: PE.matmul(pp[0:mw, 0:w_], Wsb[:, k, m0:m0 + mw], h_[:, k, 0:w_], start=(k == 0), stop=(k == KC - 1)),
                                 reads=[f"W{ncol}_{k}", hn], writes=[pn])
                        if oi % 2 == 0:
                            T.op('act', lambda: A.activation(oo[0:mw, 0:w_], pp[0:mw, 0:w_], AF.Copy), reads=[pn, on], writes=[on])
                        else:
                            T.op('dve', lambda: V.tensor_copy(oo[0:mw, 0:w_], pp[0:mw, 0:w_]), reads=[pn, on], writes=[on])
                        T.dma('sp', pxo[m0:m0 + mw, c0:c0 + w_], oo[0:mw, 0:w_], reads=[on])
                        oi += 1
            T.barrier()
            S.close()
            identb = S_glob_bf("identb", ident)
            JJb = [S_glob_bf("JJb0", JJ[0]), S_glob_bf("JJb1", JJ[1])]
            headselb = S_glob_bf("headselb", headsel)
            TW = 256
            NCH = TW // 64
            tiles = []
            for (s0, L) in ((0, CTX), (CTX, SEQ)):
                for c0 in range(s0, s0 + L, TW):
                    assert c0 + TW <= s0 + L
                    tiles.append((s0, s0 + L, c0, s0 == CTX))

            S = Scope(nc, T)
            w2a2_f = S.sb("w2a2_f", [128, 2, 2, 128])
            T.dma('sp', w2a2_f[:], w2a2, writes=['w2a2f'])
            w2a2_sb = S.sb("w2a2_sb", [128, 2, 2, 128], BF16)
            T.op('dve', lambda: V.tensor_copy(w2a2_sb[:], w2a2_f[:]), reads=['w2a2f'], writes=['w2a2'])
            rmask = S.sb("rmask", [128, TW])
            T.op('pool', lambda: P.memset(rmask[:], 1.0), writes=['rmask'])
            T.op('pool', lambda: P.memset(rmask[:, 0:TW:64], 0.0), reads=['rmask'], writes=['rmask'])

            class Strm:
                pass

            def mk_stream(si):
                s = Strm()
                s.si = si
                n = lambda x: f"{x}_{si}"
                s.n = n
                for nm in ("rl", "kl", "vl", "ll", "sg", "aa", "t0", "t1", "kkn", "kmod", "bb", "cs_", "epos", "eneg", "eprev", "rk"):
                    setattr(s, nm, S.sb(n(nm), [128, TW]))
                s.tl = S.sb(n("tl"), [128, TW], BF16); s.llb = S.sb(n("llb"), [128, TW], BF16)
                s.raw = S.sb(n("raw"), [128, 4, TW + 2])
                s.AR = S.sb(n("AR"), [128, NCH, 128], BF16); s.BK = S.sb(n("BK"), [128, NCH, 128], BF16)
                s.V2 = S.sb(n("V2"), [128, NCH, 128], BF16); s.RK2 = S.sb(n("RK2"), [128, NCH, 128], BF16)
                s.Yout = S.sb(n("Yout"), [128, 2, TW])
                s.GB = S.sb(n("GB"), [128, 128], BF16); s.GK = S.sb(n("GK"), [128, 128], BF16)
                s.PT = [S.sb(n(f"PT{i}"), [128, 256], BF16) for i in range(2)]
                s.PkT = [S.sb(n(f"PkT{i}"), [128, 128], BF16) for i in range(2)]
                s.Tbd = S.sb(n("Tbd"), [128, 128], BF16); s.BKT = S.sb(n("BKT"), [128, 128], BF16)
                s.VV = S.sb(n("VV"), [128, 128], BF16); s.UU = S.sb(n("UU"), [128, 128], BF16)
                s.WY = S.sb(n("WY"), [128, 128], BF16); s.Ysb = S.sb(n("Ysb"), [128, 128], BF16); s.Bsb = S.sb(n("Bsb"), [128, 128], BF16)
                s.Sbdb = [S.sb(n(f"Sbdb{i}"), [128, 128], BF16) for i in range(2)]
                s.csb = S.sb(n("csb"), [128, 2])
                s.Sbd = [S.sb(n(f"Sbd{i}"), [128, 128]) for i in range(2)]
                s.psa = S.ps(n("psa"), [128, 512])
                s.psb = S.ps(n("psb"), [128, 512])
                for t_, nm in ((s.VV, "VV"), (s.UU, "UU"), (s.Ysb, "Ysb"), (s.Bsb, "Bsb"), (s.Sbd[0], "Sbd0"), (s.Sbd[1], "Sbd1"), (s.Sbdb[0], "Sbdb0"), (s.Sbdb[1], "Sbdb1")):
                    T.op('pool', lambda: P.memset(t_[:], 0.0), writes=[n(nm)])
                return s

            def rwkv_stream(s, d, p):
                n = s.n
                px = pxf if d == 0 else pxb
                rows = [p * 128, 256 + p * 128, 512 + p * 128, 768]
                mucol = [3 * p + 0, 3 * p + 1, 3 * p + 2, 6 + d]
                dst = [s.rl, s.kl, s.vl, s.ll]
                dstn = [n("rl"), n("kl"), n("vl"), n("ll")]
                w0c = vec_sb[:, 8 + 2 * d + p: 9 + 2 * d + p]
                a0c = vec_sb[:, 12 + 2 * d + p: 13 + 2 * d + p]
                kkc = vec_sb[:, 16 + p:17 + p]; kac = vec_sb[:, 18 + p:19 + p]; rkc = vec_sb[:, 20 + p:21 + p]
                cur = 0
                for i in range(2):
                    T.op('pool', lambda: P.memset(s.Sbd[i][:], 0.0), reads=[n(f"Sbd{i}")], writes=[n(f"Sbd{i}")])
                    T.op('pool', lambda: P.memset(s.Sbdb[i][:], 0.0), reads=[n(f"Sbdb{i}")], writes=[n(f"Sbdb{i}")])
                for (s0, s1, c0, isx) in tiles:
                    for qi in range(4):
                        lo_ = max(c0 - 1, s0); hi_ = min(c0 + TW + 1, s1)
                        if c0 - 1 < s0:
                            T.op('pool', lambda: P.memset(s.raw[:, qi, 0:1], 0.0), reads=[n(f"raw{qi}")], writes=[n(f"raw{qi}")])
                        if c0 + TW + 1 > s1:
                            T.op('pool', lambda: P.memset(s.raw[:, qi, TW + 1:TW + 2], 0.0), reads=[n(f"raw{qi}")], writes=[n(f"raw{qi}")])
                        T.dma('sp', s.raw[:, qi, lo_ - (c0 - 1): hi_ - (c0 - 1)], px[rows[qi]:rows[qi] + 128, lo_:hi_],
                              reads=[n(f"raw{qi}")], writes=[n(f"raw{qi}")])
                    for qi in range(4):
                        mc = mucol[qi]
                        T.op('dve', lambda: V.tensor_tensor(s.t0[:], s.raw[:, qi, 0:TW], s.raw[:, qi, 2:TW + 2], op=ALU.add),
                             reads=[n(f"raw{qi}")], writes=[n("t0")])
                        T.op('act', lambda: A.activation(s.t1[:], s.raw[:, qi, 1:TW + 1], AF.Copy, scale=omu[:, mc:mc + 1]),
                             reads=[n(f"raw{qi}"), 'omu'], writes=[n("t1")])
                        T.op('dve', lambda: V.scalar_tensor_tensor(dst[qi][:], s.t0[:], hmu[:, mc:mc + 1], s.t1[:], ALU.mult, ALU.add),
                             reads=[n("t0"), n("t1"), 'hmu'], writes=[dstn[qi]])
                    T.op('act', lambda: A.activation(s.tl[0:64, :], s.ll[0:64, :], AF.Tanh), reads=[n("ll")], writes=[n("tl")])
                    T.op('pe', lambda: PE.matmul(s.psa[:, 0:TW], w2a2_sb[0:64, d, p, :], s.tl[0:64, :], start=True, stop=True),
                         reads=['w2a2', n("tl")], writes=[n("psa")])
                    T.op('act', lambda: A.activation(s.sg[:], s.psa[:, 0:TW], AF.Sigmoid, bias=w0c), reads=[n("psa"), 'vec'], writes=[n("sg")])
                    T.op('pool', lambda: P.tensor_copy(s.llb[64:128, :], s.ll[64:128, :]), reads=[n("ll")], writes=[n("llb")])
                    T.op('pe', lambda: PE.matmul(s.psb[:, 0:TW], w2a2_sb[64:128, d, p, :], s.llb[64:128, :], start=True, stop=True),
                         reads=['w2a2', n("llb")], writes=[n("psb")])
                    T.op('act', lambda: A.activation(s.aa[:], s.psb[:, 0:TW], AF.Sigmoid, bias=a0c), reads=[n("psb"), 'vec'], writes=[n("aa")])
                    yield
                    T.op('act', lambda: A.activation(s.t0[:], s.kl[:], AF.Square, scale=kkc), reads=[n("kl"), 'vec'], writes=[n("t0")])
                    T.op('pe', lambda: PE.matmul(s.psa[:, 0:TW], onesbd, s.t0[:], start=True, stop=True), reads=['cst', n("t0")], writes=[n("psa")])
                    T.op('dve', lambda: V.tensor_scalar(s.t1[:], s.psa[:, 0:TW], 1e-24, None, ALU.max), reads=[n("psa")], writes=[n("t1")])
                    T.op('act', lambda: A.activation(s.t1[:], s.t1[:], AF.Sqrt), reads=[n("t1")], writes=[n("t1")])
                    T.op('dve', lambda: V.reciprocal(s.t1[:], s.t1[:]), reads=[n("t1")], writes=[n("t1")])
                    T.op('dve', lambda: V.scalar_tensor_tensor(s.kkn[:], s.kl[:], kkc, s.t1[:], ALU.mult, ALU.mult),
                         reads=[n("kl"), n("t1"), 'vec'], writes=[n("kkn")])
                    T.op('dve', lambda: V.tensor_scalar(s.t0[:], s.aa[:], -1.0, kac, ALU.add, ALU.mult), reads=[n("aa"), 'vec'], writes=[n("t0")])
                    T.op('dve', lambda: V.scalar_tensor_tensor(s.kmod[:], s.t0[:], 1.0, s.kl[:], ALU.add, ALU.mult),
                         reads=[n("t0"), n("kl")], writes=[n("kmod")])
                    T.op('pool', lambda: P.tensor_tensor(s.bb[:], s.kkn[:], s.aa[:], op=ALU.mult), reads=[n("kkn"), n("aa")], writes=[n("bb")])
                    T.op('dve', lambda: V.tensor_tensor_scan(s.cs_[:], rmask[:], s.sg[:], 0.0, ALU.mult, ALU.add),
                         reads=['rmask', n("sg")], writes=[n("cs_")])
                    T.op('pool', lambda: P.tensor_tensor(s.t1[:], s.cs_[:], s.sg[:], op=ALU.subtract), reads=[n("cs_"), n("sg")], writes=[n("t1")])
                    T.op('act', lambda: A.activation(s.epos[:], s.cs_[:], AF.Exp, scale=-C0), reads=[n("cs_")], writes=[n("epos")])
                    T.op('act', lambda: A.activation(s.eneg[:], s.cs_[:], AF.Exp, scale=C0), reads=[n("cs_")], writes=[n("eneg")])
                    T.op('act', lambda: A.activation(s.eprev[:], s.t1[:], AF.Exp, scale=-C0), reads=[n("t1")], writes=[n("eprev")])
                    c3 = lambda t_, h: t_[64 * h:64 * h + 64, :].rearrange("p (c t) -> p c t", t=64)
                    for h in range(2):
                        lo = 64 * h; ot = 64 * (1 - h)
                        T.op('dve', lambda: V.scalar_tensor_tensor(s.AR[lo:lo + 64, :, lo:lo + 64], c3(s.eprev, h), -1.0, c3(s.kkn, h), ALU.mult, ALU.mult),
                             reads=[n("eprev"), n("kkn"), n("AR")], writes=[n("AR")])
                        T.op('pool', lambda: P.tensor_tensor(s.AR[lo:lo + 64, :, ot:ot + 64], c3(s.epos, h), c3(s.rl, h), op=ALU.mult),
                             reads=[n("epos"), n("rl"), n("AR")], writes=[n("AR")])
                        T.op('dve', lambda: V.tensor_tensor(s.BK[lo:lo + 64, :, lo:lo + 64], c3(s.eneg, h), c3(s.bb, h), op=ALU.mult),
                             reads=[n("eneg"), n("bb"), n("BK")], writes=[n("BK")])
                        T.op('pool', lambda: P.tensor_tensor(s.BK[lo:lo + 64, :, ot:ot + 64], c3(s.eneg, h), c3(s.kmod, h), op=ALU.mult),
                             reads=[n("eneg"), n("kmod"), n("BK")], writes=[n("BK")])
                    vl3 = s.vl[:].rearrange("p (c t) -> p c t", t=64)
                    T.op('pool', lambda: P.tensor_copy(s.V2[:, :, 0:64], vl3), reads=[n("vl"), n("V2")], writes=[n("V2")])
                    T.op('act', lambda: A.activation(s.V2[:, :, 64:128], vl3, AF.Copy), reads=[n("vl"), n("V2")], writes=[n("V2")])
                    T.op('dve', lambda: V.scalar_tensor_tensor(s.rk[:], s.rl[:], rkc, s.kmod[:], ALU.mult, ALU.mult),
                         reads=[n("rl"), n("kmod"), 'vec'], writes=[n("rk")])
                    rk3 = s.rk[:].rearrange("p (c t) -> p c t", t=64)
                    T.op('pool', lambda: P.tensor_copy(s.RK2[:, :, 0:64], rk3), reads=[n("rk"), n("RK2")], writes=[n("RK2")])
                    T.op('act', lambda: A.activation(s.RK2[:, :, 64:128], rk3, AF.Copy), reads=[n("rk"), n("RK2")], writes=[n("RK2")])
                    yield
                    for c in range(NCH):
                        gA = s.psa[:, 0:128]; gB = s.psb[:, 0:128]
                        T.op('pe', lambda: PE.matmul(gA, s.BK[0:64, c, :], s.AR[0:64, c, :], start=True, stop=True),
                             reads=[n("BK"), n("AR")], writes=[n("psa")])
                        T.op('pe', lambda: PE.matmul(gB, s.BK[64:128, c, :], s.AR[64:128, c, :], start=True, stop=True),
                             reads=[n("BK"), n("AR")], writes=[n("psb")])
                        T.op('dve', lambda: V.tensor_tensor(s.GB[0:64, :], gA[0:64, :], maskA[0:64, :], op=ALU.mult), reads=[n("psa"), 'cst', n("GB")], writes=[n("GB")])
                        T.op('dve', lambda: V.tensor_tensor(s.GK[64:128, :], gA[64:128, :], maskA[64:128, :], op=ALU.mult), reads=[n("psa"), 'cst', n("GK")], writes=[n("GK")])
                        T.op('dve', lambda: V.tensor_tensor(s.GK[0:64, :], gB[0:64, :], maskB[0:64, :], op=ALU.mult), reads=[n("psb"), 'cst', n("GK")], writes=[n("GK")])
                        T.op('dve', lambda: V.tensor_tensor(s.GB[64:128, :], gB[64:128, :], maskB[64:128, :], op=ALU.mult), reads=[n("psb"), 'cst', n("GB")], writes=[n("GB")])
                        yield
                        pt = s.PT[0]; ptn = n("PT0")
                        T.op('pool', lambda: P.tensor_tensor(pt[:, 0:128], s.GB[:], bdm, op=ALU.mult), reads=[n("GB"), 'cst', ptn], writes=[ptn])
                        T.op('pool', lambda: P.tensor_copy(pt[:, 128:256], ident), reads=['cst', ptn], writes=[ptn])
                        T.op('pe', lambda: PE.matmul(s.psb[:, 256:384], pt[:, 0:128], identb, start=True, stop=True), reads=[ptn, 'identb'], writes=[n("psb")])
                        T.op('act', lambda: A.activation(s.PkT[0][:], s.psb[:, 256:384], AF.Copy), reads=[n("psb")], writes=[n("PkT0")])
                        yield
                        pi = 0
                        for lvl in range(6):
                            last = lvl == 5
                            pt = s.PT[pi]; ptn = n(f"PT{pi}"); pkt = s.PkT[pi]; pktn = n(f"PkT{pi}")
                            npt = s.PT[1 - pi]; nptn = n(f"PT{1 - pi}"); npkt = s.PkT[1 - pi]; npktn = n(f"PkT{1 - pi}")
                            if not last:
                                T.op('pe', lambda: PE.matmul(s.psb[:, 0:256], pkt[:], pt[:, 0:256], start=True, stop=True), reads=[pktn, ptn], writes=[n("psb")])
                                T.op('pe', lambda: PE.matmul(s.psb[:, 256:384], pt[:, 0:128], pkt[:], start=True, stop=True), reads=[pktn, ptn], writes=[n("psb")])
                                T.op('act', lambda: A.activation(npt[:, 0:128], s.psb[:, 0:128], AF.Copy), reads=[n("psb"), nptn], writes=[nptn])
                                T.op('dve', lambda: V.tensor_tensor(npt[:, 128:256], s.psb[:, 128:256], pt[:, 128:256], op=ALU.add), reads=[n("psb"), ptn, nptn], writes=[nptn])
                                T.op('act', lambda: A.activation(npkt[:], s.psb[:, 256:384], AF.Copy), reads=[n("psb")], writes=[npktn])
                            else:
                                T.op('pe', lambda: PE.matmul(s.psb[:, 0:128], pkt[:], pt[:, 128:256], start=True, stop=True), reads=[pktn, ptn], writes=[n("psb")])
                                T.op('dve', lambda: V.tensor_tensor(s.Tbd[:], s.psb[:, 0:128], pt[:, 128:256], op=ALU.add), reads=[n("psb"), ptn], writes=[n("Tbd")])
                            pi = 1 - pi
                            yield
                        T.op('pe', lambda: PE.matmul(s.psa[:, 256:384], s.BK[:, c, :], identb, start=True, stop=True), reads=[n("BK"), 'identb'], writes=[n("psa")])
                        T.op('act', lambda: A.activation(s.BKT[:], s.psa[:, 256:384], AF.Copy), reads=[n("psa")], writes=[n("BKT")])
                        T.op('pe', lambda: PE.matmul(s.psa[:, 384:512], s.V2[:, c, :], identb, start=True, stop=True), reads=[n("V2"), 'identb'], writes=[n("psa")])
                        T.op('dve', lambda: V.tensor_copy(s.VV[64:128, 0:64], s.psa[64:128, 384:448]), reads=[n("psa"), n("VV")], writes=[n("VV")])
                        T.op('act', lambda: A.activation(s.VV[0:64, 64:128], s.psa[0:64, 448:512], AF.Copy), reads=[n("psa"), n("VV")], writes=[n("VV")])
                        T.op('pe', lambda: PE.matmul(s.psa[:, 128:130], s.RK2[:, c, :], headselb, start=True, stop=True), reads=[n("RK2"), 'headselb'], writes=[n("psa")])
                        T.op('dve', lambda: V.tensor_copy(s.csb[:], s.psa[:, 128:130]), reads=[n("psa")], writes=[n("csb")])
                        yield
                        Sc = s.Sbd[cur]; Scn = n(f"Sbd{cur}"); Sn = s.Sbd[1 - cur]; Snn = n(f"Sbd{1 - cur}")
                        Scb = s.Sbdb[cur]; Scbn = n(f"Sbdb{cur}"); Snb = s.Sbdb[1 - cur]; Snbn = n(f"Sbdb{1 - cur}")
                        T.op('pe', lambda: PE.matmul(s.psa[:, 0:128], s.GK[:], s.VV[:], start=True, stop=False), reads=[n("GK"), n("VV")], writes=[n("psa")])
                        T.op('pe', lambda: PE.matmul(s.psa[:, 0:128], s.AR[:, c, :], Scb[:], start=False, stop=True), reads=[n("AR"), Scbn], writes=[n("psa")])
                        T.op('act', lambda: A.activation(s.WY[:], s.psa[:, 0:128], AF.Copy), reads=[n("psa")], writes=[n("WY")])
                        yield
                        T.op('pe', lambda: PE.matmul(s.psa[:, 128:256], s.Tbd[:], s.WY[:], start=True, stop=True), reads=[n("Tbd"), n("WY")], writes=[n("psa")])
                        T.op('dve', lambda: V.tensor_copy(s.UU[0:64, 0:64], s.psa[0:64, 128:192]), reads=[n("psa"), n("UU")], writes=[n("UU")])
                        T.op('act', lambda: A.activation(s.UU[64:128, 64:128], s.psa[64:128, 192:256], AF.Copy), reads=[n("psa"), n("UU")], writes=[n("UU")])
                        yield
                        T.op('pe', lambda: PE.matmul(s.psa[:, 256:384], s.BKT[:], s.VV[:], start=True, stop=False), reads=[n("BKT"), n("VV")], writes=[n("psa")])
                        T.op('pe', lambda: PE.matmul(s.psa[:, 256:384], ident, Sc[:], start=False, stop=False), reads=['cst', Scn], writes=[n("psa")])
                        T.op('pe', lambda: PE.matmul(s.psa[:, 256:384], s.BKT[:], s.UU[:], start=False, stop=True), reads=[n("BKT"), n("UU")], writes=[n("psa")])
                        ce = c * 64 + 63
                        T.op('dve', lambda: V.tensor_scalar(Sn[0:64, 0:64], s.psa[0:64, 256:320], s.epos[0:64, ce:ce + 1], None, ALU.mult),
                             reads=[n("psa"), n("epos"), Snn], writes=[Snn])
                        T.op('act', lambda: A.activation(Sn[64:128, 64:128], s.psa[64:128, 320:384], AF.Copy, scale=s.epos[64:128, ce:ce + 1]),
                             reads=[n("psa"), n("epos"), Snn], writes=[Snn])
                        T.op('pool', lambda: P.tensor_copy(Snb[:], Sn[:]), reads=[Snn, Snbn], writes=[Snbn])
                        if isx:
                            T.op('pe', lambda: PE.matmul(s.psa[:, 384:512], s.GB[:], s.UU[:], start=True, stop=True), reads=[n("GB"), n("UU")], writes=[n("psa")])
                            T.op('dve', lambda: V.tensor_tensor(s.Ysb[64:128, 0:64], s.psa[64:128, 384:448], s.WY[64:128, 0:64], op=ALU.add),
                                 reads=[n("psa"), n("WY"), n("Ysb")], writes=[n("Ysb")])
                            T.op('dve', lambda: V.tensor_tensor(s.Ysb[0:64, 64:128], s.psa[0:64, 448:512], s.WY[0:64, 64:128], op=ALU.add),
                                 reads=[n("psa"), n("WY"), n("Ysb")], writes=[n("Ysb")])
                            T.op('pool', lambda: P.tensor_scalar(s.Bsb[64:128, 0:64], s.VV[64:128, 0:64], s.csb[64:128, 0:1], None, ALU.mult),
                                 reads=[n("VV"), n("csb"), n("Bsb")], writes=[n("Bsb")])
                            T.op('pool', lambda: P.tensor_scalar(s.Bsb[0:64, 64:128], s.VV[0:64, 64:128], s.csb[0:64, 1:2], None, ALU.mult),
                                 reads=[n("VV"), n("csb"), n("Bsb")], writes=[n("Bsb")])
                            yield
                            T.op('pe', lambda: PE.matmul(s.psb[:, 384:448], s.Ysb[:], JJb[d], start=True, stop=True), reads=[n("Ysb"), 'JJb'], writes=[n("psb")])
                            T.op('pe', lambda: PE.matmul(s.psb[:, 448:512], s.Bsb[:], JJb[d], start=True, stop=True), reads=[n("Bsb"), 'JJb'], writes=[n("psb")])
                            cp = c if d == 0 else NCH - 1 - c
                            T.op('act', lambda: A.activation(s.Yout[:, 0, cp * 64:cp * 64 + 64], s.psb[:, 384:448], AF.Copy), reads=[n("psb"), n("Yout")], writes=[n("Yout")])
                            T.op('dve', lambda: V.tensor_copy(s.Yout[:, 1, cp * 64:cp * 64 + 64], s.psb[:, 448:512]), reads=[n("psb"), n("Yout")], writes=[n("Yout")])
                        cur = 1 - cur
                        yield
                    if isx:
                        r0 = c0 - CTX
                        f0 = r0 if d == 0 else SEQ - r0 - TW
                        T.dma('sp', yT[d, p * 128:(p + 1) * 128, f0:f0 + TW], s.Yout[:, 0, :], reads=[n("Yout")])
                        T.dma('sp', bT[d, p * 128:(p + 1) * 128, f0:f0 + TW], s.Yout[:, 1, :], reads=[n("Yout")])

            streams = [mk_stream(i) for i in range(4)]
            gens = [rwkv_stream(streams[2 * d + p], d, p) for d in range(2) for p in range(2)]
            alive = [True] * 4
            while any(alive):
                for i, g_ in enumerate(gens):
                    if alive[i]:
                        try:
                            next(g_)
                        except StopIteration:
                            alive[i] = False
            T.barrier()
            S.close()

            S = Scope(nc, T)
            FW = 512 if SEQ % 512 == 0 else 256
            g2a_sb = S.sb("g2a_sb", [128, 256]); g2b_sb = S.sb("g2b_sb", [32, 256])
            T.dma('sp', g2a_sb[:], g2a, writes=['g2a']); T.dma('sp', g2b_sb[:], g2b, writes=['g2b'])
            graw0 = S.sb("graw0", [128, FW + 2]); graw1 = S.sb("graw1", [32, FW + 2])
            sgd0 = S.sb("sgd0", [128, FW]); sgd1 = S.sb("sgd1", [32, FW])
            ft0 = S.sb("ft0", [128, FW]); ft1 = S.sb("ft1", [128, FW])
            yy = [S.sb(f"yy{i}", [128, FW]) for i in range(4)]
            yc = S.sb("yc", [128, FW]); fsq = S.sb("fsq", [128, FW]); frs = S.sb("frs", [128, FW]); fz = S.sb("fz", [128, FW]); fo = S.sb("fo", [128, FW])
            psF = [S.ps(f"psF{i}", [128, 512]) for i in range(3)]
            for c0 in range(0, SEQ, FW):
                for (gr, grn, r0_, nr, mc) in ((graw0, 'graw0', 896, 128, 8), (graw1, 'graw1', 1024, 32, 9)):
                    lo_ = max(c0 - 1, 0); hi_ = min(c0 + FW + 1, SEQ)
                    if c0 == 0:
                        T.op('pool', lambda: P.memset(gr[0:nr, 0:1], 0.0), reads=[grn], writes=[grn])
                    if c0 + FW + 1 > SEQ:
                        T.op('pool', lambda: P.memset(gr[0:nr, FW + 1:FW + 2], 0.0), reads=[grn], writes=[grn])
                    T.dma('sp', gr[0:nr, lo_ - (c0 - 1): hi_ - (c0 - 1)], pxf[r0_:r0_ + nr, CTX + lo_:CTX + hi_], reads=[grn], writes=[grn])
                    sg_ = sgd0 if nr == 128 else sgd1; sgn = 'sgd0' if nr == 128 else 'sgd1'
                    T.op('dve', lambda: V.tensor_tensor(ft0[0:nr, :], gr[0:nr, 0:FW], gr[0:nr, 2:FW + 2], op=ALU.add), reads=[grn, 'ft0'], writes=['ft0'])
                    T.op('act', lambda: A.activation(ft1[0:nr, :], gr[0:nr, 1:FW + 1], AF.Copy, scale=omu[0:nr, mc:mc + 1]), reads=[grn, 'omu', 'ft1'], writes=['ft1'])
                    T.op('dve', lambda: V.scalar_tensor_tensor(ft0[0:nr, :], ft0[0:nr, :], hmu[0:nr, mc:mc + 1], ft1[0:nr, :], ALU.mult, ALU.add),
                         reads=['ft0', 'ft1', 'hmu'], writes=['ft0'])
                    T.op('act', lambda: A.activation(sg_[0:nr, :], ft0[0:nr, :], AF.Sigmoid), reads=['ft0'], writes=[sgn])
                for p in range(2):
                    lnw = vec_sb[:, 22 + p:23 + p]; lnb = vec_sb[:, 24 + p:25 + p]
                    srcs = [yT[0], yT[1], bT[0], bT[1]]
                    for i in range(4):
                        T.dma('sp', yy[i][:], srcs[i][p * 128:(p + 1) * 128, c0:c0 + FW], writes=[f"yy{i}"])
                    T.op('dve', lambda: V.tensor_tensor(yy[0][:], yy[0][:], yy[1][:], op=ALU.add), reads=['yy0', 'yy1'], writes=['yy0'])
                    T.op('pool', lambda: P.tensor_tensor(yy[2][:], yy[2][:], yy[3][:], op=ALU.add), reads=['yy2', 'yy3'], writes=['yy2'])
                    T.op('pe', lambda: PE.matmul(psF[0][:, 0:FW], ones64[:], yy[0][:], start=True, stop=True), reads=['ones64', 'yy0'], writes=['psF0'])
                    T.op('dve', lambda: V.tensor_tensor(yc[:], yy[0][:], psF[0][:, 0:FW], op=ALU.subtract), reads=['yy0', 'psF0'], writes=['yc'])
                    T.op('act', lambda: A.activation(fsq[:], yc[:], AF.Square), reads=['yc'], writes=['fsq'])
                    T.op('pe', lambda: PE.matmul(psF[1][:, 0:FW], ones64[:], fsq[:], start=True, stop=True), reads=['ones64', 'fsq'], writes=['psF1'])
                    T.op('act', lambda: A.activation(frs[:], psF[1][:, 0:FW], AF.Sqrt, bias=GN_EPS), reads=['psF1'], writes=['frs'])
                    T.op('dve', lambda: V.reciprocal(frs[:], frs[:]), reads=['frs'], writes=['frs'])
                    T.op('dve', lambda: V.tensor_tensor(yc[:], yc[:], frs[:], op=ALU.mult), reads=['yc', 'frs'], writes=['yc'])
                    T.op('act', lambda: A.activation(fz[:], yc[:], AF.Identity, bias=lnb, scale=lnw), reads=['yc', 'vec'], writes=['fz'])
                    T.op('pool', lambda: P.tensor_tensor(fz[:], fz[:], yy[2][:], op=ALU.add), reads=['fz', 'yy2'], writes=['fz'])
                    T.op('pe', lambda: PE.matmul(psF[2][:, 0:FW], g2a_sb[:, p * 128:(p + 1) * 128], sgd0[:], start=True, stop=False), reads=['g2a', 'sgd0'], writes=['psF2'])
                    T.op('pe', lambda: PE.matmul(psF[2][:, 0:FW], g2b_sb[0:32, p * 128:(p + 1) * 128], sgd1[0:32, :], start=False, stop=True), reads=['g2b', 'sgd1'], writes=['psF2'])
                    T.op('dve', lambda: V.tensor_tensor(fo[:], fz[:], psF[2][:, 0:FW], op=ALU.mult), reads=['fz', 'psF2'], writes=['fo'])
                    T.dma('sp', mixT_rw[p * 128:(p + 1) * 128, c0:c0 + FW], fo[:], reads=['fo'])
            T.barrier()
            S.close()

            S = Scope(nc, T)
            AW = 512 if SEQ % 512 == 0 else 256
            NBK = SEQ // 128; NCB = CTX // 128
            QR, QPR, KR, KPR, VR = 1056, 1312, 1568, 1632, 1696
            kT = S.sb("kT", [128, SEQ], BF16); kTc = S.sb("kTc", [128, CTX], BF16)
            Vtm = S.sb("Vtm", [128, NBK, 64], BF16); Vtmc = S.sb("Vtmc", [128, NCB, 64], BF16)
            kraw = S.sb("kraw", [128, AW]); kpraw = S.sb("kpraw", [128, AW]); vraw = S.sb("vraw", [64, AW])
            cos_t = S.sb("cos_t", [128, AW]); sin_t = S.sb("sin_t", [128, AW])
            at0 = S.sb("at0", [128, AW]); at1 = S.sb("at1", [128, AW])
            qraw = S.sb("qraw", [64, 4, AW]); qpraw = S.sb("qpraw", [64, 4, AW]); qT = S.sb("qT", [64, 4, AW], BF16)
            cos4 = S.sb("cos4", [64, 4, AW]); sin4 = S.sb("sin4", [64, 4, AW]); aq0 = S.sb("aq0", [64, 4, AW]); aq1 = S.sb("aq1", [64, 4, AW])
            es = S.sb("es", [128, 4])
            T.dma('sp', es[:], sinkb, writes=['es'])
            T.op('act', lambda: A.activation(es[:], es[:], AF.Exp), reads=['es'], writes=['es'])
            mask4 = [S.sb(f"mask4_{i}", [128, 4, 128], BF16) for i in range(2)]
            for i, tri in enumerate((triLO, triHI)):
                for h in range(4):
                    T.op('dve', lambda: V.tensor_copy(mask4[i][:, h, :], tri), reads=['cst', f"mask4_{i}"], writes=[f"mask4_{i}"])
            ones64bf = S.sb("ones64bf", [128, 64], BF16)
            T.op('pool', lambda: P.memset(ones64bf[:], 1.0), writes=['ones64bf'])
            PTr = [S.sb(f"PTr{i}", [128, 512], BF16) for i in range(6)]
            den = S.sb("den", [64, 512]); att = S.sb("att", [64, 512])
            psA_ = [S.ps(f"psA{i}", [128, 512]) for i in range(3)]
            psO_ = S.ps("psAO", [128, 512]); psD_ = S.ps("psAD", [128, 512]); psVt = S.ps("psVt", [128, 512])
            for h in range(2):
                T.dma('sp', kraw[64 * h:64 * h + 64, 0:CTX], pxf[KR:KR + 64, 0:CTX], reads=['kraw'], writes=['kraw'])
            T.op('dve', lambda: V.tensor_copy(kTc[:], kraw[:, 0:CTX]), reads=['kraw'], writes=['kTc'])
            T.dma('sp', vraw[:, 0:CTX], pxf[VR:VR + 64, 0:CTX], writes=['vraw'])
            for j in range(NCB):
                T.op('pe', lambda: PE.transpose(psVt[:, 0:64], vraw[0:64, j * 128:(j + 1) * 128], ident[0:64, 0:64]), reads=['vraw', 'cst'], writes=['psVt'])
                T.op('dve', lambda: V.tensor_copy(Vtmc[:, j, :], psVt[:, 0:64]), reads=['psVt'], writes=['Vtmc'])
            for c0 in range(0, SEQ, AW):
                for h in range(2):
                    T.dma('sp', kraw[64 * h:64 * h + 64, :], pxf[KR:KR + 64, CTX + c0:CTX + c0 + AW], reads=['kraw'], writes=['kraw'])
                    T.dma('sp', kpraw[64 * h:64 * h + 64, :], pxf[KPR:KPR + 64, CTX + c0:CTX + c0 + AW], reads=['kpraw'], writes=['kpraw'])
                T.dma('sp', cos_t[:], cosT[:, c0:c0 + AW], writes=['cos_t'])
                T.dma('sp', sin_t[:], sinT[:, c0:c0 + AW], writes=['sin_t'])
                T.dma('sp', vraw[:, 0:AW], pxf[VR:VR + 64, CTX + c0:CTX + c0 + AW], writes=['vraw'])
                T.op('dve', lambda: V.tensor_tensor(at0[:], kraw[:], cos_t[:], op=ALU.mult), reads=['kraw', 'cos_t'], writes=['at0'])
                T.op('pool', lambda: P.tensor_tensor(at1[:], kpraw[:], sin_t[:], op=ALU.mult), reads=['kpraw', 'sin_t'], writes=['at1'])
                T.op('dve', lambda: V.tensor_tensor(kT[:, c0:c0 + AW], at0[:], at1[:], op=ALU.add), reads=['at0', 'at1'], writes=['kT'])
                for j in range(AW // 128):
                    T.op('pe', lambda: PE.transpose(psVt[:, 0:64], vraw[0:64, j * 128:(j + 1) * 128], ident[0:64, 0:64]), reads=['vraw', 'cst'], writes=['psVt'])
                    T.op('dve', lambda: V.tensor_copy(Vtm[:, c0 // 128 + j, :], psVt[:, 0:64]), reads=['psVt'], writes=['Vtm'])
            pi_ = 0; ai = 0
            for c0 in range(0, SEQ, AW):
                for h in range(4):
                    T.dma('sp', qraw[:, h, :], pxf[QR + h * 64:QR + h * 64 + 64, CTX + c0:CTX + c0 + AW], reads=['qraw'], writes=['qraw'])
                    T.dma('sp', qpraw[:, h, :], pxf[QPR + h * 64:QPR + h * 64 + 64, CTX + c0:CTX + c0 + AW], reads=['qpraw'], writes=['qpraw'])
                    T.dma('sp', cos4[:, h, :], cosT[0:64, c0:c0 + AW], reads=['cos4'], writes=['cos4'])
                    T.dma('sp', sin4[:, h, :], sinT[0:64, c0:c0 + AW], reads=['sin4'], writes=['sin4'])
                T.op('dve', lambda: V.tensor_tensor(aq0[:], qraw[:], cos4[:], op=ALU.mult), reads=['qraw', 'cos4'], writes=['aq0'])
                T.op('pool', lambda: P.tensor_tensor(aq1[:], qpraw[:], sin4[:], op=ALU.mult), reads=['qpraw', 'sin4'], writes=['aq1'])
                T.op('dve', lambda: V.tensor_tensor(qT[:], aq0[:], aq1[:], op=ALU.add), reads=['aq0', 'aq1', 'qT'], writes=['qT'])
                for jb in range(AW // 128):
                    nq = c0 // 128 + jb
                    kbs = [('c', j, None) for j in range(NCB)]
                    if nq - 1 >= 0:
                        kbs.append(('x', nq - 1, 0))
                    kbs.append(('x', nq, None))
                    if nq + 1 < NBK:
                        kbs.append(('x', nq + 1, 1))
                    pts = []
                    for (kind, kb, mk) in kbs:
                        pa = psA_[ai % 3]; pan = f"psA{ai % 3}"; ai += 1
                        for h in range(4):
                            ksrc = kTc if kind == 'c' else kT
                            T.op('pe', lambda: PE.matmul(pa[:, h * 128:(h + 1) * 128], ksrc[0:64, kb * 128:(kb + 1) * 128],
                                                         qT[0:64, h, jb * 128:(jb + 1) * 128], start=True, stop=True),
                                 reads=['kT', 'kTc', 'qT'], writes=[pan])
                        pt = PTr[pi_ % 6]; ptn = f"PTr{pi_ % 6}"; pi_ += 1
                        T.op('act', lambda: A.activation(pt[:], pa[:], AF.Exp, scale=0.125), reads=[pan], writes=[ptn])
                        if mk is not None:
                            T.op('pool', lambda: P.tensor_tensor(pt[:], pt[:], mask4[mk][:].rearrange("p h n -> p (h n)"), op=ALU.mult),
                                 reads=[ptn, f"mask4_{mk}"], writes=[ptn])
                        pts.append((pt, ptn, kind, kb))
                    for i, (pt, ptn, kind, kb) in enumerate(pts):
                        vsrc = Vtmc if kind == 'c' else Vtm
                        T.op('pe', lambda: PE.matmul(psO_[0:64, :], vsrc[:, kb, :], pt[:], start=(i == 0), stop=(i == len(pts) - 1)),
                             reads=['Vtm', 'Vtmc', ptn], writes=['psAO'])
                        T.op('pe', lambda: PE.matmul(psD_[0:64, :], ones64bf[:], pt[:], start=(i == 0), stop=(i == len(pts) - 1)),
                             reads=['ones64bf', ptn], writes=['psAD'])
                    for h in range(4):
                        T.op('dve', lambda: V.tensor_scalar(den[:, h * 128:(h + 1) * 128], psD_[0:64, h * 128:(h + 1) * 128], es[0:64, h:h + 1], None, ALU.add),
                             reads=['psAD', 'es', 'den'], writes=['den'])
                    T.op('dve', lambda: V.reciprocal(den[:], den[:]), reads=['den'], writes=['den'])
                    T.op('dve', lambda: V.tensor_tensor(att[:], psO_[0:64, :], den[:], op=ALU.mult), reads=['psAO', 'den'], writes=['att'])
                    q0 = nq * 128
                    T.dma('sp', mixT_att[0:256, q0:q0 + 128].rearrange("(h p) n -> p h n", p=64), att[:].rearrange("p (h n) -> p h n", n=128), reads=['att'])
            T.barrier()
            S.close()


        def phase_b():
            S = Scope(nc, T)
            wo_sb = S.sb("wo_sb", [128, KC, D], BF16)
            wst = [S.sb(f"bwst{i}", [128, D]) for i in range(2)]
            for k in range(KC):
                w = wst[k % 2]; wn = f"bwst{k % 2}"
                T.dma('sp', w[:], w_out[k * 128:(k + 1) * 128, :], reads=[wn], writes=[wn])
                E = V if k % 2 == 0 else P
                T.op('dve' if k % 2 == 0 else 'pool', lambda: E.tensor_copy(wo_sb[:, k, :], w[:]), reads=[wn], writes=[f"wo{k}"])
            gta = S.sb("gta", [128, D])
            bcast_row(gta[:], modrow[0, 0:1, 2 * D:3 * D], 'gta')
            mt = S.sb("bmt", [128, KC, 512]); mtb = S.sb("bmtb", [128, KC, 512], BF16)
            xt = [S.sb(f"bx{i}", [128, D]) for i in range(2)]
            o1 = [S.sb(f"bo{i}", [128, D]) for i in range(2)]
            ps = [S.ps(f"bps{i}", [128, 512]) for i in range(8)]
            bi = 0
            pre = Pre([(xt[b % 2][:], f"bx{b % 2}", x_in[b * 128:(b + 1) * 128, :]) for b in range(NBLK)])
            for c0 in range(0, SEQ, 512):
                T.dma('sp', mt[:], mixs[:, c0:c0 + 512].rearrange("(k p) n -> p k n", p=128), reads=['bmt'], writes=['bmt'])
                T.op('act', lambda: A.activation(mtb[:], mt[:], AF.Copy), reads=['bmt', 'bmtb'], writes=['bmtb'])
                for j in range(4):
                    t0 = c0 + j * 128
                    x_ = xt[bi % 2]; xn = f"bx{bi % 2}"; o_ = o1[bi % 2]; on = f"bo{bi % 2}"
                    pre.get(bi)
                    for ct in range(4):
                        pp = ps[(bi * 4 + ct) % 8]; pn = f"bps{(bi * 4 + ct) % 8}"
                        for k in range(KC):
                            T.op('pe', lambda: PE.matmul(pp[:], mtb[:, k, j * 128:(j + 1) * 128], wo_sb[:, k, ct * 512:(ct + 1) * 512], start=(k == 0), stop=(k == KC - 1)),
                                 reads=['bmtb', f"wo{k}"], writes=[pn])
                        T.op('dve', lambda: V.tensor_tensor(o_[:, ct * 512:(ct + 1) * 512], pp[:], gta[:, ct * 512:(ct + 1) * 512], op=ALU.mult),
                             reads=[pn, 'gta', on], writes=[on])
                    T.op('pool', lambda: P.tensor_tensor(o_[:], o_[:], x_[:], op=ALU.add), reads=[on, xn], writes=[on])
                    T.dma('sp', x1[t0:t0 + 128, :], o_[:], reads=[on])
                    bi += 1
            T.barrier()
            S.close()

        def phase_moe(l, xin, acc):
            S = Scope(nc, T)
            geff, shr = mod_rows(S, l, 0, 4, 3, 1 + 2 * l, "mfm")
            rw_sb = S.sb("rw_sb", [128, KC, 16])
            T.dma('sp', rw_sb[:], router_w[l].rearrange("(k p) e -> p k e", p=128), writes=['rw_sb'])
            xt = [S.sb(f"mx{i}", [128, D]) for i in range(2)]
            hr = [S.sb(f"mhr{i}", [128, XR]) for i in range(2)]
            hT = S.sb("mhT", [128, KC, 128])
            Esb = S.sb("mE", [16, 128]); rec = S.sb("mrec", [16, 128]); aff = S.sb("maff", [16, 128])
            pst = [S.ps(f"mps{i}", [128, 512]) for i in range(4)]
            psl = S.ps("mpsl", [128, 512]); pss = S.ps("mpss", [128, 512]); psa = S.ps("mpsa", [128, 512])
            for i in range(2):
                T.op('pool', lambda: P.memset(hr[i][:, D:XR], 0.0), writes=[f"mhr{i}"])
            pre = Pre([(xt[b % 2][:], f"mx{b % 2}", xin[b * 128:(b + 1) * 128, :]) for b in range(NBLK)])
            for b in range(NBLK):
                t0 = b * 128
                x_ = xt[b % 2]; xn = f"mx{b % 2}"; h_ = hr[b % 2]; hn = f"mhr{b % 2}"
                pre.get(b)
                T.dma('sp', acc[t0:t0 + 128, :], x_[:], reads=[xn])
                rms_tok(S, x_, xn, 128, "mfm", geff, shr, h_[:, 0:D], hn)
                for kq in range(4):
                    pp = pst[kq]; pn = f"mps{kq}"
                    for k4 in range(4):
                        k = kq * 4 + k4
                        T.op('pe', lambda: PE.transpose(pp[:, k4 * 128:(k4 + 1) * 128], h_[:, k * 128:(k + 1) * 128], ident), reads=[hn, 'cst'], writes=[pn])
                    if kq % 2 == 0:
                        T.op('act', lambda: A.activation(hT[:, kq * 4:(kq + 1) * 4, :], pp[:].rearrange("p (k n) -> p k n", n=128), AF.Copy), reads=[pn, 'mhT'], writes=['mhT'])
                    else:
                        T.op('dve', lambda: V.tensor_copy(hT[:, kq * 4:(kq + 1) * 4, :], pp[:].rearrange("p (k n) -> p k n", n=128)), reads=[pn, 'mhT'], writes=['mhT'])
                for k in range(KC):
                    T.op('pe', lambda: PE.matmul(psl[0:16, 0:128], rw_sb[:, k, :], hT[:, k, :], start=(k == 0), stop=(k == KC - 1)), reads=['rw_sb', 'mhT'], writes=['mpsl'])
                T.op('act', lambda: A.activation(Esb[:], psl[0:16, 0:128], AF.Exp), reads=['mpsl'], writes=['mE'])
                T.op('pe', lambda: PE.matmul(pss[0:16, 0:128], onesbd[0:16, 0:16], Esb[:], start=True, stop=True), reads=['cst', 'mE'], writes=['mpss'])
                T.op('dve', lambda: V.reciprocal(rec[:], pss[0:16, 0:128]), reads=['mpss'], writes=['mrec'])
                T.op('dve', lambda: V.tensor_tensor(aff[:], Esb[:], rec[:], op=ALU.mult), reads=['mE', 'mrec', 'maff'], writes=['maff'])
                T.dma('sp', affT[:, t0:t0 + 128], aff[:], reads=['maff'])
                T.op('pe', lambda: PE.transpose(psa[:, 0:16], aff[:], ident[0:16, 0:16]), reads=['maff', 'cst'], writes=['mpsa'])
                T.op('act', lambda: A.activation(h_[:, D:D + 16], psa[:, 0:16], AF.Copy), reads=['mpsa', hn], writes=[hn])
                T.op('dve', lambda: V.tensor_scalar(h_[:, D + 16:D + 17], iota_p[:], float(t0), None, ALU.add), reads=['iota_p', hn], writes=[hn])
                T.dma('sp', hrow[t0:t0 + 128, :], h_[:], reads=[hn])
            T.barrier()
            S.close()
            S = Scope(nc, T)
            NJ = SEQ // 8
            af = S.sb("taf", [128, NJ]); cmp_ = S.sb("tcmp", [128, NJ]); onesf = S.sb("tones", [128, NJ]); pre = S.sb("tpre", [128, NJ])
            lo = S.sb("tlo", [128, 1]); hi = S.sb("thi", [128, 1]); mid = S.sb("tmid", [128, 1]); cnt = S.sb("tcnt", [128, 2]); ge = S.sb("tge", [128, 1])
            d1 = S.sb("td1", [128, 1])
            psc = S.ps("tpsc", [128, 512])
            T.dma('sp', af[:], affT.rearrange("e (j n) -> (e j) n", j=8), writes=['taf'])
            T.op('pool', lambda: P.memset(lo[:], 0.0), writes=['tlo'])
            T.op('pool', lambda: P.memset(hi[:], 1.0), writes=['thi'])
            T.op('pool', lambda: P.memset(cnt[:], 0.0), writes=['tcnt'])
            T.op('pool', lambda: P.memset(onesf[:], 1.0), writes=['tones'])
            for it in range(34):
                T.op('dve', lambda: V.tensor_scalar(mid[:], lo[:], hi[:, 0:1], 0.5, ALU.add, ALU.mult), reads=['tlo', 'thi'], writes=['tmid'])
                T.op('dve', lambda: V.tensor_scalar(cmp_[:], af[:], mid[:, 0:1], None, ALU.is_ge, ALU.add, accum_out=cnt[:, 0:1]),
                     reads=['taf', 'tmid', 'tcnt'], writes=['tcmp', 'tcnt'])
                T.op('pe', lambda: PE.matmul(psc[:, 0:2], ones8, cnt[:], start=True, stop=True), reads=['cst2', 'tcnt'], writes=['tpsc'])
                T.op('dve', lambda: V.tensor_scalar(ge[:], psc[:, 0:1], float(CAP) - 0.5, None, ALU.is_ge), reads=['tpsc'], writes=['tge'])
                T.op('dve', lambda: V.tensor_tensor(d1[:], mid[:], lo[:], op=ALU.subtract), reads=['tmid', 'tlo'], writes=['td1'])
                T.op('dve', lambda: V.scalar_tensor_tensor(lo[:], d1[:], ge[:, 0:1], lo[:], ALU.mult, ALU.add), reads=['td1', 'tge', 'tlo'], writes=['tlo'])
                T.op('dve', lambda: V.tensor_tensor(d1[:], hi[:], mid[:], op=ALU.subtract), reads=['tmid', 'thi'], writes=['td1'])
                T.op('dve', lambda: V.scalar_tensor_tensor(hi[:], d1[:], ge[:, 0:1], mid[:], ALU.mult, ALU.add), reads=['td1', 'tge', 'tmid', 'thi'], writes=['thi'])
            T.op('dve', lambda: V.tensor_scalar(cmp_[:], af[:], lo[:, 0:1], None, ALU.is_ge), reads=['taf', 'tlo'], writes=['tcmp'])
            T.op('dve', lambda: V.tensor_tensor_scan(pre[:], onesf[:], cmp_[:], 0.0, ALU.mult, ALU.add), reads=['tones', 'tcmp'], writes=['tpre'])
            T.op('dve', lambda: V.tensor_copy(cnt[:, 0:1], pre[:, NJ - 1:NJ]), reads=['tpre', 'tcnt'], writes=['tcnt'])
            T.op('dve', lambda: V.tensor_copy(cnt[:, 1:2], pre[:, NJ - 1:NJ]), reads=['tpre', 'tcnt'], writes=['tcnt'])
            T.op('pe', lambda: PE.matmul(psc[:, 0:2], low8, cnt[:], start=True, stop=True), reads=['cst2', 'tcnt'], writes=['tpsc'])
            T.op('dve', lambda: V.tensor_copy(d1[:], psc[:, 0:1]), reads=['tpsc'], writes=['td1'])
            T.op('dve', lambda: V.scalar_tensor_tensor(pre[:], pre[:], d1[:, 0:1], cmp_[:], ALU.add, ALU.mult), reads=['tpre', 'td1', 'tcmp'], writes=['tpre'])
            T.op('dve', lambda: V.tensor_scalar(pre[:], pre[:], -1.0, None, ALU.add), reads=['tpre'], writes=['tpre'])
            T.dma('sp', slot_d.rearrange("e (j n) -> (e j) n", j=8), pre[:], reads=['tpre'], writes=['slot_d'])
            stm = S.sb("stm", [128, 16, NBLK]); neg = S.sb("sneg", [128, 16, NBLK]); su = S.sb("su", [128, 16, NBLK], U32)
            colp = S.sb("colp", [128, 1])
            T.op('dve', lambda: V.tensor_scalar(colp[:], iota_p[:], float(CAP + 1), None, ALU.add), reads=['iota_p'], writes=['colp'])
            T.dma('sp', stm[:], slot_d.rearrange("e (p b) -> p e b", b=NBLK), reads=['slot_d'], writes=['stm'])
            T.op('dve', lambda: V.tensor_scalar(neg[:], stm[:], 0.0, None, ALU.is_lt), reads=['stm'], writes=['sneg'])
            T.op('dve', lambda: V.scalar_tensor_tensor(stm[:], neg[:], colp[:, 0:1], stm[:], ALU.mult, ALU.add), reads=['sneg', 'colp', 'stm'], writes=['stm'])
            T.op('dve', lambda: V.tensor_copy(su[:], stm[:]), reads=['stm'], writes=['su'])
            ht = [S.sb(f"dht{i}", [128, XR]) for i in range(2)]
            hrow_v = hrow.rearrange("(p b) c -> p b c", b=NBLK)
            bc_reg = nc.gpsimd.to_reg(CAP - 1)
            for b in range(NBLK):
                h_ = ht[b % 2]; hn = f"dht{b % 2}"
                T.dma('sp', h_[:], hrow_v[:, b, :], reads=[hn], writes=[hn])
                for e in range(16):
                    T.idma(Xg[e], bass.IndirectOffsetOnAxis(ap=su[:, e, b:b + 1], axis=0), h_[:], None, reads=[hn, 'su'], bounds_check=bc_reg, oob_is_err=False)
            T.barrier()
            S.close()
            S = Scope(nc, T)
            SW = min(512, CAP); NSJ = SW // 128
            gtf = S.sb("gtf", [128, D])
            bcast_row(gtf[:], modrow[l, 0:1, 5 * D:6 * D], 'gtf')
            Wg_sb = S.sb("Wg_sb", [128, KC, 1024], BF16); Wu_sb = S.sb("Wu_sb", [128, KC, 1024], BF16); Wd_sb = S.sb("Wd_sb", [128, 8, D], BF16)
            ws = [S.sb(f"ews{i}", [128, 1024]) for i in range(2)]
            xg_t = [S.sb(f"exg{i}", [128, XR]) for i in range(2)]
            XT = S.sb("eXT", [128, KC, SW], BF16); hid = S.sb("ehid", [128, 8, SW], BF16); gs = S.sb("egs", [128, SW], BF16)
            side = S.sb("eside", [128, 4, 17]); tix = S.sb("etix", [128, 4], U32)
            Ysb = [S.sb(f"eY{i}", [128, D]) for i in range(2)]
            pst = [S.ps(f"eps{i}", [128, 512]) for i in range(2)]
            psg = S.ps("epsg", [128, 512]); psu = S.ps("epsu", [128, 512])
            psy = [S.ps(f"epsy{i}", [128, 512]) for i in range(4)]
            wi = 0; xi = 0; yi = 0; ti = 0
            for e in range(16):
                for k in range(KC):
                    for (src, dst, dn) in ((wg, Wg_sb, "Wg"), (wu, Wu_sb, "Wu")):
                        w = ws[wi % 2]; wn = f"ews{wi % 2}"; wi += 1
                        T.dma('sp', w[:], src[l, e, k * 128:(k + 1) * 128, :], reads=[wn], writes=[wn])
                        if wi % 2 == 0:
                            T.op('pool', lambda: P.tensor_copy(dst[:, k, :], w[:]), reads=[wn, dn], writes=[dn])
                        else:
                            T.op('dve', lambda: V.tensor_copy(dst[:, k, :], w[:]), reads=[wn, dn], writes=[dn])
                for f in range(8):
                    for hf in range(2):
                        w = ws[wi % 2]; wn = f"ews{wi % 2}"; wi += 1
                        T.dma('sp', w[:], wd[l, e, f * 128:(f + 1) * 128, hf * 1024:(hf + 1) * 1024], reads=[wn], writes=[wn])
                        if wi % 2 == 0:
                            T.op('pool', lambda: P.tensor_copy(Wd_sb[:, f, hf * 1024:(hf + 1) * 1024], w[:]), reads=[wn, "Wd"], writes=["Wd"])
                        else:
                            T.op('dve', lambda: V.tensor_copy(Wd_sb[:, f, hf * 1024:(hf + 1) * 1024], w[:]), reads=[wn, "Wd"], writes=["Wd"])
                for s0 in range(0, CAP, SW):
                    for j in range(NSJ):
                        xg = xg_t[xi % 2]; xgn = f"exg{xi % 2}"; xi += 1
                        T.dma('sp', xg[:], Xg[e][s0 + j * 128:s0 + (j + 1) * 128, :], reads=[xgn], writes=[xgn])
                        T.op('pool', lambda: P.tensor_copy(side[:, j, :], xg[:, D:D + 17]), reads=[xgn, 'eside'], writes=['eside'])
                        for kq in range(4):
                            pp = pst[ti % 2]; pn = f"eps{ti % 2}"; ti += 1
                            for k4 in range(4):
                                k = kq * 4 + k4
                                T.op('pe', lambda: PE.transpose(pp[:, k4 * 128:(k4 + 1) * 128], xg[:, k * 128:(k + 1) * 128], ident), reads=[xgn, 'cst'], writes=[pn])
                            o_ = XT[:, kq * 4:(kq + 1) * 4, j * 128:(j + 1) * 128]
                            i_ = pp[:].rearrange("p (k n) -> p k n", n=128)
                            if ti % 2 == 0:
                                T.op('act', lambda: A.activation(o_, i_, AF.Copy), reads=[pn, 'eXT'], writes=['eXT'])
                            else:
                                T.op('dve', lambda: V.tensor_copy(o_, i_), reads=[pn, 'eXT'], writes=['eXT'])
                    T.op('dve', lambda: V.tensor_copy(tix[:, 0:NSJ], side[:, 0:NSJ, 16]), reads=['eside', 'etix'], writes=['etix'])
                    for m in range(8):
                        for k in range(KC):
                            T.op('pe', lambda: PE.matmul(psg[:, 0:SW], Wg_sb[:, k, m * 128:(m + 1) * 128], XT[:, k, :], start=(k == 0), stop=(k == KC - 1)),
                                 reads=['Wg', 'eXT'], writes=['epsg'])
                        for k in range(KC):
                            T.op('pe', lambda: PE.matmul(psu[:, 0:SW], Wu_sb[:, k, m * 128:(m + 1) * 128], XT[:, k, :], start=(k == 0), stop=(k == KC - 1)),
                                 reads=['Wu', 'eXT'], writes=['epsu'])
                        T.op('act', lambda: A.activation(gs[:], psg[:, 0:SW], AF.Silu), reads=['epsg', 'egs'], writes=['egs'])
                        T.op('dve', lambda: V.tensor_tensor(hid[:, m, :], gs[:], psu[:, 0:SW], op=ALU.mult), reads=['egs', 'epsu', 'ehid'], writes=['ehid'])
                    for j in range(NSJ):
                        y_ = Ysb[yi % 2]; yn = f"eY{yi % 2}"; yi += 1
                        for ct in range(4):
                            for f in range(8):
                                T.op('pe', lambda: PE.matmul(psy[ct][:], hid[:, f, j * 128:(j + 1) * 128], Wd_sb[:, f, ct * 512:(ct + 1) * 512], start=(f == 0), stop=(f == 7)),
                                     reads=['ehid', 'Wd'], writes=[f"epsy{ct}"])
                            T.op('dve', lambda: V.scalar_tensor_tensor(y_[:, ct * 512:(ct + 1) * 512], psy[ct][:], side[:, j, e:e + 1], gtf[:, ct * 512:(ct + 1) * 512], ALU.mult, ALU.mult),
                                 reads=[f"epsy{ct}", 'eside', 'gtf', yn], writes=[yn])
                        T.idma(acc, bass.IndirectOffsetOnAxis(ap=tix[:, j:j + 1], axis=0), y_[:], None, reads=[yn, 'etix', 'acc'], writes=['acc'], compute_op=ALU.add)
            T.barrier()
            S.close()

        def phase_pool(xin, xout):
            S = Scope(nc, T)
            geff, shr = mod_rows(S, 1, 0, 1, 0, 2, "pm")
            xt = [S.sb(f"px{i}", [128, D]) for i in range(2)]
            hh = S.sb("ph", [128, D]); hTt = S.sb("phT", [128, KC, 512])
            pst = [S.ps(f"pps{i}", [128, 512]) for i in range(4)]
            pi = 0
            pre = Pre([(xt[b % 2][:], f"px{b % 2}", xin[b * 128:(b + 1) * 128, :]) for b in range(NBLK)])
            for c0 in range(0, SEQ, 512):
                for j in range(4):
                    t0 = c0 + j * 128
                    x_ = xt[j % 2]; xn = f"px{j % 2}"
                    pre.get(t0 // 128)
                    rms_tok(S, x_, xn, 128, "pm", geff, shr, hh, 'ph')
                    for kq in range(4):
                        pp = pst[pi % 4]; pn = f"pps{pi % 4}"; pi += 1
                        for k4 in range(4):
                            k = kq * 4 + k4
                            T.op('pe', lambda: PE.transpose(pp[:, k4 * 128:(k4 + 1) * 128], hh[:, k * 128:(k + 1) * 128], ident), reads=['ph', 'cst'], writes=[pn])
                        o_ = hTt[:, kq * 4:(kq + 1) * 4, j * 128:(j + 1) * 128]
                        i_ = pp[:].rearrange("p (k n) -> p k n", n=128)
                        if pi % 2 == 0:
                            T.op('act', lambda: A.activation(o_, i_, AF.Copy), reads=[pn, 'phT'], writes=['phT'])
                        else:
                            T.op('dve', lambda: V.tensor_copy(o_, i_), reads=[pn, 'phT'], writes=['phT'])
                T.dma('sp', hT1[:, c0:c0 + 512].rearrange("(k p) n -> p k n", p=128), hTt[:], reads=['phT'])
            T.barrier()
            S.close()
            S = Scope(nc, T)
            pw_sb = S.sb("pw_sb", [128, 4, 4, 512], BF16)
            pws = S.sb("pws", [128, 512])
            for cg in range(4):
                for kk in range(4):
                    T.dma('sp', pws[:], pool_w[cg, kk * 128:(kk + 1) * 128, :], reads=['pws'], writes=['pws'])
                    T.op('dve', lambda: V.tensor_copy(pw_sb[:, cg, kk, :], pws[:]), reads=['pws', 'pw_sb'], writes=['pw_sb'])
            sg = S.sb("psg_", [128, D]); sg2 = S.sb("psg2", [128, D])
            bcast_row(sg[:], nrm[5], 'psg_')
            bcast_row(sg2[:], modrow[1, 0:1, 2 * D:3 * D], 'psg2')
            T.op('dve', lambda: V.tensor_tensor(sg[:], sg[:], sg2[:], op=ALU.mult), reads=['psg_', 'psg2'], writes=['psg_'])
            HW_ = 528
            hh2 = S.sb("ph2", [128, KC, HW_]); ic = S.sb("pic", [128, 4, 512])
            sa = S.sb("psa", [128, HW_]); sb_ = S.sb("psb", [128, HW_])
            dT = S.sb("pdT", [128, KC, 512], BF16)
            xt = [S.sb(f"qx{i}", [128, D]) for i in range(2)]
            o1 = [S.sb(f"qo{i}", [128, D]) for i in range(2)]
            ps = [S.ps(f"qps{i}", [128, 512]) for i in range(8)]
            bi = 0
            pre = Pre([(xt[b % 2][:], f"qx{b % 2}", xin[b * 128:(b + 1) * 128, :]) for b in range(NBLK)])
            for c0 in range(0, SEQ, 512):
                lo_ = max(c0 - 8, 0); hi_ = min(c0 + 520, SEQ)
                if c0 == 0:
                    T.op('pool', lambda: P.memset(hh2[:, :, 0:8], 0.0), reads=['ph2'], writes=['ph2'])
                if c0 + 520 > SEQ:
                    T.op('pool', lambda: P.memset(hh2[:, :, 520:528], 0.0), reads=['ph2'], writes=['ph2'])
                T.dma('sp', hh2[:, :, lo_ - (c0 - 8):hi_ - (c0 - 8)], hT1[:, lo_:hi_].rearrange("(k p) n -> p k n", p=128), reads=['ph2'], writes=['ph2'])
                for wi_ in range(4):
                    T.dma('sp', ic[:, wi_, :], invcnt[wi_, :, c0:c0 + 512].partition_broadcast(128), reads=['pic'], writes=['pic'])
                for k in range(KC):
                    cg = k // 4
                    h_ = hh2[:, k, :]
                    T.op('dve', lambda: V.tensor_tensor(sa[:, 1:528], h_[:, 0:527], h_[:, 1:528], op=ALU.add), reads=['ph2', 'psa'], writes=['psa'])
                    cur_, curn, oth, othn = sa, 'psa', sb_, 'psb'
                    if cg >= 1:
                        T.op('pool', lambda: P.tensor_tensor(oth[:, 2:526], cur_[:, 1:525], cur_[:, 3:527], op=ALU.add), reads=[curn, othn], writes=[othn])
                        cur_, curn, oth, othn = oth, othn, cur_, curn
                    if cg >= 2:
                        T.op('dve', lambda: V.tensor_tensor(oth[:, 4:524], cur_[:, 2:522], cur_[:, 6:526], op=ALU.add), reads=[curn, othn], writes=[othn])
                        cur_, curn, oth, othn = oth, othn, cur_, curn
                    if cg >= 3:
                        T.op('pool', lambda: P.tensor_tensor(oth[:, 8:520], cur_[:, 4:516], cur_[:, 12:524], op=ALU.add), reads=[curn, othn], writes=[othn])
                        cur_, curn, oth, othn = oth, othn, cur_, curn
                    T.op('dve', lambda: V.tensor_tensor(oth[:, 8:520], cur_[:, 8:520], ic[:, cg, :], op=ALU.mult), reads=[curn, 'pic', othn], writes=[othn])
                    T.op('pool', lambda: P.tensor_tensor(dT[:, k, :], oth[:, 8:520], h_[:, 8:520], op=ALU.subtract), reads=[othn, 'ph2', 'pdT'], writes=['pdT'])
                for j in range(4):
                    t0 = c0 + j * 128
                    x_ = xt[bi % 2]; xn = f"qx{bi % 2}"; o_ = o1[bi % 2]; on = f"qo{bi % 2}"
                    pre.get(bi)
                    for cg in range(4):
                        pp = ps[(bi * 4 + cg) % 8]; pn = f"qps{(bi * 4 + cg) % 8}"
                        for kk in range(4):
                            T.op('pe', lambda: PE.matmul(pp[:], dT[:, cg * 4 + kk, j * 128:(j + 1) * 128], pw_sb[:, cg, kk, :], start=(kk == 0), stop=(kk == 3)),
                                 reads=['pdT', 'pw_sb'], writes=[pn])
                        T.op('dve', lambda: V.tensor_tensor(o_[:, cg * 512:(cg + 1) * 512], pp[:], sg[:, cg * 512:(cg + 1) * 512], op=ALU.mult),
                             reads=[pn, 'psg_', on], writes=[on])
                    T.op('pool', lambda: P.tensor_tensor(o_[:], o_[:], x_[:], op=ALU.add), reads=[on, xn], writes=[on])
                    T.dma('sp', xout[t0:t0 + 128, :], o_[:], reads=[on])
                    bi += 1
            T.barrier()
            S.close()

        def phase_final(xin):
            S = Scope(nc, T)
            nf = S.sb("fnf", [128, D])
            bcast_row(nf[:], nrm[4], 'fnf')
            xt = [S.sb(f"fx{i}", [128, D]) for i in range(2)]
            ot = [S.sb(f"fo{i}", [128, D]) for i in range(2)]
            pre = Pre([(xt[b % 2][:], f"fx{b % 2}", xin[b * 128:(b + 1) * 128, :]) for b in range(NBLK)])
            for b in range(NBLK):
                t0 = b * 128
                x_ = xt[b % 2]; xn = f"fx{b % 2}"; o_ = ot[b % 2]; on = f"fo{b % 2}"
                pre.get(b)
                rms_tok(S, x_, xn, 128, 'fnf', nf, None, o_, on)
                T.dma('sp', out[t0:t0 + 128, :], o_[:], reads=[on], writes=['out'])
            T.barrier()
            S.close()

        stages = [("mod", phase_mod), ("n0", phase_n0)] + [(f"mix{g}", (lambda g=g: mixer_group(g))) for g in range(4)] + [
            ("b", phase_b), ("moe0", lambda: phase_moe(0, x1, acc0)), ("pool", lambda: phase_pool(acc0, x3)),
            ("moe1", lambda: phase_moe(1, x3, acc1)), ("final", lambda: phase_final(acc1))]
        for name, fn in stages:
            fn()
            if upto == name:
                break
        T.finish()
    return nc


def _pcol(v):
    v = np.asarray(v, np.float32)
    return np.ascontiguousarray(v.reshape(-1, 128).T)


def _partner_perm():
    d = np.arange(64)
    return np.where((d % 32) < 16, d + 16, d - 16)


def _consts():
    c = np.zeros((128, 9, 128), np.float32)
    r = np.arange(128)[:, None]; q = np.arange(128)[None, :]
    s = r % 64; t = q % 64
    c[:, 0, :] = (r == q)
    c[:, 1, :] = ((r // 64) == (q // 64))
    c[:, 2, :] = np.where(q < 64, s < t, s <= t)
    c[:, 3, :] = np.where(q < 64, s <= t, s < t)
    c[:, 4, :] = ((r // 64) == (q // 64))
    n = np.arange(64)[None, :]
    c[:, 5, 0:64] = (s == n)
    c[:, 5, 64:128] = (s == 63 - n)
    c[:, 6, 0] = (np.arange(128) < 64)
    c[:, 6, 1] = (np.arange(128) >= 64)
    c[:, 7, :] = (r >= q)
    c[:, 8, :] = (r <= q)
    return c


def _rope_tables(SEQ, grid_w=64, theta=10000.0):
    rows = SEQ // grid_w
    row = np.repeat(np.arange(rows, dtype=np.float32), grid_w)
    col = np.tile(np.arange(grid_w, dtype=np.float32), rows)
    n_freq = 16
    inv = (np.float32(theta) ** (-np.arange(n_freq, dtype=np.float32) / np.float32(n_freq))).astype(np.float32)
    ar = (row[:, None] * inv).astype(np.float32); ac = (col[:, None] * inv).astype(np.float32)
    cosT = np.zeros((128, SEQ), np.float32); sinT = np.zeros((128, SEQ), np.float32)
    for p in range(128):
        d = p % 64
        ang = ar[:, d % 16] if d < 32 else ac[:, d % 16]
        cosT[p] = np.cos(ang)
        sinT[p] = np.sin(ang) * (-1.0 if (d % 32) < 16 else 1.0)
    return cosT, sinT


def prep_stageA(inp, mod_x, mod_c, b, g, SEQ, CTX, weights_only=False):
    f = np.float32
    m = {}
    if not weights_only:
        x = np.asarray(inp["x"][b], f); ctx = np.asarray(inp["ctx"][b], f)
        m["xT"] = np.ascontiguousarray(x.T); m["xTr"] = np.ascontiguousarray(x[::-1].T)
        m["cT"] = np.ascontiguousarray(ctx.T); m["cTr"] = np.ascontiguousarray(ctx[::-1].T)
        mx = np.asarray(mod_x, f).reshape(6, D); mc = np.asarray(mod_c, f).reshape(6, D)
        m["modv"] = np.ascontiguousarray(np.stack([_pcol(inp["norm_mix"][0]), _pcol(mx[1]), _pcol(mx[0]), _pcol(mc[1]), _pcol(mc[0])], axis=1))
    w = np.asarray(inp["w_in"][0], f)
    hs = slice(256 * g, 256 * g + 256)
    perm = _partner_perm()
    qcols = 3488 + 256 * g + np.arange(256)
    qpcols = 3488 + 256 * g + (np.arange(4)[:, None] * 64 + perm[None, :]).reshape(-1)
    kcols = 4512 + 64 * g + np.arange(64); kpcols = 4512 + 64 * g + perm
    vcols = 4768 + 64 * g + np.arange(64)
    rkv = np.concatenate([np.arange(0, 1024)[hs], np.arange(1024, 2048)[hs], np.arange(2048, 3072)[hs]])
    colsf = np.concatenate([rkv, np.arange(3072, 3136), np.arange(3200, 3264), np.arange(3328, 3488), qcols, qpcols, kcols, kpcols, vcols])
    colsb = np.concatenate([rkv, np.arange(3136, 3200), np.arange(3264, 3328)])
    assert len(colsf) == NF and len(colsb) == NB
    m["Wf"] = np.ascontiguousarray(w[:, colsf]); m["Wb"] = np.ascontiguousarray(w[:, colsb])
    mu = np.asarray(inp["shift_mu"][0], f)
    vec = np.zeros((128, 32), f)
    for p in range(2):
        sl = slice(256 * g + 128 * p, 256 * g + 128 * p + 128)
        for qi in range(3):
            vec[:, 3 * p + qi] = mu[qi * 1024:(qi + 1) * 1024][sl]
        for d in range(2):
            vec[:, 8 + 2 * d + p] = np.asarray(inp["decay_w0"][0][d], f)[sl]
            vec[:, 12 + 2 * d + p] = np.asarray(inp["iclr_a0"][0][d], f)[sl]
        vec[:, 16 + p] = np.asarray(inp["k_k"][0], f)[sl]; vec[:, 18 + p] = np.asarray(inp["k_a"][0], f)[sl]
        vec[:, 20 + p] = np.asarray(inp["r_k"][0], f)[sl]; vec[:, 22 + p] = np.asarray(inp["ln_w"][0], f)[sl]
        vec[:, 24 + p] = np.asarray(inp["ln_b"][0], f)[sl]
    for d in range(2):
        vec[0:64, 6 + d] = mu[3072 + 64 * d:3072 + 64 * d + 64]
        vec[64:128, 6 + d] = mu[3200 + 64 * d:3200 + 64 * d + 64]
    m["vec"] = vec
    w2a2 = np.zeros((128, 2, 2, 128), f)
    for d in range(2):
        for p in range(2):
            sl = slice(256 * g + 128 * p, 256 * g + 128 * p + 128)
            w2a2[0:64, d, p, :] = np.asarray(inp["decay_w2"][0][d], f)[:, sl]
            w2a2[64:128, d, p, :] = np.asarray(inp["iclr_a2"][0][d], f)[:, sl]
    m["w2a2"] = w2a2
    g2 = np.asarray(inp["gate_g2"][0], f)
    m["g2a"] = np.ascontiguousarray(g2[0:128, hs]); m["g2b"] = np.ascontiguousarray(g2[128:160, hs])
    mg = np.zeros((128, 2), f); mg[:, 0] = mu[3328:3456]; mg[0:32, 1] = mu[3456:3488]
    m["mugd"] = mg
    if not weights_only:
        cosT, sinT = _rope_tables(SEQ)
        m["cosT"] = cosT; m["sinT"] = sinT
    m["sinkb"] = np.ascontiguousarray(np.broadcast_to(np.asarray(inp["sink"][0], f)[4 * g:4 * g + 4][None, :], (128, 4)))
    m["cst"] = _consts()
    return m


def _consts2():
    c = np.zeros((128, 4, 128), np.float32)
    r = np.arange(128)[:, None]; q = np.arange(128)[None, :]
    c[:, 0, :] = (r == 127 - q)
    c[:, 1, :] = ((r // 8) == (q // 8))
    c[:, 2, :] = ((r // 8) == (q // 8)) & (r < q)
    return c


def prep_all(inp, b, SEQ, CTX):
    f = np.float32
    m = {}
    m["x"] = np.ascontiguousarray(np.asarray(inp["x"][b], f)); m["ctx"] = np.ascontiguousarray(np.asarray(inp["ctx"][b], f))
    m["cvec"] = np.ascontiguousarray(np.stack([_pcol(inp["c"][b]), _pcol(inp["c_ctx"])], axis=2))
    m["ada_w"] = np.asarray(inp["ada_w"], f); m["ada_b"] = np.asarray(inp["ada_b"], f).reshape(2, 1, 6 * D)
    m["nrm"] = np.stack([np.asarray(inp["norm_mix"][0], f), np.asarray(inp["norm_ffn"][0], f), np.asarray(inp["norm_mix"][1], f),
                         np.asarray(inp["norm_ffn"][1], f), np.asarray(inp["norm_final"], f), np.asarray(inp["pool_scale"][0], f)]).reshape(6, 1, D)
    zero_mod = np.zeros(6 * D, f)
    parts = [prep_stageA(inp, zero_mod, zero_mod, b, g, SEQ, CTX, weights_only=True) for g in range(4)]
    m["Wf_all"] = np.stack([p["Wf"] for p in parts]); m["Wb_all"] = np.stack([p["Wb"] for p in parts])
    m["vec_all"] = np.stack([p["vec"] for p in parts]); m["w2a2_all"] = np.stack([p["w2a2"] for p in parts])
    m["g2a_all"] = np.stack([p["g2a"] for p in parts]); m["g2b_all"] = np.stack([p["g2b"] for p in parts])
    m["mugd"] = parts[0]["mugd"]; m["sinkb_all"] = np.stack([p["sinkb"] for p in parts])
    m["cosT"], m["sinT"] = _rope_tables(SEQ)
    m["cst"] = _consts(); m["cst2"] = _consts2()
    m["w_out"] = np.asarray(inp["w_out"][0], f)
    m["pool_w"] = np.asarray(inp["pool_w"][0], f)
    pos = np.arange(SEQ)
    ic = np.zeros((4, 1, SEQ), f)
    for i, w in enumerate((2, 4, 8, 16)):
        lo = np.clip(pos - w // 2, 0, SEQ); hi = np.clip(pos + w // 2, 0, SEQ)
        ic[i, 0] = 1.0 / (hi - lo).astype(f)
    m["invcnt"] = ic
    m["router_w"] = np.asarray(inp["router_w"], f)
    m["exp_w_gate"] = np.asarray(inp["exp_w_gate"], f); m["exp_w_up"] = np.asarray(inp["exp_w_up"], f); m["exp_w_down"] = np.asarray(inp["exp_w_down"], f)
    return m


def kernel(**inputs):
    SEQ, CTX = 16384, 256
    nc = build_all(SEQ, CTX)
    maps = [prep_all(inputs, b, SEQ, CTX) for b in range(2)]
    res = run_bass_kernel_spmd(nc, maps, core_ids=[0, 1])
    return np.stack([np.asarray(res.results[b]["out"], np.float32) for b in range(2)])
```
